# Optimizing a Trainium2 kernel written in Bass

```python
import math
import jax
import jax.numpy as jnp
from jax import lax
import numpy as np

D_MODEL = 2048
BATCH = 2
SEQ = 4096
DEPTH = 2

GRID_W = 64
CTX_LEN = 256
CHUNK = 64
SHORT_CONV = 5
NORM_EPS = 1e-6
N_MOD = 6
N_BRANCH = 3

M_WIDTH = D_MODEL // 2
M_HEAD = 64
M_HEADS = M_WIDTH // M_HEAD
M_GROUPS = 2
M_STATE = 128
R_WIDTH = D_MODEL // 2
R_HEAD = 64
R_HEADS = R_WIDTH // R_HEAD
R_DECAY_LORA = 64
R_ICLR_LORA = 64
R_GATE_LORA = 128
R_GN_EPS = 64e-5
R_DECAY_SCALE = math.exp(-0.5)
G_WIDTH = D_MODEL // 2
G_HEAD = 128
G_HEADS = G_WIDTH // G_HEAD
MOE_GROUPS = 4
MOE_PER_GROUP = 8
MOE_EXPERTS = MOE_GROUPS * MOE_PER_GROUP
MOE_TOPK = 2
MOE_FF = D_MODEL // 4

IN_COLS = (
    ("m_z", M_WIDTH), ("m_x", M_WIDTH), ("m_B", M_GROUPS * M_STATE), ("m_C", M_GROUPS * M_STATE),
    ("m_dt", 2 * M_HEADS),
    ("r_r", R_WIDTH), ("r_k", R_WIDTH), ("r_v", R_WIDTH), ("r_w", 2 * R_DECAY_LORA),
    ("r_a", R_ICLR_LORA), ("r_g", R_GATE_LORA),
    ("g_q", G_WIDTH), ("g_k", G_WIDTH), ("g_v", G_WIDTH), ("g_a", 2 * G_HEADS), ("g_b", 2 * G_HEADS),
    ("g_g", G_WIDTH),
    ("gate", N_BRANCH * D_MODEL),
)
IN_WIDTH = sum(w for _, w in IN_COLS)

kernel_name = "hybrid_ssd_rwkv7_gdn_hmoe_prefix_dit"


def rms_norm(x, g):
    xf = x.astype(jnp.float32)
    y = xf * lax.rsqrt(jnp.mean(xf * xf, axis=-1, keepdims=True) + NORM_EPS)
    return y.astype(x.dtype) * g


def l2_normalize(x):
    xf = x.astype(jnp.float32)
    return (xf * lax.rsqrt(jnp.sum(xf * xf, axis=-1, keepdims=True) + 1e-6)).astype(x.dtype)


def modulate(h, shift, scale):
    return h * (1 + scale) + shift


def split_cols(p):
    out, start = {}, 0
    for name, width in IN_COLS:
        out[name] = p[..., start:start + width]
        start += width
    return out


def centred_dwconv(x, w, b=None):
    k, ch = w.shape
    y = lax.conv_general_dilated(x, w[:, None, :], window_strides=(1,), padding=((k // 2, k // 2),),
                                 dimension_numbers=("NWC", "WIO", "NWC"), feature_group_count=ch)
    return y if b is None else y + b


def centred_shift(x, mu):
    prev = jnp.pad(x[:, :-1], ((0, 0), (1, 0), (0, 0)))
    nxt = jnp.pad(x[:, 1:], ((0, 0), (0, 1), (0, 0)))
    return x + mu[0] * (prev - x) + mu[1] * (nxt - x)


def raster_to_column_major(a):
    b, t, ch = a.shape
    rows = t // GRID_W
    return a.reshape(b, rows, GRID_W, ch).transpose(0, 2, 1, 3).reshape(b, t, ch)


def column_major_to_raster(a):
    b, t, ch = a.shape
    rows = t // GRID_W
    return a.reshape(b, GRID_W, rows, ch).transpose(0, 2, 1, 3).reshape(b, t, ch)


def to_chunks(a):
    b, t, h = a.shape[:3]
    a = a.reshape(b, t // CHUNK, CHUNK, h, *a.shape[3:])
    return jnp.moveaxis(a, (1, 3), (0, 2))


def from_chunks(a):
    a = jnp.moveaxis(a, (0, 2), (1, 3))
    b, nc, q, h, d = a.shape
    return a.reshape(b, nc * q, h, d)


def chunk_masks():
    idx = jnp.arange(CHUNK)
    return idx[:, None] >= idx[None, :], idx[:, None] > idx[None, :]


def ssd_chunked(args, s0):
    xdt, da, bm, cm = (a.astype(jnp.float32) for a in args)
    xc, ac, bc, cc = (to_chunks(a) for a in (xdt, da, bm, cm))
    lower, _ = chunk_masks()
    lc = jnp.cumsum(ac, axis=-1)
    decay = jnp.exp(jnp.where(lower, lc[..., :, None] - lc[..., None, :], -jnp.inf))
    scores = jnp.einsum("cbhis,cbhjs->cbhij", cc, bc) * decay
    y_intra = jnp.einsum("cbhij,cbhjp->cbhip", scores, xc)
    c_dec = cc * jnp.exp(lc)[..., None]
    b_end = bc * jnp.exp(lc[..., -1:] - lc)[..., None]
    a_end = jnp.exp(lc[..., -1])

    def step(state, inp):
        y_i, c_i, b_i, x_i, a_i = inp
        y = y_i + jnp.einsum("bhis,bhps->bhip", c_i, state)
        state = state * a_i[..., None, None] + jnp.einsum("bhjs,bhjp->bhps", b_i, x_i)
        return state, y

    s_fin, yc = lax.scan(step, s0, (y_intra, c_dec, b_end, xc, a_end))
    return from_chunks(yc), s_fin


def gdn_chunked(args, s0):
    q, k, v, g, beta = (a.astype(jnp.float32) for a in args)
    qc, kc, vc, gc, bc = (to_chunks(a) for a in (q, k, v, g, beta))
    lower, strict = chunk_masks()
    gcum = jnp.cumsum(gc, axis=-1)
    decay = jnp.exp(jnp.where(lower, gcum[..., :, None] - gcum[..., None, :], -jnp.inf))
    kb = kc * bc[..., None]
    a_mat = jnp.einsum("cbhid,cbhjd->cbhij", kb, kc) * decay * strict
    eye = jnp.eye(CHUNK, dtype=a_mat.dtype)
    rhs = jnp.concatenate([vc * bc[..., None], kb * jnp.exp(gcum)[..., None]], axis=-1)
    sol = lax.linalg.triangular_solve(a_mat + eye, rhs, left_side=True, lower=True, unit_diagonal=True)
    dv = vc.shape[-1]
    u, w = sol[..., :dv], sol[..., dv:]
    qk = jnp.einsum("cbhid,cbhjd->cbhij", qc, kc) * decay
    q_dec = qc * jnp.exp(gcum)[..., None]
    k_end = kc * jnp.exp(gcum[..., -1:] - gcum)[..., None]
    g_end = jnp.exp(gcum[..., -1])

    def step(state, inp):
        qk_i, u_i, w_i, qd_i, ke_i, ge_i = inp
        v_new = u_i - jnp.einsum("bhid,bhdv->bhiv", w_i, state)
        o = jnp.einsum("bhid,bhdv->bhiv", qd_i, state) + jnp.einsum("bhij,bhjv->bhiv", qk_i, v_new)
        state = state * ge_i[..., None, None] + jnp.einsum("bhjd,bhjv->bhdv", ke_i, v_new)
        return state, o

    s_fin, oc = lax.scan(step, s0, (qk, u, w, q_dec, k_end, g_end))
    return from_chunks(oc), s_fin


def rwkv7_scan(args, s0):
    tm = tuple(jnp.moveaxis(a.astype(jnp.float32), 1, 0) for a in args)

    def step(state, inp):
        r_t, w_t, k_t, v_t, kk_t, a_t = inp
        sa = jnp.einsum("bhvk,bhk->bhv", state, -kk_t)
        state = (state * w_t[:, :, None, :] + sa[..., None] * (kk_t * a_t)[:, :, None, :]
                 + v_t[..., None] * k_t[:, :, None, :])
        return state, jnp.einsum("bhvk,bhk->bhv", state, r_t)

    s_fin, y = lax.scan(step, s0, tm)
    return jnp.moveaxis(y, 0, 1), s_fin


def bidirectional(scan_fn, ctx_dirs, lat_dirs, s0):
    y_ctx, y_lat = [], []
    for d in range(2):
        ca, la = ctx_dirs[d], lat_dirs[d]
        if d == 1:
            ca = tuple(jnp.flip(a, axis=1) for a in ca)
            la = tuple(jnp.flip(a, axis=1) for a in la)
        yc, s_ctx = scan_fn(ca, s0)
        yl, _ = scan_fn(la, s_ctx)
        if d == 1:
            yc, yl = jnp.flip(yc, axis=1), jnp.flip(yl, axis=1)
        y_ctx.append(yc)
        y_lat.append(yl)
    dt = ctx_dirs[0][0].dtype
    return (y_ctx[0] + y_ctx[1]).astype(dt), (y_lat[0] + y_lat[1]).astype(dt)


def mamba_branch(pc, pl, conv_w, conv_b, a_log, dt_bias, d_skip, norm_g, ctx_out):
    heads_per_group = M_HEADS // M_GROUPS
    gs = M_GROUPS * M_STATE
    a_neg = -jnp.exp(a_log.astype(jnp.float32))

    def prep(p, column_major):
        xbc = jnp.concatenate([p["m_x"], p["m_B"], p["m_C"]], axis=-1)
        dt_raw = p["m_dt"]
        if column_major:
            xbc, dt_raw = raster_to_column_major(xbc), raster_to_column_major(dt_raw)
        xbc = jax.nn.silu(centred_dwconv(xbc, conv_w, conv_b))
        b, t, _ = xbc.shape
        xs = xbc[..., :M_WIDTH].reshape(b, t, M_HEADS, M_HEAD)
        bm = jnp.repeat(xbc[..., M_WIDTH:M_WIDTH + gs].reshape(b, t, M_GROUPS, M_STATE), heads_per_group, axis=2)
        cm = jnp.repeat(xbc[..., M_WIDTH + gs:].reshape(b, t, M_GROUPS, M_STATE), heads_per_group, axis=2)
        dt = jax.nn.softplus(dt_raw.reshape(b, t, 2, M_HEADS) + dt_bias)
        dirs = tuple((xs * dt[:, :, d, :, None], dt[:, :, d] * a_neg[d], bm, cm) for d in range(2))
        return xs, dirs

    xs_c, dirs_c = prep(pc, False)
    xs_l, dirs_l = prep(pl, True)
    s0 = jnp.zeros((xs_l.shape[0], M_HEADS, M_HEAD, M_STATE), jnp.float32)
    y_c, y_l = bidirectional(ssd_chunked, dirs_c, dirs_l, s0)

    def finish(y, xs, z, column_major):
        b, t = y.shape[:2]
        y = (y + xs * d_skip[:, None]).reshape(b, t, M_WIDTH)
        if column_major:
            y = column_major_to_raster(y)
        yz = (y * jax.nn.silu(z)).reshape(b, t, M_GROUPS, M_WIDTH // M_GROUPS)
        return rms_norm(yz, norm_g.reshape(M_GROUPS, M_WIDTH // M_GROUPS)).reshape(b, t, M_WIDTH)

    out_c = finish(y_c, xs_c, pc["m_z"], False) if ctx_out else None
    return out_c, finish(y_l, xs_l, pl["m_z"], True)


def rwkv_branch(pc, pl, shift_mu, w0, w_up, a0, a_up, g_up, k_k, k_a, r_k, ln_g, ln_b, ctx_out):
    def prep(p):
        rkv = centred_shift(jnp.concatenate([p["r_r"], p["r_k"], p["r_v"]], axis=-1), shift_mu)
        b, t, _ = rkv.shape
        r, k, v = jnp.split(rkv, 3, axis=-1)
        decay_in = w0 + jnp.einsum("btdl,dlr->btdr", jnp.tanh(p["r_w"].reshape(b, t, 2, R_DECAY_LORA)), w_up)
        decay = jnp.exp(-R_DECAY_SCALE * jax.nn.sigmoid(decay_in))
        a = jax.nn.sigmoid(a0 + p["r_a"] @ a_up)
        g = jax.nn.sigmoid(p["r_g"]) @ g_up
        heads = lambda u: u.reshape(b, t, R_HEADS, R_HEAD)
        kk = l2_normalize(heads(k * k_k))
        k = k * (1 + (a - 1) * k_a)
        r, k, v, a = heads(r), heads(k), heads(v), heads(a)
        dirs = tuple((r, heads(decay[:, :, d]), k, v, kk, a) for d in range(2))
        return (r, k, v, g), dirs

    aux_c, dirs_c = prep(pc)
    aux_l, dirs_l = prep(pl)
    s0 = jnp.zeros((aux_l[0].shape[0], R_HEADS, R_HEAD, R_HEAD), jnp.float32)
    y_c, y_l = bidirectional(rwkv7_scan, dirs_c, dirs_l, s0)

    def finish(y, aux):
        r, k, v, g = aux
        b, t = y.shape[:2]
        yf = y.astype(jnp.float32)
        mean = jnp.mean(yf, axis=-1, keepdims=True)
        var = jnp.mean(jnp.square(yf - mean), axis=-1, keepdims=True)
        yn = ((yf - mean) * lax.rsqrt(var + R_GN_EPS)).astype(y.dtype)
        yn = yn * ln_g.reshape(R_HEADS, R_HEAD) + ln_b.reshape(R_HEADS, R_HEAD)
        bonus = jnp.sum(r * k * r_k, axis=-1, keepdims=True) * v
        return (yn + bonus).reshape(b, t, R_WIDTH) * g

    out_c = finish(y_c, aux_c) if ctx_out else None
    return out_c, finish(y_l, aux_l)


def gdn_branch(pc, pl, conv_w, a_log, dt_bias, norm_g, ctx_out):
    a_neg = -jnp.exp(a_log.astype(jnp.float32))

    def prep(p):
        qkv = jax.nn.silu(centred_dwconv(jnp.concatenate([p["g_q"], p["g_k"], p["g_v"]], axis=-1), conv_w))
        b, t, _ = qkv.shape
        q, k, v = (u.reshape(b, t, G_HEADS, G_HEAD) for u in jnp.split(qkv, 3, axis=-1))
        q = l2_normalize(q) * G_HEAD ** -0.5
        k = l2_normalize(k)
        log_decay = a_neg * jax.nn.softplus(p["g_a"].reshape(b, t, 2, G_HEADS) + dt_bias)
        beta = jax.nn.sigmoid(p["g_b"].reshape(b, t, 2, G_HEADS))
        return tuple((q, k, v, log_decay[:, :, d], beta[:, :, d]) for d in range(2))

    dirs_c, dirs_l = prep(pc), prep(pl)
    s0 = jnp.zeros((dirs_l[0][0].shape[0], G_HEADS, G_HEAD, G_HEAD), jnp.float32)
    y_c, y_l = bidirectional(gdn_chunked, dirs_c, dirs_l, s0)

    def finish(y, gate):
        b, t = y.shape[:2]
        return (rms_norm(y, norm_g) * jax.nn.silu(gate.reshape(b, t, G_HEADS, G_HEAD))).reshape(b, t, G_WIDTH)

    out_c = finish(y_c, pc["g_g"]) if ctx_out else None
    return out_c, finish(y_l, pl["g_g"])


def merge_branches(p, y_m, y_r, y_g, w_br_m, w_br_r, w_br_g, w_out):
    b, t = y_m.shape[:2]
    gates = jax.nn.sigmoid(p["gate"].reshape(b, t, N_BRANCH, D_MODEL))
    merged = (gates[:, :, 0] * (y_m @ w_br_m) + gates[:, :, 1] * (y_r @ w_br_r)
              + gates[:, :, 2] * (y_g @ w_br_g))
    return merged @ w_out


def hier_moe(h, grp_w, grp_b, exp_w, exp_b, w1, w3, w2):
    b, t, d = h.shape
    hf = h.reshape(b * t, d)
    grp_logits = (hf @ grp_w + grp_b).astype(jnp.float32)
    grp = jnp.argmax(grp_logits, axis=-1)
    p_grp = jnp.max(jax.nn.softmax(grp_logits, axis=-1), axis=-1, keepdims=True)
    grp_1h = jax.nn.one_hot(grp, MOE_GROUPS, dtype=jnp.float32)
    exp_logits = (hf @ exp_w + exp_b).astype(jnp.float32).reshape(-1, MOE_GROUPS, MOE_PER_GROUP)
    in_grp = jnp.einsum("nge,ng->ne", exp_logits, grp_1h)
    top_p, top_i = lax.top_k(jax.nn.softmax(in_grp, axis=-1), MOE_TOPK)
    top_p = top_p / jnp.sum(top_p, axis=-1, keepdims=True) * p_grp
    w_in_grp = jnp.sum(jax.nn.one_hot(top_i, MOE_PER_GROUP, dtype=jnp.float32) * top_p[..., None], axis=1)
    combine = (grp_1h[:, :, None] * w_in_grp[:, None, :]).astype(h.dtype)
    y = jnp.zeros_like(hf)
    for gi in range(MOE_GROUPS):
        sl = slice(gi * MOE_PER_GROUP, (gi + 1) * MOE_PER_GROUP)
        hid = jax.nn.silu(jnp.einsum("nd,edf->nef", hf, w1[sl])) * jnp.einsum("nd,edf->nef", hf, w3[sl])
        y = y + jnp.einsum("nef,efd->nd", hid * combine[:, gi, :, None], w2[sl])
    return y.reshape(b, t, d)


def setup_inputs(seed: int = 0) -> dict:
    key = jax.random.key(seed)
    ks = iter(list(jax.random.split(key, 48)))
    L, D = DEPTH, D_MODEL

    def nrm(shape, scale):
        return jax.random.normal(next(ks), shape, jnp.float32) * scale

    def gain(shape):
        return 1.0 + nrm(shape, 0.02)

    def unif(shape, lo, hi):
        return jax.random.uniform(next(ks), shape, jnp.float32, lo, hi)

    def dt_bias(shape):
        dt = jnp.exp(unif(shape, math.log(1e-3), math.log(1e-1)))
        return dt + jnp.log(-jnp.expm1(-dt))

    m_conv_ch = M_WIDTH + 2 * M_GROUPS * M_STATE
    return {
        "x": nrm((BATCH, SEQ, D), 1.0),
        "c": nrm((BATCH, D), 1.0),
        "ctx": nrm((BATCH, CTX_LEN, D), 1.0),
        "c_ctx": nrm((D,), 1.0),
        "ada_w": nrm((L, D, N_MOD * D), 0.5 * D ** -0.5),
        "ada_b": nrm((L, N_MOD * D), 0.02),
        "norm1_g": gain((L, D)),
        "norm2_g": gain((L, D)),
        "w_in": nrm((L, D, IN_WIDTH), D ** -0.5),
        "m_conv_w": nrm((L, SHORT_CONV, m_conv_ch), SHORT_CONV ** -0.5),
        "m_conv_b": nrm((L, m_conv_ch), 0.02),
        "m_A_log": jnp.log(unif((L, 2, M_HEADS), 1.0, 16.0)),
        "m_dt_bias": dt_bias((L, 2, M_HEADS)),
        "m_D": gain((L, M_HEADS)),
        "m_norm_g": gain((L, M_WIDTH)),
        "r_shift_mu": unif((L, 2, 3 * R_WIDTH), 0.0, 0.5),
        "r_w0": unif((L, 2, R_WIDTH), -4.0, 2.0),
        "r_w_up": nrm((L, 2, R_DECAY_LORA, R_WIDTH), 0.5 * R_DECAY_LORA ** -0.5),
        "r_a0": nrm((L, R_WIDTH), 0.1),
        "r_a_up": nrm((L, R_ICLR_LORA, R_WIDTH), R_ICLR_LORA ** -0.5),
        "r_g_up": nrm((L, R_GATE_LORA, R_WIDTH), R_GATE_LORA ** -0.5),
        "r_k_k": 1.0 + nrm((L, R_WIDTH), 0.1),
        "r_k_a": 1.0 + nrm((L, R_WIDTH), 0.1),
        "r_r_k": nrm((L, R_HEADS, R_HEAD), 0.1),
        "r_ln_g": gain((L, R_WIDTH)),
        "r_ln_b": nrm((L, R_WIDTH), 0.02),
        "g_conv_w": nrm((L, SHORT_CONV, 3 * G_WIDTH), SHORT_CONV ** -0.5),
        "g_A_log": jnp.log(unif((L, 2, G_HEADS), 1.0, 16.0)),
        "g_dt_bias": dt_bias((L, 2, G_HEADS)),
        "g_norm_g": gain((L, G_HEAD)),
        "w_br_m": nrm((L, M_WIDTH, D), M_WIDTH ** -0.5),
        "w_br_r": nrm((L, R_WIDTH, D), R_WIDTH ** -0.5),
        "w_br_g": nrm((L, G_WIDTH, D), G_WIDTH ** -0.5),
        "w_out": nrm((L, D, D), D ** -0.5),
        "moe_grp_w": nrm((L, D, MOE_GROUPS), D ** -0.5),
        "moe_grp_b": nrm((L, MOE_GROUPS), 0.01),
        "moe_exp_w": nrm((L, D, MOE_EXPERTS), D ** -0.5),
        "moe_exp_b": nrm((L, MOE_EXPERTS), 0.01),
        "moe_w1": nrm((L, MOE_EXPERTS, D, MOE_FF), D ** -0.5),
        "moe_w3": nrm((L, MOE_EXPERTS, D, MOE_FF), D ** -0.5),
        "moe_w2": nrm((L, MOE_EXPERTS, MOE_FF, D), MOE_FF ** -0.5),
        "final_norm_g": gain((D,)),
    }


def reference(x, c, ctx, c_ctx, ada_w, ada_b, norm1_g, norm2_g, w_in,
              m_conv_w, m_conv_b, m_A_log, m_dt_bias, m_D, m_norm_g,
              r_shift_mu, r_w0, r_w_up, r_a0, r_a_up, r_g_up, r_k_k, r_k_a, r_r_k, r_ln_g, r_ln_b,
              g_conv_w, g_A_log, g_dt_bias, g_norm_g,
              w_br_m, w_br_r, w_br_g, w_out,
              moe_grp_w, moe_grp_b, moe_exp_w, moe_exp_b, moe_w1, moe_w3, moe_w2, final_norm_g):
    lat, cx = x, ctx
    for l in range(DEPTH):
        ctx_out = l < DEPTH - 1
        mod_l = jnp.split((jax.nn.silu(c) @ ada_w[l] + ada_b[l])[:, None, :], N_MOD, axis=-1)
        mod_c = jnp.split(jax.nn.silu(c_ctx) @ ada_w[l] + ada_b[l], N_MOD, axis=-1)
        hl = modulate(rms_norm(lat, norm1_g[l]), mod_l[0], mod_l[1])
        hc = modulate(rms_norm(cx, norm1_g[l]), mod_c[0], mod_c[1])
        pl, pc = split_cols(hl @ w_in[l]), split_cols(hc @ w_in[l])
        ym_c, ym_l = mamba_branch(pc, pl, m_conv_w[l], m_conv_b[l], m_A_log[l], m_dt_bias[l], m_D[l],
                                  m_norm_g[l], ctx_out)
        yr_c, yr_l = rwkv_branch(pc, pl, r_shift_mu[l], r_w0[l], r_w_up[l], r_a0[l], r_a_up[l], r_g_up[l],
                                 r_k_k[l], r_k_a[l], r_r_k[l], r_ln_g[l], r_ln_b[l], ctx_out)
        yg_c, yg_l = gdn_branch(pc, pl, g_conv_w[l], g_A_log[l], g_dt_bias[l], g_norm_g[l], ctx_out)
        proj = (w_br_m[l], w_br_r[l], w_br_g[l], w_out[l])
        moe_p = (moe_grp_w[l], moe_grp_b[l], moe_exp_w[l], moe_exp_b[l], moe_w1[l], moe_w3[l], moe_w2[l])
        lat = lat + mod_l[2] * merge_branches(pl, ym_l, yr_l, yg_l, *proj)
        lat = lat + mod_l[5] * hier_moe(modulate(rms_norm(lat, norm2_g[l]), mod_l[3], mod_l[4]), *moe_p)
        if ctx_out:
            cx = cx + mod_c[2] * merge_branches(pc, ym_c, yr_c, yg_c, *proj)
            cx = cx + mod_c[5] * hier_moe(modulate(rms_norm(cx, norm2_g[l]), mod_c[3], mod_c[4]), *moe_p)
    return rms_norm(lat, final_norm_g)
```

```python
import math
import contextlib
import numpy as np
import concourse.bass as bass
import concourse.mybir as mybir
from concourse.bass_utils import run_bass_kernel_spmd

F32 = mybir.dt.float32
BF16 = mybir.dt.bfloat16
AF = mybir.ActivationFunctionType
ALU = mybir.AluOpType
AX = mybir.AxisListType


def make_cfg(D=2048, SEQ=4096, CTX=256, DEPTH=2, BATCH=2):
    c = dict(D=D, SEQ=SEQ, CTX=CTX, DEPTH=DEPTH, BATCH=BATCH, GRID_W=64, CHUNK=64)
    c['T'] = CTX + SEQ
    c['MW'] = D // 2; c['MH'] = c['MW'] // 64; c['MG'] = 2; c['MS'] = 128
    c['RW'] = D // 2; c['RH'] = c['RW'] // 64
    c['GW'] = D // 2; c['GH'] = c['GW'] // 128
    c['NE'] = 32; c['FF'] = D // 4
    cols = (("m_z", c['MW']), ("m_x", c['MW']), ("m_B", 256), ("m_C", 256), ("m_dt", 2 * c['MH']),
            ("r_r", c['RW']), ("r_k", c['RW']), ("r_v", c['RW']), ("r_w", 128), ("r_a", 64), ("r_g", 128),
            ("g_q", c['GW']), ("g_k", c['GW']), ("g_v", c['GW']), ("g_a", 2 * c['GH']), ("g_b", 2 * c['GH']),
            ("g_g", c['GW']), ("gate", 3 * D))
    off = {}
    s = 0
    for n, w in cols:
        off[n] = (s, w)
        s += w
    c['off'] = off
    c['INW'] = s
    return c


class Sched:
    NDS = 24
    serialize = False
    ser_engs = ()

    def __init__(self, nc):
        self.nc = nc
        self.eng = {'pe': nc.tensor, 'act': nc.scalar, 'dve': nc.vector, 'pool': nc.gpsimd, 'sp': nc.sync}
        self.prog = {e: [] for e in self.eng}
        self.ccnt = {e: 0 for e in self.eng}
        self.sems = {}
        for e in self.eng:
            self.sems[('c', e)] = nc.alloc_semaphore(name=f"c_{e}")
        self.dcnt = [0] * self.NDS
        for i in range(self.NDS):
            self.sems[('d', i)] = nc.alloc_semaphore(name=f"d_{i}")
        self.drr = 0
        self.seen = {e: {} for e in self.eng}
        self.rec = {}
        self.rows = {}
        self.psum = set()
        self.nops = 0

    def reg_tensor(self, name, shape, space):
        rs = 1
        for s in shape[1:]:
            rs *= s
        self.rows[name] = rs if space != 'dram' else None
        if space == 'ps':
            self.psum.add(name)

    def region(self, ap):
        name = ap.name
        pat = ap.ap
        off = int(ap.offset)
        rs = self.rows[name]
        if name in self.psum:
            return name, (0, 128, 0, rs)
        if rs is None:
            lo = hi = off
            for st, n in pat:
                if n > 1:
                    if st >= 0:
                        hi += st * (n - 1)
                    else:
                        lo += st * (n - 1)
            return name, (0, 1, lo, hi + 1)
        p0 = off // rs
        f0 = off % rs
        npart = pat[0][1]
        lo = hi = f0
        for st, n in pat[1:]:
            if n > 1:
                if st >= 0:
                    hi += st * (n - 1)
                else:
                    lo += st * (n - 1)
        return name, (p0, p0 + npart, lo, hi + 1)

    def dense(self, ap):
        name = ap.name
        if name in self.psum:
            return True
        pat = ap.ap
        rs = self.rows[name]
        n = 1
        for st, c in (pat if rs is None else pat[1:]):
            n *= c if st != 0 else 1
        _, reg = self.region(ap)
        return n == reg[3] - reg[2]

    @staticmethod
    def _ov(a, b):
        return a[0] < b[1] and b[0] < a[1] and a[2] < b[3] and b[2] < a[3]

    @staticmethod
    def _cov(a, b):
        return a[0] <= b[0] and a[1] >= b[1] and a[2] <= b[2] and a[3] >= b[3]

    def _deps(self, reads, writes, me=None):
        deps = {}
        rr = [self.region(a) for a in reads]
        ww = [self.region(a) + (self.dense(a),) for a in writes]
        for name, reg in rr:
            ps = name in self.psum
            for r in self.rec.get(name, ()):
                if (r[1] == 'W' or (ps and r[2] != me)) and self._ov(r[0], reg):
                    deps[r[2]] = max(deps.get(r[2], 0), r[3])
        for name, reg, _dn in ww:
            for r in self.rec.get(name, ()):
                if self._ov(r[0], reg):
                    deps[r[2]] = max(deps.get(r[2], 0), r[3])
        return deps, rr, ww

    def _record(self, rr, ww, key, val):
        for name, reg, dn in ww:
            lst = self.rec.setdefault(name, [])
            if dn:
                lst[:] = [r for r in lst if not self._cov(reg, r[0])]
            lst.append([reg, 'W', key, val])
        for name, reg in rr:
            lst = self.rec.setdefault(name, [])
            for r in lst:
                if r[1] == 'R' and r[2] == key and r[0] == reg:
                    r[3] = max(r[3], val)
                    break
            else:
                lst.append([reg, 'R', key, val])

    def _emit_waits(self, e, deps, skip_self=False):
        for key, val in deps.items():
            if skip_self and key == ('c', e):
                continue
            if key == ('c', 'pe'):
                if e == 'pe':
                    continue
                if self.ccnt['pe'] <= val:
                    self.ccnt['pe'] += 1
                    self.prog['pe'].append(('op', self.dummy_fn, key, 1))
                val = val + 1
            if self.seen[e].get(key, 0) >= val:
                continue
            self.seen[e][key] = val
            self.prog[e].append(('wait', key, val))

    def op(self, e, fn, reads, writes, pe_chain=False):
        deps, rr, ww = self._deps(reads, writes, me=('c', e))
        self._emit_waits(e, deps, skip_self=pe_chain)
        self.ccnt[e] += 1
        key = ('c', e)
        self.prog[e].append(('op', fn, key, 1))
        self._record(rr, ww, key, self.ccnt[e])
        self.nops += 1
        if self.serialize or e in self.ser_engs:
            self.barrier()

    def dma(self, out, in_, q='sp', **kw):
        deps, rr, ww = self._deps([in_], [out])
        k = self.drr
        self.drr = (self.drr + 1) % self.NDS
        key = ('d', k)
        if self.dcnt[k] > 0:
            deps[key] = max(deps.get(key, 0), self.dcnt[k])
        self._emit_waits(q, deps)
        self.dcnt[k] += 16

        def fn(eng, out=out, in_=in_, kw=kw):
            return eng.dma_start(out=out, in_=in_, **kw)
        self.prog[q].append(('op', fn, key, 16))
        self._record(rr, ww, key, self.dcnt[k])
        self.nops += 1
        if self.serialize or 'dma' in self.ser_engs:
            self.barrier()

    def _all_tokens(self):
        final = {}
        for e in self.eng:
            if self.ccnt[e] > 0:
                final[('c', e)] = self.ccnt[e]
        for i in range(self.NDS):
            if self.dcnt[i] > 0:
                final[('d', i)] = self.dcnt[i]
        return final

    def barrier(self, drop=()):
        final = self._all_tokens()
        for e in self.eng:
            self._emit_waits(e, dict(final))
        for n in drop:
            self.rec.pop(n, None)

    def finalize(self, block):
        self._emit_waits('sp', self._all_tokens())
        sems = self.sems

        def mk(e):
            prog = self.prog[e]

            def body(eng):
                for it in prog:
                    if it[0] == 'wait':
                        eng.wait_ge(sems[it[1]], it[2])
                    else:
                        it[1](eng).then_inc(sems[it[2]], it[3])
            return body
        block.tensor(mk('pe'))
        block.scalar(mk('act'))
        block.vector(mk('dve'))
        block.gpsimd(mk('pool'))
        block.sync(mk('sp'))


def _is_ap(x):
    return hasattr(x, 'ap') and hasattr(x, 'offset')


class KB:
    def __init__(self, nc, cfg, es):
        self.nc = nc
        self.cfg = cfg
        self.S = Sched(nc)
        self.uid = 0
        self.rr = 0
        self.pool_eng = 'pool'
        self.dma_queues = ('sp', 'act')
        self.es = es
        dsb = self.sb(es, "dmy_sb", [128, 8], BF16)
        dps = self.ps(es, "dmy_ps", [128, 8])
        self.S.dummy_fn = lambda e: e.matmul(dps[0:8, 0:8], lhsT=dsb[0:8, 0:8], rhs=dsb[0:8, 0:8], start=True, stop=True)
        self.memset(dsb[:], 0.0)
        self.mm(dps[0:8, 0:8], dsb[0:8, 0:8], dsb[0:8, 0:8])

    def dram(self, name, shape, dt=F32, kind=None):
        if kind is None:
            t = self.nc.dram_tensor(name, list(shape), dt)
        else:
            t = self.nc.dram_tensor(name, list(shape), dt, kind=kind)
        self.S.reg_tensor(name, shape, 'dram')
        return t.ap()

    def sb(self, es, name, shape, dt=F32):
        self.uid += 1
        name = f"{name}_{self.uid}"
        t = es.enter_context(self.nc.sbuf_tensor(name, list(shape), dt))
        self.S.reg_tensor(name, shape, 'sb')
        return t

    def ps(self, es, name, shape, dt=F32):
        self.uid += 1
        name = f"{name}_{self.uid}"
        full = [128, 512] if dt == F32 else [128, 1024]
        t = es.enter_context(self.nc.psum_tensor(name, full, dt))
        self.S.reg_tensor(name, full, 'ps')
        n = 1
        for d in shape[1:]:
            n *= d
        assert n <= full[1] and shape[0] <= 128
        v = t[0:shape[0], 0:n]
        if len(shape) == 3:
            v = v.rearrange("p (a b) -> p a b", b=shape[2])
        return v

    def dma(self, out, in_, q=None, **kw):
        if q is None:
            q = self.dma_queues[self.rr % len(self.dma_queues)]
            self.rr += 1
        self.S.dma(out, in_, q=q, **kw)

    def mm(self, out, lhsT, rhs, start=True, stop=True):
        self.S.op('pe', lambda e: e.matmul(out, lhsT=lhsT, rhs=rhs, start=start, stop=stop),
                  [lhsT, rhs] + ([] if start else [out]), [out], pe_chain=not start)

    def tr(self, out, in_, ident):
        self.S.op('pe', lambda e: e.transpose(out, in_, ident), [in_, ident], [out])

    def act(self, out, in_, func, bias=None, scale=None, accum_out=None, eng='act'):
        kw = {}
        rd = [in_]
        wr = [out]
        if bias is not None:
            kw['bias'] = bias
            if _is_ap(bias):
                rd.append(bias)
        if scale is not None:
            kw['scale'] = scale
            if _is_ap(scale):
                rd.append(scale)
        if accum_out is not None:
            kw['accum_out'] = accum_out
            wr.append(accum_out)
        self.S.op('act', lambda e: e.activation(out=out, in_=in_, func=func, **kw), rd, wr)

    def tt(self, out, in0, in1, op, eng='dve'):
        self.S.op(eng, lambda e: e.tensor_tensor(out=out, in0=in0, in1=in1, op=op), [in0, in1], [out])

    def ts(self, out, in0, s1, op0, s2=None, op1=None, eng='dve', accum_out=None):
        rd = [in0] + [s for s in (s1, s2) if _is_ap(s)]
        kw = {}
        wr = [out]
        if op1 is not None:
            kw['op1'] = op1
        if accum_out is not None:
            kw['accum_out'] = accum_out
            wr.append(accum_out)
        self.S.op(eng, lambda e: e.tensor_scalar(out=out, in0=in0, scalar1=s1, scalar2=s2, op0=op0, **kw), rd, wr)

    def stt(self, out, in0, scalar, in1, op0, op1):
        rd = [in0, in1] + ([scalar] if _is_ap(scalar) else [])
        self.S.op('dve', lambda e: e.scalar_tensor_tensor(out=out, in0=in0, scalar=scalar, in1=in1, op0=op0, op1=op1),
                  rd, [out])

    def red(self, out, in_, op, axis=AX.X):
        self.S.op('dve', lambda e: e.tensor_reduce(out=out, in_=in_, axis=axis, op=op), [in_], [out])

    def cp(self, out, in_, eng='dve'):
        if eng == 'act':
            self.S.op('act', lambda e: e.activation(out=out, in_=in_, func=AF.Identity), [in_], [out])
        else:
            self.S.op(eng, lambda e: e.tensor_copy(out=out, in_=in_), [in_], [out])

    def memset(self, ap, val, eng='dve'):
        self.S.op(eng, lambda e: e.memset(ap, val), [], [ap])

    def recip(self, out, in_):
        self.S.op('dve', lambda e: e.reciprocal(out=out, in_=in_), [in_], [out])

    def rsqrt(self, es_tmp, out, in_, scale, eps):
        self.ts(out, in_, scale, ALU.mult, eps, ALU.add)
        self.act(out, out, AF.Sqrt)
        self.recip(out, out)


def stage_modvec(K, es0, c_b, c_ctx, ada_w, ada_b, n1g, n2g, modx):
    cfg = K.cfg
    D = cfg['D']
    KC = D // 128
    with contextlib.ExitStack() as es:
        cT = K.sb(es, "cT", [128, KC, 2])
        K.dma(cT[:, :, 0], c_b.rearrange("(k p) -> p k", p=128), q='sp', allow_slow_non_contiguous=True)
        K.dma(cT[:, :, 1], c_ctx.rearrange("(k p) -> p k", p=128), q='sp', allow_slow_non_contiguous=True)
        K.act(cT[:], cT[:], AF.Silu)
        mod = K.sb(es, "mod", [2, 6 * D])
        bt = [K.sb(es, f"mbt{i}", [2, 512]) for i in range(2)]
        wt = [K.sb(es, f"mw{i}", [128, KC, 512]) for i in range(2)]
        acc = [K.ps(es, f"macc{i}", [2, 512]) for i in range(2)]
        nb = 6 * D // 512
        for cb in range(nb):
            w = wt[cb % 2]
            bb = bt[cb % 2]
            dma_w(K, w[:], ada_w[:, cb * 512:(cb + 1) * 512])
            bc_row(K, bb[:], ada_b[cb * 512:(cb + 1) * 512], n=2, q='sp')
            a = acc[cb % 2]
            for k in range(KC):
                K.mm(a[:], cT[:, k, :], w[:, k, :], start=(k == 0), stop=(k == KC - 1))
            K.tt(mod[:, cb * 512:(cb + 1) * 512], a[:], bb[:], ALU.add)
        g = K.sb(es, "mg", [2, 2, D])
        bc_row(K, g[:, 0, :], n1g, n=2, q='sp')
        bc_row(K, g[:, 1, :], n2g, n=2, q='sp')
        K.stt(mod[:, 1 * D:2 * D], mod[:, 1 * D:2 * D], 1.0, g[:, 0, :], ALU.add, ALU.mult)
        K.stt(mod[:, 4 * D:5 * D], mod[:, 4 * D:5 * D], 1.0, g[:, 1, :], ALU.add, ALU.mult)
        for r_, src in enumerate((1, 0, 2, 4, 3, 5)):
            K.dma(modx[:, r_, :], mod[:, src * D:(src + 1) * D], q='sp')
        K.S.barrier()


def dma_w(K, dst, src, q=None):
    KCn = dst.shape[1]
    for k0 in range(0, KCn, 4):
        k1 = min(KCn, k0 + 4)
        K.dma(dst[:, k0:k1, :], src[k0 * 128:k1 * 128, :].rearrange("(k p) n -> p k n", p=128), q=q)


def bc_row(K, dst, row_ap, n=128, q=None):
    K.dma(dst, row_ap.rearrange("(o f) -> o f", o=1).partition_broadcast(n).rearrange("p o f -> p (o f)"), q=q)


def norm_mod_tile(K, es, xt, hb, a_bc, sh_bc, ss, D, junk):
    K.act(junk, xt, AF.Square, accum_out=ss)
    K.rsqrt(es, ss, ss, 1.0 / D, 1e-6)
    K.stt(junk, xt, ss, a_bc, ALU.mult, ALU.mult)
    K.tt(hb, junk, sh_bc, ALU.add)


def stage_inproj(K, lat, modx, w_in, P, PG, identb):
    cfg = K.cfg
    D = cfg['D']
    KC = D // 128
    INW = cfg['INW']
    nct = cfg['CTX'] // 128
    nlt = cfg['SEQ'] // 128
    g0 = cfg['off']['gate'][0]
    groups = [(0, nct, 1)]
    t = nct
    while t < nct + nlt:
        n = min(8, nct + nlt - t)
        groups.append((t, n, 0))
        t += n
    blocks = []
    c = 0
    while c < INW:
        lim = g0 if c < g0 else INW
        n = min(512, lim - c)
        blocks.append((c, n, c >= g0))
        c += n
    with contextlib.ExitStack() as es:
        xt = [K.sb(es, f"xt{i}", [128, D]) for i in range(2)]
        junk = K.sb(es, "junk", [128, D])
        hb = K.sb(es, "hb", [128, D], BF16)
        a_bc = K.sb(es, "abc", [128, D])
        sh_bc = K.sb(es, "shbc", [128, D])
        ss = K.sb(es, "ss", [128, 2])
        hT = K.sb(es, "hT", [128, KC, 8 * 128], BF16)
        wt = [K.sb(es, f"wt{i}", [128, KC, 512], BF16) for i in range(2)]
        ev = [K.sb(es, f"ev{i}", [128, 512]) for i in range(3)]
        pt = [K.ps(es, f"pt{i}", [128, 8, 128], BF16) for i in range(2)]
        acc = [K.ps(es, f"acc{i}", [128, 512]) for i in range(3)]
        it = 0
        wi = 0
        for (t0, n, mi) in groups:
            bc_row(K, a_bc[:], modx[mi, 0, :], q='sp')
            bc_row(K, sh_bc[:], modx[mi, 1, :], q='sp')
            for j in range(n):
                x = xt[j % 2]
                K.dma(x[:], lat[(t0 + j) * 128:(t0 + j + 1) * 128, :])
                norm_mod_tile(K, es, x[:], hb[:], a_bc[:], sh_bc[:], ss[:, 0:1], D, junk[:])
                for k0 in range(0, KC, 8):
                    p = pt[(k0 // 8) % 2]
                    kn = min(8, KC - k0)
                    for k in range(kn):
                        K.tr(p[:, k, :], hb[:, (k0 + k) * 128:(k0 + k + 1) * 128], identb[:])
                    K.cp(hT[:, k0:k0 + kn, j * 128:(j + 1) * 128], p[:, 0:kn, :], eng=('dve' if (k0 // 8) % 2 else 'act'))
            for (c0, ncol, isg) in blocks:
                w = wt[wi % 2]
                wi += 1
                dma_w(K, w[:, :, 0:ncol], w_in[:, c0:c0 + ncol], q='pool')
                for j in range(n):
                    a = acc[it % 3]
                    e = ev[it % 3]
                    for k in range(KC):
                        K.mm(a[:, 0:ncol], hT[:, k, j * 128:(j + 1) * 128], w[:, k, 0:ncol], start=(k == 0), stop=(k == KC - 1))
                    if isg:
                        K.act(e[:, 0:ncol], a[:, 0:ncol], AF.Sigmoid)
                    elif it % 2 == 0:
                        K.cp(e[:, 0:ncol], a[:, 0:ncol], eng='act')
                    else:
                        K.cp(e[:, 0:ncol], a[:, 0:ncol], eng='dve')
                    if isg:
                        K.dma(PG[(t0 + j) * 128:(t0 + j + 1) * 128, c0 - g0:c0 - g0 + ncol], e[:, 0:ncol], q='sp')
                    else:
                        K.dma(P[(t0 + j) * 128:(t0 + j + 1) * 128, c0:c0 + ncol], e[:, 0:ncol], q='sp')
                    it += 1
        K.S.barrier()


def load_rows(K, dst, arr, c0, ncols, base, seglen, s0, n, perm=None, q=None):
    lo = max(s0, 0)
    hi = min(s0 + n, seglen)
    if hi <= lo:
        return
    if perm is None:
        K.dma(dst[lo - s0:hi - s0, 0:ncols], arr[base + lo:base + hi, c0:c0 + ncols], q=q)
        return
    A, B = perm
    i = lo
    while i < hi:
        a, b = divmod(i, B)
        m = min(B - b, hi - i)
        r0 = base + b * A + a
        K.dma(dst[i - s0:i - s0 + m, 0:ncols], arr[r0:r0 + (m - 1) * A + 1:A, c0:c0 + ncols], q=q)
        i += m


def bc3(ap, axis, shape):
    return ap.unsqueeze(axis).to_broadcast(list(shape))


def softplus_(K, x, bias_bc):
    K.tt(x, x, bias_bc, ALU.add)
    K.act(x, x, AF.Exp)
    K.act(x, x, AF.Ln, bias=1.0)


def stage_mamba_prep(K, P, conv_w, conv_b, A_log, dt_bias, mxbc, mdt, mdA):
    cfg = K.cfg
    MW, MH = cfg['MW'], cfg['MH']
    NCH = MW + 512
    CTX, SEQ = cfg['CTX'], cfg['SEQ']
    rows = SEQ // 64
    ox = cfg['off']['m_x'][0]
    odt = cfg['off']['m_dt'][0]
    with contextlib.ExitStack() as es:
        wk = K.sb(es, "mcw", [128, 5, NCH])
        for k in range(5):
            bc_row(K, wk[:, k, :], conv_w[k, :])
        cb = K.sb(es, "mcb", [128, NCH])
        bc_row(K, cb[:], conv_b)
        aneg = K.sb(es, "aneg", [128, 2 * MH])
        bc_row(K, aneg[:], A_log.rearrange("a b -> (a b)"))
        K.act(aneg[:], aneg[:], AF.Exp)
        dtb = K.sb(es, "dtb", [128, 2 * MH])
        bc_row(K, dtb[:], dt_bias.rearrange("a b -> (a b)"))
        xs = [K.sb(es, f"mx{k}", [128, NCH]) for k in range(5)]
        acc = K.sb(es, "macc", [128, NCH])
        dt = K.sb(es, "mdt", [128, 2 * MH])
        dA = K.sb(es, "mdA", [128, 2 * MH])
        for seg, (base, seglen, perm) in enumerate([(0, CTX, None), (CTX, SEQ, (64, rows))]):
            for s0 in range(0, seglen, 128):
                for k in range(5):
                    edge = (s0 + k - 2 < 0) or (s0 + k - 2 + 128 > seglen)
                    if edge:
                        K.memset(xs[k][:], 0.0, eng='pool')
                    load_rows(K, xs[k][:], P, ox, NCH, base, seglen, s0 + k - 2, 128, perm)
                K.tt(acc[:], xs[0][:], wk[:, 0, :], ALU.mult)
                for k in range(1, 5):
                    K.tt(xs[k][:], xs[k][:], wk[:, k, :], ALU.mult, eng='pool')
                    K.tt(acc[:], acc[:], xs[k][:], ALU.add)
                K.tt(acc[:], acc[:], cb[:], ALU.add)
                K.act(acc[:], acc[:], AF.Silu)
                K.dma(mxbc[base + s0:base + s0 + 128, :], acc[:], q='sp')
                load_rows(K, dt[:], P, odt, 2 * MH, base, seglen, s0, 128, perm)
                softplus_(K, dt[:], dtb[:])
                K.stt(dA[:], dt[:], -1.0, aneg[:], ALU.mult, ALU.mult)
                K.dma(mdt[base + s0:base + s0 + 128, :], dt[:], q='sp')
                K.dma(mdA[base + s0:base + s0 + 128, :], dA[:], q='sp')
        K.S.barrier()


def stage_mamba_scan(K, cst, mxbc, mdt, mdA, ymd):
    cfg = K.cfg
    MW, MH = cfg['MW'], cfg['MH']
    HG = MH // 2
    CTX, SEQ, T = cfg['CTX'], cfg['SEQ'], cfg['T']
    nchunk = T // 64
    ident, ones = cst[:, 0, :], cst[:, 1, :]
    HB = min(8, MH)
    with contextlib.ExitStack() as es:
        H = K.sb(es, "mH", [128, MH, 64])
        X = [K.sb(es, f"mX{i}", [64, MW + 512]) for i in range(2)]
        dtt = [K.sb(es, f"mdtt{i}", [64, 2 * MH]) for i in range(2)]
        dAt = [K.sb(es, f"mdAt{i}", [64, 2 * MH]) for i in range(2)]
        BCT = K.sb(es, "mBCT", [128, 4, 64])
        Z = K.sb(es, "mZ", [64, MH, 64])
        G = K.sb(es, "mG", [64, MH])
        eG = K.sb(es, "meG", [64, MH])
        eE = K.sb(es, "meE", [64, MH])
        gC = K.sb(es, "mgC", [128, MH])
        Dm = K.sb(es, "mD", [64, MH, 64])
        sc = K.sb(es, "msc", [64, 2, 64])
        xdt = K.sb(es, "mxdt", [64, MH, 64])
        xe = K.sb(es, "mxe", [64, MH, 64])
        Y = K.sb(es, "mY", [64, MH, 64])
        p_t = K.ps(es, "mp_t", [128, 4, 64])[:]
        p_g = K.ps(es, "mp_g", [128, 4, MH])[:]
        p_sc = K.ps(es, "mp_sc", [64, 2, 64])[:]
        p_gb = K.ps(es, "mp_gb", [64, HB, 64])
        p_y = K.ps(es, "mp_y", [64, HB, 64])
        p_c = K.ps(es, "mp_c", [64, HB, 64])
        p_h = K.ps(es, "mp_h", [128, HB, 64])
        for d in range(2):
            tri = cst[0:64, 2 + d, 0:64]
            K.memset(H[:], 0.0)
            order = list(range(nchunk))
            if d == 1:
                order = list(range(CTX // 64 - 1, -1, -1)) + list(range(nchunk - 1, CTX // 64 - 1, -1))
            for ci, c in enumerate(order):
                x = X[ci % 2]
                dt_ = dtt[ci % 2]
                dA_ = dAt[ci % 2]
                K.dma(x[:], mxbc[c * 64:(c + 1) * 64, :])
                K.dma(dt_[:], mdt[c * 64:(c + 1) * 64, :])
                K.dma(dA_[:], mdA[c * 64:(c + 1) * 64, :])
                dAd = dA_[:, d * MH:(d + 1) * MH]
                dtd = dt_[:, d * MH:(d + 1) * MH]
                for j in range(4):
                    K.tr(p_t[:, j, :], x[:, MW + j * 128:MW + (j + 1) * 128], ident[0:64, 0:64])
                K.cp(BCT[:], p_t, eng='act')
                K.mm(p_g[0:64, 0, :], tri, dAd)
                K.mm(p_g[0:64, 1, :], ones[0:64, 0:64], dAd)
                K.mm(p_g[:, 2, :], ones[0:64, :], dAd)
                K.cp(G[:], p_g[0:64, 0, :])
                K.act(eG[:], p_g[0:64, 0, :], AF.Exp)
                K.tt(eE[:], p_g[0:64, 1, :], G[:], ALU.subtract)
                K.act(eE[:], eE[:], AF.Exp)
                K.act(gC[:], p_g[:, 2, :], AF.Exp)
                K.tt(Z[:], bc3(dAd, 2, [64, MH, 64]), bc3(tri, 1, [64, MH, 64]), ALU.mult, eng=K.pool_eng)
                for g in range(2):
                    K.mm(p_sc[:, g, :], BCT[:, g, :], BCT[:, 2 + g, :])
                K.tt(sc[:], p_sc, bc3(tri, 1, [64, 2, 64]), ALU.mult)
                xs3 = x[:, 0:MW].rearrange("p (h e) -> p h e", e=64)
                K.tt(xdt[:], xs3, bc3(dtd, 2, [64, MH, 64]), ALU.mult, eng=K.pool_eng)
                K.tt(xe[:], xdt[:], bc3(eE[:], 2, [64, MH, 64]), ALU.mult, eng=K.pool_eng)
                for h0 in range(0, MH, HB):
                    hs = slice(h0, h0 + HB)
                    K.mm(p_gb[:], ones[0:64, 0:64], Z[:, hs, :])
                    K.tt(Dm[:, hs, :], p_gb[:], bc3(G[:, hs], 2, [64, HB, 64]), ALU.subtract)
                    K.ts(Dm[:, hs, :], Dm[:, hs, :], 0.0, ALU.min)
                    K.act(Dm[:, hs, :], Dm[:, hs, :], AF.Exp)
                    for g in range(2):
                        ga, gb_ = max(h0, g * HG), min(h0 + HB, (g + 1) * HG)
                        if gb_ > ga:
                            K.tt(Dm[:, ga:gb_, :], Dm[:, ga:gb_, :], bc3(sc[:, g, :], 1, [64, gb_ - ga, 64]), ALU.mult)
                    for h in range(h0, h0 + HB):
                        g = h // HG
                        K.mm(p_y[:, h - h0, :], Dm[:, h, :], xdt[:, h, :])
                        K.mm(p_c[:, h - h0, :], BCT[:, 2 + g, :], H[:, h, :])
                    for h in range(h0, h0 + HB):
                        g = h // HG
                        K.mm(p_h[:, h - h0, :], x[:, MW + g * 128:MW + (g + 1) * 128], xe[:, h, :])
                    K.cp(Y[:, hs, :], p_y[:], eng='act')
                    K.tt(Z[:, hs, :], p_c[:], bc3(eG[:, hs], 2, [64, HB, 64]), ALU.mult)
                    K.tt(Y[:, hs, :], Y[:, hs, :], Z[:, hs, :], ALU.add)
                    K.tt(H[:, hs, :], H[:, hs, :], bc3(gC[:, hs], 2, [128, HB, 64]), ALU.mult)
                    K.tt(H[:, hs, :], H[:, hs, :], p_h[:], ALU.add)
                K.dma(ymd[d][c * 64:(c + 1) * 64, :], Y[:].rearrange("p h e -> p (h e)"), q='sp')
        K.S.barrier()


def stage_mamba_finish(K, P, mxbc, ymd, D_skip, norm_g, ymf):
    cfg = K.cfg
    MW, MH = cfg['MW'], cfg['MH']
    CTX, SEQ = cfg['CTX'], cfg['SEQ']
    rows = SEQ // 64
    oz = cfg['off']['m_z'][0]
    GWD = MW // 2
    with contextlib.ExitStack() as es:
        dsk = K.sb(es, "dsk", [128, MH])
        bc_row(K, dsk[:], D_skip)
        ng = K.sb(es, "mng", [128, MW])
        bc_row(K, ng[:], norm_g)
        y0 = [K.sb(es, f"fy0{i}", [128, MW]) for i in range(2)]
        y1 = [K.sb(es, f"fy1{i}", [128, MW]) for i in range(2)]
        xs = [K.sb(es, f"fxs{i}", [128, MW]) for i in range(2)]
        z = [K.sb(es, f"fz{i}", [128, MW]) for i in range(2)]
        junk = K.sb(es, "fjunk", [128, GWD])
        ss = K.sb(es, "fss", [128, 2])
        i = 0
        for (base, seglen, perm) in [(0, CTX, None), (CTX, SEQ, (rows, 64))]:
            for s0 in range(0, seglen, 128):
                a, b, c, zz = y0[i % 2], y1[i % 2], xs[i % 2], z[i % 2]
                i += 1
                load_rows(K, a[:], ymd[0], 0, MW, base, seglen, s0, 128, perm)
                load_rows(K, b[:], ymd[1], 0, MW, base, seglen, s0, 128, perm)
                load_rows(K, c[:], mxbc, 0, MW, base, seglen, s0, 128, perm)
                K.dma(zz[:], P[base + s0:base + s0 + 128, oz:oz + MW])
                K.tt(a[:], a[:], b[:], ALU.add)
                c3 = c[:].rearrange("p (h e) -> p h e", e=64)
                K.tt(c3, c3, bc3(dsk[:], 2, [128, MH, 64]), ALU.mult, eng='pool')
                K.tt(a[:], a[:], c[:], ALU.add)
                K.act(zz[:], zz[:], AF.Silu)
                K.tt(a[:], a[:], zz[:], ALU.mult)
                for g in range(2):
                    K.act(junk[:], a[:, g * GWD:(g + 1) * GWD], AF.Square, accum_out=ss[:, g:g + 1])
                K.rsqrt(es, ss[:], ss[:], 1.0 / GWD, 1e-6)
                for g in range(2):
                    K.stt(a[:, g * GWD:(g + 1) * GWD], a[:, g * GWD:(g + 1) * GWD], ss[:, g:g + 1],
                          ng[:, g * GWD:(g + 1) * GWD], ALU.mult, ALU.mult)
                K.dma(ymf[base + s0:base + s0 + 128, :], a[:], q='sp')
        K.S.barrier()


def to_featmajor(K, src_bf, dstT, j, KCn, pt, identb):
    for k0 in range(0, KCn, 8):
        p = pt[(k0 // 8) % 2]
        kn = min(8, KCn - k0)
        for k in range(kn):
            K.tr(p[:, k, :], src_bf[:, (k0 + k) * 128:(k0 + k + 1) * 128], identb[:])
        K.cp(dstT[:, k0:k0 + kn, j * 128:(j + 1) * 128], p[:, 0:kn, :], eng=('dve' if (k0 // 8) % 2 else 'act'))


def token_groups(cfg, gs):
    nct = cfg['CTX'] // 128
    nlt = cfg['SEQ'] // 128
    groups = []
    t = 0
    while t < nct:
        n = min(gs, nct - t)
        groups.append((t, n, 1))
        t += n
    while t < nct + nlt:
        n = min(gs, nct + nlt - t)
        groups.append((t, n, 0))
        t += n
    return groups


def stage_merge(K, PG, ybr, w_br, w_out, modx, lat, identb, t_lo=0):
    cfg = K.cfg
    D = cfg['D']
    KC = D // 128
    BW = cfg['MW']
    KB_ = BW // 128
    g0 = cfg['off']['gate'][0]
    GS = 4
    NCB = D // 512
    with contextlib.ExitStack() as es:
        yt = [K.sb(es, f"gy{i}", [128, BW]) for i in range(2)]
        yb = K.sb(es, "gyb", [128, BW], BF16)
        yT = K.sb(es, "gyT", [128, KB_, GS * 128], BF16)
        wb = [K.sb(es, f"gwb{i}", [128, KB_, 512], BF16) for i in range(2)]
        mg = K.sb(es, "gmg", [128, GS, D])
        gt = [K.sb(es, f"ggt{i}", [128, 512]) for i in range(2)]
        tmp = K.sb(es, "gtmp", [128, 512])
        mb = K.sb(es, "gmb", [128, D], BF16)
        mT = K.sb(es, "gmT", [128, KC, GS * 128], BF16)
        wo = [K.sb(es, f"gwo{i}", [128, KC, 512], BF16) for i in range(2)]
        g1 = K.sb(es, "gg1", [128, D])
        lt = [K.sb(es, f"glt{i}", [128, 512]) for i in range(2)]
        pt = [K.ps(es, f"gpt{i}", [128, 8, 128], BF16) for i in range(2)]
        acc = [K.ps(es, f"gacc{i}", [128, 512]) for i in range(3)]
        it = 0
        wi = 0
        for (t0, n, mi) in token_groups(cfg, GS):
            if t0 < t_lo:
                continue
            bc_row(K, g1[:], modx[mi, 2, :], q='sp')
            for br in range(3):
                for j in range(n):
                    y = yt[j % 2]
                    K.dma(y[:], ybr[br][(t0 + j) * 128:(t0 + j + 1) * 128, :])
                    K.cp(yb[:], y[:], eng='pool')
                    to_featmajor(K, yb, yT, j, KB_, pt, identb)
                for cb in range(NCB):
                    w = wb[wi % 2]
                    wi += 1
                    dma_w(K, w[:], w_br[br][:, cb * 512:(cb + 1) * 512], q='pool')
                    for j in range(n):
                        a = acc[it % 3]
                        g = gt[it % 2]
                        it += 1
                        K.dma(g[:], PG[(t0 + j) * 128:(t0 + j + 1) * 128, br * D + cb * 512:br * D + (cb + 1) * 512])
                        for k in range(KB_):
                            K.mm(a[:], yT[:, k, j * 128:(j + 1) * 128], w[:, k, :], start=(k == 0), stop=(k == KB_ - 1))
                        if br == 0:
                            K.tt(mg[:, j, cb * 512:(cb + 1) * 512], a[:], g[:], ALU.mult)
                        else:
                            K.tt(tmp[:], a[:], g[:], ALU.mult)
                            K.tt(mg[:, j, cb * 512:(cb + 1) * 512], mg[:, j, cb * 512:(cb + 1) * 512], tmp[:], ALU.add, eng='pool')
            for j in range(n):
                K.cp(mb[:], mg[:, j, :], eng='act')
                to_featmajor(K, mb, mT, j, KC, pt, identb)
            for cb in range(NCB):
                w = wo[wi % 2]
                wi += 1
                dma_w(K, w[:], w_out[:, cb * 512:(cb + 1) * 512], q='pool')
                for j in range(n):
                    a = acc[it % 3]
                    l_ = lt[it % 2]
                    it += 1
                    rs = slice((t0 + j) * 128, (t0 + j + 1) * 128)
                    K.dma(l_[:], lat[rs, cb * 512:(cb + 1) * 512])
                    for k in range(KC):
                        K.mm(a[:], mT[:, k, j * 128:(j + 1) * 128], w[:, k, :], start=(k == 0), stop=(k == KC - 1))
                    K.tt(tmp[:], a[:], g1[:, cb * 512:(cb + 1) * 512], ALU.mult)
                    K.tt(l_[:], l_[:], tmp[:], ALU.add)
                    K.dma(lat[rs, cb * 512:(cb + 1) * 512], l_[:], q='sp')
        K.S.barrier()


def stage_moe(K, lat, modx, grp_w, grp_b, exp_w, exp_b, w1, w3, w2, ident, identb, t_lo=0):
    cfg = K.cfg
    D = cfg['D']
    KC = D // 128
    FF = cfg['FF']
    FC = FF // 128
    NE = cfg['NE']
    GS = 4
    NCB = D // 512
    NR = 4 + NE
    with contextlib.ExitStack() as es:
        xt0 = K.sb(es, "ex0", [128, D])
        xt = [xt0, xt0]
        h = K.sb(es, "eh", [128, D])
        hb = K.sb(es, "ehb", [128, D], BF16)
        a_bc = K.sb(es, "eabc", [128, D])
        sh_bc = K.sb(es, "eshbc", [128, D])
        g2 = K.sb(es, "eg2", [128, D])
        ss = K.sb(es, "ess", [128, 2])
        hT = K.sb(es, "ehT", [128, KC, GS * 128], BF16)
        hTf = K.sb(es, "ehTf", [128, KC, 128])
        rw = K.sb(es, "erw", [128, KC, NR])
        rb = K.sb(es, "erb", [128, NR])
        K.dma(rw[:, :, 0:4], grp_w.rearrange("(k p) n -> p k n", p=128), q='sp', allow_slow_non_contiguous=True)
        K.dma(rw[:, :, 4:NR], exp_w.rearrange("(k p) n -> p k n", p=128), q='sp', allow_slow_non_contiguous=True)
        bc_row(K, rb[:, 0:4], grp_b, q='sp')
        bc_row(K, rb[:, 4:NR], exp_b, q='sp')
        lg = K.sb(es, "elg", [128, NR])
        sm = K.sb(es, "esm", [128, 16])
        oh = K.sb(es, "eoh", [128, 4])
        ig = K.sb(es, "eig", [128, 8])
        i2 = K.sb(es, "ei2", [128, 8])
        m1 = K.sb(es, "em1", [128, 8])
        m2 = K.sb(es, "em2", [128, 8])
        comb = K.sb(es, "ecomb", [128, GS, NE])
        wa = [K.sb(es, "ewa0", [128, KC, FF], BF16)] * 2
        wc = [K.sb(es, "ewc0", [128, KC, FF], BF16)] * 2
        wd = [K.sb(es, "ewd0", [128, FC, D], BF16)] * 2
        sl = K.sb(es, "esl", [128, 512])
        hidT = K.sb(es, "ehidT", [128, FC, GS * 128], BF16)
        yacc = K.sb(es, "eyacc", [128, GS, D])
        junk = yacc[:, 0, :]
        pt0 = K.ps(es, "ept0", [128, 8, 128], BF16)
        pt = [pt0, pt0]
        ptf = K.ps(es, "eptf", [128, 4, 128])
        pr = K.ps(es, "epr", [128, NR])
        p1 = K.ps(es, "ep1", [128, 512])
        p3 = K.ps(es, "ep3", [128, 512])
        py0 = K.ps(es, "epy0", [128, 512])
        py = [py0, py0]
        wi = 0
        it = 0
        for (t0, n, mi) in token_groups(cfg, GS):
            if t0 < t_lo:
                continue
            bc_row(K, a_bc[:], modx[mi, 3, :], q='sp')
            bc_row(K, sh_bc[:], modx[mi, 4, :], q='sp')
            bc_row(K, g2[:], modx[mi, 5, :], q='sp')
            for j in range(n):
                x = xt[j % 2]
                K.dma(x[:], lat[(t0 + j) * 128:(t0 + j + 1) * 128, :])
                norm_mod_tile(K, es, x[:], h[:], a_bc[:], sh_bc[:], ss[:, 0:1], D, junk)
                K.cp(hb[:], h[:], eng='pool')
                to_featmajor(K, hb, hT, j, KC, pt, identb)
                for k0 in range(0, KC, 4):
                    for k in range(4):
                        K.tr(ptf[:, k, :], h[:, (k0 + k) * 128:(k0 + k + 1) * 128], ident[:])
                    K.cp(hTf[:, k0:k0 + 4, :], ptf[:], eng='act')
                for k in range(KC):
                    K.mm(pr[:], hTf[:, k, :], rw[:, k, :], start=(k == 0), stop=(k == KC - 1))
                K.tt(lg[:], pr[:], rb[:], ALU.add)
                K.red(sm[:, 0:1], lg[:, 0:4], ALU.max)
                K.ts(oh[:], lg[:, 0:4], sm[:, 0:1], ALU.is_equal)
                K.ts(sm[:, 4:8], lg[:, 0:4], sm[:, 0:1], ALU.subtract)
                K.act(sm[:, 4:8], sm[:, 4:8], AF.Exp)
                K.red(sm[:, 1:2], sm[:, 4:8], ALU.add)
                K.recip(sm[:, 1:2], sm[:, 1:2])
                K.ts(ig[:], lg[:, 4:12], oh[:, 0:1], ALU.mult)
                for g in range(1, 4):
                    K.stt(ig[:], lg[:, 4 + 8 * g:12 + 8 * g], oh[:, g:g + 1], ig[:], ALU.mult, ALU.add)
                K.red(sm[:, 2:3], ig[:], ALU.max)
                K.ts(m1[:], ig[:], sm[:, 2:3], ALU.is_equal)
                K.stt(i2[:], m1[:], -1e30, ig[:], ALU.mult, ALU.add)
                K.red(sm[:, 3:4], i2[:], ALU.max)
                K.ts(m2[:], i2[:], sm[:, 3:4], ALU.is_equal)
                K.tt(sm[:, 8:9], sm[:, 3:4], sm[:, 2:3], ALU.subtract)
                K.act(sm[:, 8:9], sm[:, 8:9], AF.Exp)
                K.ts(sm[:, 9:10], sm[:, 8:9], 1.0, ALU.add)
                K.recip(sm[:, 9:10], sm[:, 9:10])
                K.tt(sm[:, 9:10], sm[:, 9:10], sm[:, 1:2], ALU.mult)
                K.tt(sm[:, 10:11], sm[:, 9:10], sm[:, 8:9], ALU.mult)
                K.ts(m1[:], m1[:], sm[:, 9:10], ALU.mult)
                K.stt(m1[:], m2[:], sm[:, 10:11], m1[:], ALU.mult, ALU.add)
                for g in range(4):
                    K.ts(comb[:, j, g * 8:(g + 1) * 8], m1[:], oh[:, g:g + 1], ALU.mult)
            K.memset(yacc[:], 0.0, eng='pool')
            ntok = n * 128
            for e in range(NE):
                a_, c_, d_ = wa[wi % 2], wc[wi % 2], wd[wi % 2]
                wi += 1
                dma_w(K, a_[:], w1[e], q='pool')
                dma_w(K, c_[:], w3[e], q='pool')
                for cb in range(NCB):
                    K.dma(d_[:, :, cb * 512:(cb + 1) * 512], w2[e][:, cb * 512:(cb + 1) * 512].rearrange("(k p) n -> p k n", p=128), q='pool')
                for fc in range(FC):
                    for tb in range(0, ntok, 512):
                        tn = min(512, ntok - tb)
                        for k in range(KC):
                            K.mm(p1[:, 0:tn], a_[:, k, fc * 128:(fc + 1) * 128], hT[:, k, tb:tb + tn], start=(k == 0), stop=(k == KC - 1))
                        for k in range(KC):
                            K.mm(p3[:, 0:tn], c_[:, k, fc * 128:(fc + 1) * 128], hT[:, k, tb:tb + tn], start=(k == 0), stop=(k == KC - 1))
                        K.act(sl[:, 0:tn], p1[:, 0:tn], AF.Silu)
                        K.tt(hidT[:, fc, tb:tb + tn], sl[:, 0:tn], p3[:, 0:tn], ALU.mult)
                for j in range(n):
                    for cb in range(NCB):
                        p = py[it % 2]
                        it += 1
                        for fc in range(FC):
                            K.mm(p[:], hidT[:, fc, j * 128:(j + 1) * 128], d_[:, fc, cb * 512:(cb + 1) * 512], start=(fc == 0), stop=(fc == FC - 1))
                        ysl = yacc[:, j, cb * 512:(cb + 1) * 512]
                        K.stt(ysl, p[:], comb[:, j, e:e + 1], ysl, ALU.mult, ALU.add)
            for j in range(n):
                x = xt[j % 2]
                rs = slice((t0 + j) * 128, (t0 + j + 1) * 128)
                K.dma(x[:], lat[rs, :])
                K.tt(yacc[:, j, :], yacc[:, j, :], g2[:], ALU.mult)
                K.tt(x[:], x[:], yacc[:, j, :], ALU.add)
                K.dma(lat[rs, :], x[:], q='sp')
        K.S.barrier()


def stage_final(K, lat, fg, out):
    cfg = K.cfg
    D = cfg['D']
    nct = cfg['CTX'] // 128
    nlt = cfg['SEQ'] // 128
    with contextlib.ExitStack() as es:
        g = K.sb(es, "fg", [128, D])
        bc_row(K, g[:], fg, q='sp')
        xt = [K.sb(es, f"fx{i}", [128, D]) for i in range(2)]
        junk = K.sb(es, "fjk", [128, D])
        ss = K.sb(es, "fss2", [128, 1])
        for j in range(nlt):
            x = xt[j % 2]
            K.dma(x[:], lat[(nct + j) * 128:(nct + j + 1) * 128, :])
            K.act(junk[:], x[:], AF.Square, accum_out=ss[:])
            K.rsqrt(es, ss[:], ss[:], 1.0 / D, 1e-6)
            K.stt(x[:], x[:], ss[:], g[:], ALU.mult, ALU.mult)
            K.dma(out[j * 128:(j + 1) * 128, :], x[:], q='sp')
        K.S.barrier()


PARAMS = [("ada_w", lambda c: [c['DEPTH'], c['D'], 6 * c['D']]), ("ada_b", lambda c: [c['DEPTH'], 6 * c['D']]),
          ("norm1_g", lambda c: [c['DEPTH'], c['D']]), ("norm2_g", lambda c: [c['DEPTH'], c['D']]),
          ("w_in", lambda c: [c['DEPTH'], c['D'], c['INW']]),
          ("m_conv_w", lambda c: [c['DEPTH'], 5, c['MW'] + 512]), ("m_conv_b", lambda c: [c['DEPTH'], c['MW'] + 512]),
          ("m_A_log", lambda c: [c['DEPTH'], 2, c['MH']]), ("m_dt_bias", lambda c: [c['DEPTH'], 2, c['MH']]),
          ("m_D", lambda c: [c['DEPTH'], c['MH']]), ("m_norm_g", lambda c: [c['DEPTH'], c['MW']]),
          ("r_shift_mu", lambda c: [c['DEPTH'], 2, 3 * c['RW']]), ("r_w0", lambda c: [c['DEPTH'], 2, c['RW']]),
          ("r_w_up", lambda c: [c['DEPTH'], 2, 64, c['RW']]), ("r_a0", lambda c: [c['DEPTH'], c['RW']]),
          ("r_a_up", lambda c: [c['DEPTH'], 64, c['RW']]), ("r_g_up", lambda c: [c['DEPTH'], 128, c['RW']]),
          ("r_k_k", lambda c: [c['DEPTH'], c['RW']]), ("r_k_a", lambda c: [c['DEPTH'], c['RW']]),
          ("r_r_k", lambda c: [c['DEPTH'], c['RH'], 64]), ("r_ln_g", lambda c: [c['DEPTH'], c['RW']]),
          ("r_ln_b", lambda c: [c['DEPTH'], c['RW']]),
          ("g_conv_w", lambda c: [c['DEPTH'], 5, 3 * c['GW']]), ("g_A_log", lambda c: [c['DEPTH'], 2, c['GH']]),
          ("g_dt_bias", lambda c: [c['DEPTH'], 2, c['GH']]), ("g_norm_g", lambda c: [c['DEPTH'], 128]),
          ("w_br_m", lambda c: [c['DEPTH'], c['MW'], c['D']]), ("w_br_r", lambda c: [c['DEPTH'], c['RW'], c['D']]),
          ("w_br_g", lambda c: [c['DEPTH'], c['GW'], c['D']]), ("w_out", lambda c: [c['DEPTH'], c['D'], c['D']]),
          ("moe_grp_w", lambda c: [c['DEPTH'], c['D'], 4]), ("moe_grp_b", lambda c: [c['DEPTH'], 4]),
          ("moe_exp_w", lambda c: [c['DEPTH'], c['D'], c['NE']]), ("moe_exp_b", lambda c: [c['DEPTH'], c['NE']]),
          ("moe_w1", lambda c: [c['DEPTH'], c['NE'], c['D'], c['FF']]), ("moe_w3", lambda c: [c['DEPTH'], c['NE'], c['D'], c['FF']]),
          ("moe_w2", lambda c: [c['DEPTH'], c['NE'], c['FF'], c['D']]), ("final_norm_g", lambda c: [c['D']])]


def make_cst():
    c = np.zeros((128, 8, 128), np.float32)
    i = np.arange(128)
    c[:, 0, :] = np.eye(128)
    c[:, 1, :] = 1.0
    s = (i % 64)[:, None]
    t = (i % 64)[None, :]
    c[:, 2, :] = (s <= t)
    c[:, 3, :] = (s >= t)
    c[:, 4, :] = (s < t)
    c[:, 5, :] = (s > t)
    return c


def build_program(cfg, stages=None):
    nc = bass.Bass("TRN2", target_bir_lowering=False)
    D, T, SEQ, CTX, INW, MW, MH = (cfg[k] for k in ("D", "T", "SEQ", "CTX", "INW", "MW", "MH"))
    es = contextlib.ExitStack()
    with es:
        K = KB(nc, cfg, es)
        import os
        if os.environ.get('KM_SER'):
            K.S.ser_engs = tuple(os.environ['KM_SER'].split(','))
        x = K.dram("x", [SEQ, D], kind="ExternalInput")
        ctx = K.dram("ctx", [CTX, D], kind="ExternalInput")
        c_b = K.dram("c", [D], kind="ExternalInput")
        c_ctx = K.dram("c_ctx", [D], kind="ExternalInput")
        cstd = K.dram("cst", [128, 8, 128], kind="ExternalInput")
        W = {n: K.dram(n, f(cfg), kind="ExternalInput") for n, f in PARAMS}
        out = K.dram("out", [SEQ, D], kind="ExternalOutput")
        lat = K.dram("lat", [T, D])
        P = K.dram("P", [T, cfg['off']['gate'][0]])
        PG = K.dram("PG", [T, 3 * D])
        modx = K.dram("modx", [2, 6, D])
        mxbc = K.dram("mxbc", [T, MW + 512])
        mdt = K.dram("mdt", [T, 2 * MH])
        mdA = K.dram("mdA", [T, 2 * MH])
        ymd = [K.dram(f"ymd{d}", [T, MW]) for d in range(2)]
        ybr = [K.dram(f"ybr{i}", [T, MW]) for i in range(3)]
        cst = K.sb(es, "cst", [128, 8, 128])
        identb = K.sb(es, "identb", [128, 128], BF16)
        K.dma(cst[:], cstd, q='sp')
        K.cp(identb[:], cst[:, 0, :])
        ident = cst[:, 0, :]
        for r0 in range(0, CTX, 128):
            K.dma(lat[r0:r0 + 128, :], ctx[r0:r0 + 128, :])
        for r0 in range(0, SEQ, 128):
            K.dma(lat[CTX + r0:CTX + r0 + 128, :], x[r0:r0 + 128, :])
        GW, GH, RW, RH = cfg['GW'], cfg['GH'], cfg['RW'], cfg['RH']
        gqkv = K.dram("gqkv", [T, 3 * GW])
        glog = K.dram("glog", [T, 2 * GH])
        gbeta = K.dram("gbeta", [T, 2 * GH])
        ygd = [K.dram(f"ygd{d}", [T, GW]) for d in range(2)]
        RA = {n: K.dram("ra_" + n, [T, RW]) for n in ('r', 'k', 'v', 'kk', 'a', 'g')}
        RA['lw'] = K.dram("ra_lw", [T, 2 * RW])
        RA['bon'] = K.dram("ra_bon", [T, RH])
        yrd = [K.dram(f"yrd{d}", [T, RW]) for d in range(2)]
        nct = CTX // 128
        on = (lambda n: True) if stages is None else (lambda n: n in stages)
        for l in range(cfg['DEPTH']):
            last = (l == cfg['DEPTH'] - 1)
            if on('modvec'):
                stage_modvec(K, es, c_b, c_ctx, W['ada_w'][l], W['ada_b'][l], W['norm1_g'][l], W['norm2_g'][l], modx)
            if on('inproj'):
                stage_inproj(K, lat, modx, W['w_in'][l], P, PG, identb)
            if on('mprep'):
                stage_mamba_prep(K, P, W['m_conv_w'][l], W['m_conv_b'][l], W['m_A_log'][l], W['m_dt_bias'][l], mxbc, mdt, mdA)
            if on('mscan'):
                stage_mamba_scan(K, cst, mxbc, mdt, mdA, ymd)
            if on('mfin'):
                stage_mamba_finish(K, P, mxbc, ymd, W['m_D'][l], W['m_norm_g'][l], ybr[0])
            if on('rwkv'):
                stage_rwkv_prep(K, cst, P, W['r_shift_mu'][l], W['r_w0'][l], W['r_w_up'][l], W['r_a0'][l], W['r_a_up'][l],
                                W['r_g_up'][l], W['r_k_k'][l], W['r_k_a'][l], W['r_r_k'][l], RA)
                stage_rwkv_scan(K, cst, RA, yrd)
                stage_rwkv_finish(K, RA, yrd, W['r_ln_g'][l], W['r_ln_b'][l], ybr[1])
            if on('gdn'):
                stage_gdn_prep(K, P, W['g_conv_w'][l], W['g_A_log'][l], W['g_dt_bias'][l], gqkv, glog, gbeta)
                stage_gdn_scan(K, cst, gqkv, glog, gbeta, ygd)
                stage_gdn_finish(K, P, ygd, W['g_norm_g'][l], ybr[2])
            t_lo = nct if last else 0
            if on('merge'):
                stage_merge(K, PG, ybr, [W['w_br_m'][l], W['w_br_r'][l], W['w_br_g'][l]], W['w_out'][l], modx, lat, identb, t_lo=t_lo)
            if on('moe'):
                stage_moe(K, lat, modx, W['moe_grp_w'][l], W['moe_grp_b'][l], W['moe_exp_w'][l], W['moe_exp_b'][l],
                          W['moe_w1'][l], W['moe_w3'][l], W['moe_w2'][l], ident, identb, t_lo=t_lo)
        if on('final'):
            stage_final(K, lat, W['final_norm_g'], out)
        with nc.Block() as block:
            K.S.finalize(block)
    return nc, K


def kernel(**inputs):
    cfg = make_cfg()
    nc, _ = build_program(cfg)
    cst = make_cst()
    f32 = lambda a: np.ascontiguousarray(np.asarray(a, dtype=np.float32))
    shared = {n: f32(inputs[n]) for n, _ in PARAMS}
    shared["c_ctx"] = f32(inputs["c_ctx"])
    shared["cst"] = cst
    in_maps = []
    for b in range(cfg['BATCH']):
        m = dict(shared)
        m["x"] = f32(inputs["x"][b])
        m["ctx"] = f32(inputs["ctx"][b])
        m["c"] = f32(inputs["c"][b])
        in_maps.append(m)
    res = run_bass_kernel_spmd(nc, in_maps, core_ids=list(range(cfg['BATCH'])))
    return np.stack([np.asarray(r["out"], dtype=np.float32) for r in res.results], axis=0)


def conv_tile(K, xs, wk, acc, P, col0, NCH, base, seglen, s0, perm, bias=None):
    for k in range(5):
        edge = (s0 + k - 2 < 0) or (s0 + k - 2 + 128 > seglen)
        if edge:
            K.memset(xs[k][:], 0.0, eng='pool')
        load_rows(K, xs[k][:], P, col0, NCH, base, seglen, s0 + k - 2, 128, perm)
    K.tt(acc[:], xs[0][:], wk[:, 0, :], ALU.mult)
    for k in range(1, 5):
        K.tt(xs[k][:], xs[k][:], wk[:, k, :], ALU.mult, eng='pool')
        K.tt(acc[:], acc[:], xs[k][:], ALU.add)
    if bias is not None:
        K.tt(acc[:], acc[:], bias, ALU.add)
    K.act(acc[:], acc[:], AF.Silu)


def l2norm_heads(K, x3, tmp3, ssq, nh, hd, scale=1.0):
    K.tt(tmp3, x3, x3, ALU.mult)
    K.red(ssq, tmp3, ALU.add)
    K.ts(ssq, ssq, 1e-6, ALU.add)
    K.act(ssq, ssq, AF.Sqrt)
    K.recip(ssq, ssq)
    if scale != 1.0:
        K.ts(ssq, ssq, scale, ALU.mult)
    K.tt(x3, x3, bc3(ssq, 2, [x3.shape[0], nh, hd]), ALU.mult)


def solve_nilpotent(K, Mb, Nb, Pm, pM, pN, pP, ident64, nh):
    K.tt(Pm, Mb[0], bc3(ident64, 1, [64, nh, 64]), ALU.add)
    cur = 0
    for lvl in range(5):
        nxt = 1 - cur
        for h in range(nh):
            K.mm(pN[:, h, :], Mb[cur][:, h, :], Nb[cur][:, h, :])
            if lvl < 4:
                K.mm(pM[:, h, :], Nb[cur][:, h, :], Mb[cur][:, h, :])
        K.cp(Nb[nxt], pN, eng='act')
        if lvl < 4:
            K.cp(Mb[nxt], pM, eng='dve')
        for h in range(nh):
            K.mm(pP[:, h, :], Nb[nxt][:, h, :], Pm[:, h, :])
        K.tt(Pm, Pm, pP, ALU.add)
        cur = nxt


def stage_gdn_prep(K, P, conv_w, A_log, dt_bias, gqkv, glog, gbeta):
    cfg = K.cfg
    GW, GH = cfg['GW'], cfg['GH']
    NCH = 3 * GW
    CTX, SEQ = cfg['CTX'], cfg['SEQ']
    oq = cfg['off']['g_q'][0]
    oa = cfg['off']['g_a'][0]
    ob = cfg['off']['g_b'][0]
    with contextlib.ExitStack() as es:
        wk = K.sb(es, "gcw", [128, 5, NCH])
        for k in range(5):
            bc_row(K, wk[:, k, :], conv_w[k, :])
        aneg = K.sb(es, "ganeg", [128, 2 * GH])
        bc_row(K, aneg[:], A_log.rearrange("a b -> (a b)"))
        K.act(aneg[:], aneg[:], AF.Exp)
        dtb = K.sb(es, "gdtb", [128, 2 * GH])
        bc_row(K, dtb[:], dt_bias.rearrange("a b -> (a b)"))
        xs = [K.sb(es, f"gx{k}", [128, NCH]) for k in range(5)]
        acc = K.sb(es, "gacc", [128, NCH])
        ssq = K.sb(es, "gssq", [128, 2 * GH])
        la = K.sb(es, "gla", [128, 2 * GH])
        bt = K.sb(es, "gbt", [128, 2 * GH])
        for (base, seglen) in [(0, CTX), (CTX, SEQ)]:
            for s0 in range(0, seglen, 128):
                conv_tile(K, xs, wk, acc, P, oq, NCH, base, seglen, s0, None)
                qk3 = acc[:, 0:2 * GW].rearrange("p (h e) -> p h e", e=128)
                tmp3 = xs[0][:, 0:2 * GW].rearrange("p (h e) -> p h e", e=128)
                l2norm_heads(K, qk3, tmp3, ssq[:], 2 * GH, 128)
                K.ts(acc[:, 0:GW], acc[:, 0:GW], 128.0 ** -0.5, ALU.mult)
                rs = slice(base + s0, base + s0 + 128)
                K.dma(gqkv[rs, :], acc[:], q='sp')
                K.dma(la[:], P[rs, oa:oa + 2 * GH])
                softplus_(K, la[:], dtb[:])
                K.stt(la[:], la[:], -1.0, aneg[:], ALU.mult, ALU.mult)
                K.dma(glog[rs, :], la[:], q='sp')
                K.dma(bt[:], P[rs, ob:ob + 2 * GH])
                K.act(bt[:], bt[:], AF.Sigmoid)
                K.dma(gbeta[rs, :], bt[:], q='sp')
        K.S.barrier()


def stage_gdn_scan(K, cst, gqkv, glog, gbeta, ygd):
    cfg = K.cfg
    GW, GH = cfg['GW'], cfg['GH']
    CTX, T = cfg['CTX'], cfg['T']
    nchunk = T // 64
    HN = min(4, GH)
    ident, ones = cst[:, 0, :], cst[:, 1, :]
    i64 = cst[0:64, 0, 0:64]
    with contextlib.ExitStack() as es:
        H = K.sb(es, "dH", [128, HN, 128])
        qkv = [K.sb(es, f"dqkv{i}", [64, 3, HN * 128]) for i in range(2)]
        lgt = [K.sb(es, f"dlg{i}", [64, 2 * GH]) for i in range(2)]
        btt = [K.sb(es, f"dbt{i}", [64, 2 * GH]) for i in range(2)]
        kqT = K.sb(es, "dkqT", [128, 2 * HN, 64])
        Z = K.sb(es, "dZ", [64, HN, 64])
        G = K.sb(es, "dG", [64, HN])
        eG = K.sb(es, "deG", [64, HN])
        eE = K.sb(es, "deE", [64, HN])
        gC = K.sb(es, "dgC", [128, HN])
        beG = K.sb(es, "dbeG", [64, HN])
        E = K.sb(es, "dE", [64, HN, 64])
        Dt = K.sb(es, "dDt", [64, HN, 64])
        Dn = K.sb(es, "dDn", [64, HN, 64])
        Mb = [K.sb(es, f"dM{i}", [64, HN, 64]) for i in range(2)]
        Nb = [K.sb(es, f"dN{i}", [64, HN, 64]) for i in range(2)]
        Pm = K.sb(es, "dPm", [64, HN, 64])
        A1 = K.sb(es, "dA1", [64, HN, 64])
        bv = K.sb(es, "dbv", [64, HN, 128])
        bke = K.sb(es, "dbke", [64, HN, 128])
        u = K.sb(es, "du", [64, HN, 128])
        w = K.sb(es, "dw", [64, HN, 128])
        wT = K.sb(es, "dwT", [128, HN, 64])
        vn = K.sb(es, "dvn", [64, HN, 128])
        vE = K.sb(es, "dvE", [64, HN, 128])
        o = K.sb(es, "do", [64, HN, 128])
        pb = [K.ps(es, f"dpb{i}", [128, 512]) for i in range(7)]

        def v64(t, n, e):
            return t[0:64, 0:n * e].rearrange("p (a b) -> p a b", b=e)

        def v128(t, n, e):
            return t[:, 0:n * e].rearrange("p (a b) -> p a b", b=e)
        p_g = v128(pb[0], 4, HN)
        p_gb = v64(pb[1], HN, 64)
        p_tr = v128(pb[2], 2 * HN, 64)
        p_gr = v64(pb[3], 2 * HN, 64)
        pM, pN, pP = v64(pb[4], HN, 64), v64(pb[5], HN, 64), v64(pb[6], HN, 64)
        p_u, p_w = v64(pb[1], HN, 128), v64(pb[3], HN, 128)
        p_mt = v64(pb[2], HN, 64)
        p_wt = v128(pb[2], HN, 64)
        p_v, p_a, p_b = v64(pb[4], HN, 128), v64(pb[5], HN, 128), v64(pb[6], HN, 128)
        p_h = v128(pb[1], HN, 128)
        for hg in range(GH // HN):
            hc = slice(hg * HN * 128, (hg + 1) * HN * 128)
            for d in range(2):
                tri = cst[0:64, 2 + d, 0:64]
                strict_n = cst[0:64, 4 + (1 - d), 0:64]
                K.memset(H[:], 0.0)
                order = list(range(nchunk))
                if d == 1:
                    order = list(range(CTX // 64 - 1, -1, -1)) + list(range(nchunk - 1, CTX // 64 - 1, -1))
                for ci, c in enumerate(order):
                    x = qkv[ci % 2]
                    lg_, bt_ = lgt[ci % 2], btt[ci % 2]
                    rs = slice(c * 64, (c + 1) * 64)
                    for j in range(3):
                        K.dma(x[:, j, :], gqkv[rs, j * GW + hg * HN * 128:j * GW + (hg + 1) * HN * 128])
                    K.dma(lg_[:], glog[rs, :])
                    K.dma(bt_[:], gbeta[rs, :])
                    q3 = x[:, 0, :].rearrange("p (h e) -> p h e", e=128)
                    k3 = x[:, 1, :].rearrange("p (h e) -> p h e", e=128)
                    v3 = x[:, 2, :].rearrange("p (h e) -> p h e", e=128)
                    lgd = lg_[:, d * GH + hg * HN:d * GH + (hg + 1) * HN]
                    btd = bt_[:, d * GH + hg * HN:d * GH + (hg + 1) * HN]
                    K.mm(p_g[0:64, 0, :], tri, lgd)
                    K.mm(p_g[0:64, 1, :], ones[0:64, 0:64], lgd)
                    K.mm(p_g[:, 2, :], ones[0:64, :], lgd)
                    K.cp(G[:], p_g[0:64, 0, :])
                    K.act(eG[:], p_g[0:64, 0, :], AF.Exp)
                    K.tt(eE[:], p_g[0:64, 1, :], G[:], ALU.subtract)
                    K.act(eE[:], eE[:], AF.Exp)
                    K.act(gC[:], p_g[:, 2, :], AF.Exp)
                    K.tt(Z[:], bc3(lgd, 2, [64, HN, 64]), bc3(tri, 1, [64, HN, 64]), ALU.mult, eng='pool')
                    K.mm(p_gb, ones[0:64, 0:64], Z[:])
                    K.tt(E[:], p_gb, bc3(G[:], 2, [64, HN, 64]), ALU.subtract)
                    K.ts(Dt[:], E[:], 0.0, ALU.min)
                    K.act(Dt[:], Dt[:], AF.Exp)
                    K.ts(Dn[:], E[:], -1.0, ALU.mult, 0.0, ALU.min)
                    K.act(Dn[:], Dn[:], AF.Exp)
                    for h in range(HN):
                        K.tr(p_tr[:, h, :], k3[:, h, :], i64)
                        K.tr(p_tr[:, HN + h, :], q3[:, h, :], i64)
                    K.cp(kqT[:], p_tr, eng='act')
                    for h in range(HN):
                        K.mm(p_gr[:, h, :], kqT[:, h, :], kqT[:, h, :])
                        K.mm(p_gr[:, HN + h, :], kqT[:, h, :], kqT[:, HN + h, :])
                    K.tt(Nb[0][:], p_gr[:, 0:HN, :], Dn[:], ALU.mult)
                    K.tt(Nb[0][:], Nb[0][:], bc3(btd, 2, [64, HN, 64]), ALU.mult)
                    K.stt(Nb[0][:], Nb[0][:], -1.0, bc3(strict_n, 1, [64, HN, 64]), ALU.mult, ALU.mult)
                    K.tt(A1[:], p_gr[:, HN:2 * HN, :], Dt[:], ALU.mult)
                    K.tt(A1[:], A1[:], bc3(tri, 1, [64, HN, 64]), ALU.mult)
                    for h in range(HN):
                        K.tr(p_mt[:, h, :], Nb[0][:, h, :], i64)
                    K.cp(Mb[0][:], p_mt, eng='act')
                    solve_nilpotent(K, [Mb[0][:], Mb[1][:]], [Nb[0][:], Nb[1][:]], Pm[:], pM, pN, pP, i64, HN)
                    K.tt(bv[:], v3, bc3(btd, 2, [64, HN, 128]), ALU.mult, eng='pool')
                    K.tt(beG[:], btd, eG[:], ALU.mult)
                    K.tt(bke[:], k3, bc3(beG[:], 2, [64, HN, 128]), ALU.mult, eng='pool')
                    for h in range(HN):
                        K.mm(p_u[:, h, :], Pm[:, h, :], bv[:, h, :])
                        K.mm(p_w[:, h, :], Pm[:, h, :], bke[:, h, :])
                    K.cp(u[:], p_u, eng='act')
                    K.cp(w[:], p_w, eng='dve')
                    for h in range(HN):
                        K.tr(p_wt[:, h, :], w[:, h, :], i64)
                    K.cp(wT[:], p_wt, eng='act')
                    for h in range(HN):
                        K.mm(p_v[:, h, :], wT[:, h, :], H[:, h, :])
                        K.mm(p_a[:, h, :], kqT[:, HN + h, :], H[:, h, :])
                    K.tt(vn[:], u[:], p_v, ALU.subtract)
                    for h in range(HN):
                        K.mm(p_b[:, h, :], A1[:, h, :], vn[:, h, :])
                    K.tt(vE[:], vn[:], bc3(eE[:], 2, [64, HN, 128]), ALU.mult, eng='pool')
                    for h in range(HN):
                        K.mm(p_h[:, h, :], k3[:, h, :], vE[:, h, :])
                    K.tt(o[:], p_a, bc3(eG[:], 2, [64, HN, 128]), ALU.mult)
                    K.tt(o[:], o[:], p_b, ALU.add)
                    K.dma(ygd[d][rs, hc], o[:].rearrange("p h e -> p (h e)"), q='sp')
                    K.tt(H[:], H[:], bc3(gC[:], 2, [128, HN, 128]), ALU.mult)
                    K.tt(H[:], H[:], p_h, ALU.add)
        K.S.barrier()


def stage_gdn_finish(K, P, ygd, norm_g, ygf):
    cfg = K.cfg
    GW, GH = cfg['GW'], cfg['GH']
    T = cfg['T']
    og = cfg['off']['g_g'][0]
    with contextlib.ExitStack() as es:
        ng = K.sb(es, "gng", [128, 128])
        bc_row(K, ng[:], norm_g)
        y0 = [K.sb(es, f"hy0{i}", [128, GW]) for i in range(2)]
        y1 = [K.sb(es, f"hy1{i}", [128, GW]) for i in range(2)]
        gg = [K.sb(es, f"hgg{i}", [128, GW]) for i in range(2)]
        tmp = K.sb(es, "htmp", [128, GW])
        ssq = K.sb(es, "hssq", [128, GH])
        for i, t0 in enumerate(range(0, T, 128)):
            a, b, g = y0[i % 2], y1[i % 2], gg[i % 2]
            rs = slice(t0, t0 + 128)
            K.dma(a[:], ygd[0][rs, :])
            K.dma(b[:], ygd[1][rs, :])
            K.dma(g[:], P[rs, og:og + GW])
            K.tt(a[:], a[:], b[:], ALU.add)
            a3 = a[:].rearrange("p (h e) -> p h e", e=128)
            t3 = tmp[:].rearrange("p (h e) -> p h e", e=128)
            K.tt(t3, a3, a3, ALU.mult)
            K.red(ssq[:], t3, ALU.add)
            K.rsqrt(es, ssq[:], ssq[:], 1.0 / 128, 1e-6)
            K.tt(a3, a3, bc3(ssq[:], 2, [128, GH, 128]), ALU.mult)
            K.tt(a3, a3, bc3(ng[:], 1, [128, GH, 128]), ALU.mult, eng='pool')
            K.act(g[:], g[:], AF.Silu)
            K.tt(a[:], a[:], g[:], ALU.mult)
            K.dma(ygf[rs, :], a[:], q='sp')
        K.S.barrier()


def stage_rwkv_prep(K, cst, P, shift_mu, w0, w_up, a0, a_up, g_up, k_k, k_a, r_k, RA):
    cfg = K.cfg
    RW, RH = cfg['RW'], cfg['RH']
    CTX, SEQ = cfg['CTX'], cfg['SEQ']
    orr = cfg['off']['r_r'][0]
    ow = cfg['off']['r_w'][0]
    ident = cst[:, 0, :]
    NB = (RW + 511) // 512
    with contextlib.ExitStack() as es:
        mu = K.sb(es, "rmu", [128, 3, 3 * RW])
        bc_row(K, mu[:, 0, :], shift_mu[0, :])
        bc_row(K, mu[:, 1, :], shift_mu[1, :])
        K.tt(mu[:, 2, :], mu[:, 0, :], mu[:, 1, :], ALU.add)
        K.ts(mu[:, 2, :], mu[:, 2, :], -1.0, ALU.mult, 1.0, ALU.add)
        pv = K.sb(es, "rpv", [128, 5, RW])
        bc_row(K, pv[:, 0, :], w0[0, :])
        bc_row(K, pv[:, 1, :], w0[1, :])
        bc_row(K, pv[:, 2, :], a0)
        bc_row(K, pv[:, 3, :], k_k)
        bc_row(K, pv[:, 4, :], k_a)
        rkb = K.sb(es, "rrk", [128, RW])
        bc_row(K, rkb[:], r_k.rearrange("a b -> (a b)"))
        wu = K.sb(es, "rwu", [64, 3, RW])
        K.dma(wu[:, 0, :], w_up[0])
        K.dma(wu[:, 1, :], w_up[1])
        K.dma(wu[:, 2, :], a_up)
        gu = K.sb(es, "rgu", [128, RW])
        K.dma(gu[:], g_up)
        x = K.sb(es, "rx", [128, 3 * RW])
        xp = K.sb(es, "rxp", [128, 3 * RW])
        xn = K.sb(es, "rxn", [128, 3 * RW])
        lo = K.sb(es, "rlo", [128, 320])
        loT = K.sb(es, "rloT", [128, 4, 128])
        t1 = K.sb(es, "rt1", [128, RW])
        t2 = K.sb(es, "rt2", [128, RW])
        av = K.sb(es, "rav", [128, RW])
        ssq = K.sb(es, "rssq", [128, RH])
        p_tr = K.ps(es, "rp_tr", [128, 4, 128])
        p_l = [K.ps(es, f"rp_l{i}", [128, 512]) for i in range(2)]
        li = 0

        def lora(dst, lhsT, rhs):
            nonlocal li
            for nb in range(NB):
                n0, n1 = nb * 512, min(RW, (nb + 1) * 512)
                p = p_l[li % 2]
                li += 1
                K.mm(p[:, 0:n1 - n0], lhsT, rhs[:, n0:n1])
                K.cp(dst[:, n0:n1], p[:, 0:n1 - n0], eng='act')
        for (base, seglen) in [(0, CTX), (CTX, SEQ)]:
            for s0 in range(0, seglen, 128):
                rs = slice(base + s0, base + s0 + 128)
                K.dma(x[:], P[rs, orr:orr + 3 * RW])
                if s0 == 0:
                    K.memset(xp[:], 0.0, eng='pool')
                if s0 + 128 >= seglen:
                    K.memset(xn[:], 0.0, eng='pool')
                load_rows(K, xp[:], P, orr, 3 * RW, base, seglen, s0 - 1, 128)
                load_rows(K, xn[:], P, orr, 3 * RW, base, seglen, s0 + 1, 128)
                K.tt(x[:], x[:], mu[:, 2, :], ALU.mult)
                K.tt(xp[:], xp[:], mu[:, 0, :], ALU.mult, eng='pool')
                K.tt(xn[:], xn[:], mu[:, 1, :], ALU.mult, eng='pool')
                K.tt(x[:], x[:], xp[:], ALU.add)
                K.tt(x[:], x[:], xn[:], ALU.add)
                r_, k_, v_ = x[:, 0:RW], x[:, RW:2 * RW], x[:, 2 * RW:3 * RW]
                K.dma(RA['r'][rs, :], r_, q='sp')
                K.dma(RA['v'][rs, :], v_, q='sp')
                K.dma(lo[:], P[rs, ow:ow + 320])
                K.act(lo[:, 0:128], lo[:, 0:128], AF.Tanh)
                K.act(lo[:, 192:320], lo[:, 192:320], AF.Sigmoid)
                K.tr(p_tr[0:64, 0, :], lo[:, 0:64], ident)
                K.tr(p_tr[0:64, 1, :], lo[:, 64:128], ident)
                K.tr(p_tr[0:64, 2, :], lo[:, 128:192], ident)
                K.tr(p_tr[:, 3, :], lo[:, 192:320], ident)
                K.cp(loT[0:64, 0:3, :], p_tr[0:64, 0:3, :], eng='act')
                K.cp(loT[:, 3, :], p_tr[:, 3, :], eng='dve')
                for d in range(2):
                    lora(t1, loT[0:64, d, :], wu[:, d, :])
                    K.tt(t1[:], t1[:], pv[:, d, :], ALU.add)
                    K.act(t1[:], t1[:], AF.Sigmoid)
                    K.ts(t1[:], t1[:], -math.exp(-0.5), ALU.mult)
                    K.dma(RA['lw'][rs, d * RW:(d + 1) * RW], t1[:], q='sp')
                lora(av, loT[0:64, 2, :], wu[:, 2, :])
                K.tt(av[:], av[:], pv[:, 2, :], ALU.add)
                K.act(av[:], av[:], AF.Sigmoid)
                K.dma(RA['a'][rs, :], av[:], q='sp')
                lora(t2, loT[:, 3, :], gu[:])
                K.dma(RA['g'][rs, :], t2[:], q='sp')
                K.tt(t1[:], k_, pv[:, 3, :], ALU.mult)
                l2norm_heads(K, t1[:].rearrange("p (h e) -> p h e", e=64), t2[:].rearrange("p (h e) -> p h e", e=64),
                             ssq[:], RH, 64)
                K.dma(RA['kk'][rs, :], t1[:], q='sp')
                K.stt(t2[:], av[:], -1.0, pv[:, 4, :], ALU.add, ALU.mult)
                K.stt(t2[:], t2[:], 1.0, k_, ALU.add, ALU.mult)
                K.dma(RA['k'][rs, :], t2[:], q='sp')
                K.tt(t2[:], t2[:], r_, ALU.mult)
                K.tt(t2[:], t2[:], rkb[:], ALU.mult)
                K.red(ssq[:], t2[:].rearrange("p (h e) -> p h e", e=64), ALU.add)
                K.dma(RA['bon'][rs, :], ssq[:], q='sp')
        K.S.barrier()


def stage_rwkv_scan(K, cst, RA, yrd):
    cfg = K.cfg
    RW, RH = cfg['RW'], cfg['RH']
    CTX, T = cfg['CTX'], cfg['T']
    nchunk = T // 64
    HN = min(4, RH)
    W_ = HN * 64
    ones = cst[:, 1, :]
    i64 = cst[0:64, 0, 0:64]
    MID = 32
    with contextlib.ExitStack() as es:
        H = K.sb(es, "wH", [64, HN, 64])
        Hs = K.sb(es, "wHs", [64, HN, 64])
        names = ('r', 'k', 'v', 'kk', 'a')
        IN = [{n: K.sb(es, f"w{n}{i}", [64, W_]) for n in names + ('lw',)} for i in range(2)]
        trimid = K.sb(es, "wtrimid", [64, 64])
        col2 = K.sb(es, "wcol2", [64, 2])
        Gm = K.sb(es, "wGm", [64, W_])
        eP = K.sb(es, "weP", [64, W_])
        eN = K.sb(es, "weN", [64, W_])
        eX = K.sb(es, "weX", [64, W_])
        eE = K.sb(es, "weE", [64, W_])
        gcm = K.sb(es, "wgcm", [64, HN, 2])
        bt = K.sb(es, "wbt", [64, W_])
        SC = K.sb(es, "wSC", [64, 4, W_])
        Ke = K.sb(es, "wKe", [64, W_])
        Be = K.sb(es, "wBe", [64, W_])
        FT = K.sb(es, "wFT", [64, 4, HN, 64])
        Mb = [K.sb(es, f"wM{i}", [64, HN, 64]) for i in range(2)]
        Nb = [K.sb(es, f"wN{i}", [64, HN, 64]) for i in range(2)]
        Pm = K.sb(es, "wPm", [64, HN, 64])
        AK = K.sb(es, "wAK", [64, HN, 64])
        QK = K.sb(es, "wQK", [64, HN, 64])
        QB = K.sb(es, "wQB", [64, HN, 64])
        Z0 = K.sb(es, "wZ0", [64, HN, 64])
        Ut = K.sb(es, "wUt", [64, HN, 64])
        WT = K.sb(es, "wWT", [64, HN, 64])
        U = K.sb(es, "wU", [64, HN, 64])
        Y = K.sb(es, "wY", [64, HN, 64])
        pb = [K.ps(es, f"wpb{i}", [64, 512]) for i in range(7)]

        def v3(t, n):
            return t[:, 0:n * 64].rearrange("p (a b) -> p a b", b=64)
        old_ser = K.S.ser_engs
        import os
        sf, st_ = int(os.environ.get('RW_SER_FROM', '0')), int(os.environ.get('RW_SER_TO', '0'))

        def sec(i):
            K.S.ser_engs = tuple(set(old_ser) | {'act', 'dve'}) if sf <= i < st_ else old_ser
        for hg in range(RH // HN):
            hc = slice(hg * W_, (hg + 1) * W_)
            for d in range(2):
                tri = cst[0:64, 2 + d, 0:64]
                stri = cst[0:64, 4 + d, 0:64]
                strict_n = cst[0:64, 4 + (1 - d), 0:64]
                K.ts(trimid[:], tri, tri[:, MID:MID + 1], ALU.subtract)
                K.cp(col2[:, 0:1], ones[0:64, 0:1])
                K.cp(col2[:, 1:2], tri[:, MID:MID + 1])
                K.memset(H[:], 0.0)
                order = list(range(nchunk))
                if d == 1:
                    order = list(range(CTX // 64 - 1, -1, -1)) + list(range(nchunk - 1, CTX // 64 - 1, -1))
                for ci, c in enumerate(order):
                    X = IN[ci % 2]
                    rs = slice(c * 64, (c + 1) * 64)
                    for n in names:
                        K.dma(X[n][:], RA[n][rs, hc])
                    K.dma(X['lw'][:], RA['lw'][rs, d * RW + hg * W_:d * RW + (hg + 1) * W_])
                    sec(0)
                    lw = X['lw'][:]
                    K.mm(pb[0][:, 0:W_], trimid[:], lw)
                    K.mm(pb[0][:, W_:2 * W_], strict_n, lw)
                    pgc = pb[1][:, 0:2 * HN].rearrange("p (a b) -> p a b", b=2)
                    for h in range(HN):
                        K.mm(pgc[:, h, :], X['lw'][:, h * 64:(h + 1) * 64], col2[:])
                    K.cp(Gm[:], pb[0][:, 0:W_])
                    K.act(eP[:], pb[0][:, 0:W_], AF.Exp)
                    K.ts(eN[:], Gm[:], -1.0, ALU.mult)
                    K.act(eN[:], eN[:], AF.Exp)
                    K.act(eE[:], pb[0][:, W_:2 * W_], AF.Exp)
                    K.tt(eX[:], Gm[:], lw, ALU.subtract)
                    K.act(eX[:], eX[:], AF.Exp)
                    K.act(gcm[:], pgc, AF.Exp)
                    sec(1)
                    K.tt(bt[:], X['kk'][:], X['a'][:], ALU.mult, eng='pool')
                    K.tt(SC[:, 0, :], X['k'][:], eN[:], ALU.mult)
                    K.tt(SC[:, 1, :], bt[:], eN[:], ALU.mult)
                    K.stt(SC[:, 2, :], X['kk'][:], -1.0, eX[:], ALU.mult, ALU.mult)
                    K.tt(SC[:, 3, :], X['r'][:], eP[:], ALU.mult)
                    K.tt(Ke[:], X['k'][:], eE[:], ALU.mult, eng='pool')
                    K.tt(Be[:], bt[:], eE[:], ALU.mult, eng='pool')
                    K.tt(Hs[:], H[:], bc3(gcm[:, :, 1], 2, [64, HN, 64]), ALU.mult)
                    sec(2)
                    for j in range(4):
                        pt = pb[2 + (j % 2)]
                        for h in range(HN):
                            K.tr(v3(pt, HN)[:, h, :], SC[:, j, h * 64:(h + 1) * 64], i64)
                        K.cp(FT[:, j, :, :], v3(pt, HN), eng=('act' if j % 2 else 'dve'))
                    kT, bT, aT, qT = (FT[:, j, :, :] for j in range(4))
                    sec(3)
                    pMN = v3(pb[4], 2 * HN)
                    pG2 = v3(pb[5], 2 * HN)
                    pG3 = v3(pb[6], HN)
                    for h in range(HN):
                        K.mm(pMN[:, h, :], bT[:, h, :], aT[:, h, :])
                        K.mm(pMN[:, HN + h, :], aT[:, h, :], bT[:, h, :])
                        K.mm(pG2[:, h, :], kT[:, h, :], aT[:, h, :])
                        K.mm(pG2[:, HN + h, :], kT[:, h, :], qT[:, h, :])
                        K.mm(pG3[:, h, :], bT[:, h, :], qT[:, h, :])
                    K.tt(Mb[0][:], pMN[:, 0:HN, :], bc3(stri, 1, [64, HN, 64]), ALU.mult)
                    K.tt(Nb[0][:], pMN[:, HN:2 * HN, :], bc3(strict_n, 1, [64, HN, 64]), ALU.mult)
                    K.tt(AK[:], pG2[:, 0:HN, :], bc3(stri, 1, [64, HN, 64]), ALU.mult)
                    K.tt(QK[:], pG2[:, HN:2 * HN, :], bc3(tri, 1, [64, HN, 64]), ALU.mult)
                    K.tt(QB[:], pG3, bc3(tri, 1, [64, HN, 64]), ALU.mult)
                    sec(4)
                    solve_nilpotent(K, [Mb[0][:], Mb[1][:]], [Nb[0][:], Nb[1][:]], Pm[:],
                                    v3(pb[2], HN), v3(pb[3], HN), v3(pb[4], HN), i64, HN)
                    sec(5)
                    pz = v3(pb[5], HN)
                    for h in range(HN):
                        K.mm(pz[:, h, :], AK[:, h, :], X['v'][:, h * 64:(h + 1) * 64])
                    K.cp(Z0[:], pz, eng='act')
                    pu = v3(pb[6], HN)
                    pw = v3(pb[5], HN)
                    for h in range(HN):
                        K.mm(pu[:, h, :], Pm[:, h, :], Z0[:, h, :])
                        K.mm(pw[:, h, :], SC[:, 2, h * 64:(h + 1) * 64], Pm[:, h, :])
                    K.cp(Ut[:], pu, eng='act')
                    K.cp(WT[:], pw, eng='dve')
                    sec(6)
                    p1 = v3(pb[2], HN)
                    for h in range(HN):
                        K.mm(p1[:, h, :], WT[:, h, :], Hs[:, h, :])
                    K.tt(U[:], Ut[:], p1, ALU.add)
                    py = v3(pb[3], HN)
                    ph = v3(pb[4], HN)
                    for h in range(HN):
                        vh = X['v'][:, h * 64:(h + 1) * 64]
                        K.mm(py[:, h, :], qT[:, h, :], Hs[:, h, :], start=True, stop=False)
                        K.mm(py[:, h, :], QB[:, h, :], U[:, h, :], start=False, stop=False)
                        K.mm(py[:, h, :], QK[:, h, :], vh, start=False, stop=True)
                    for h in range(HN):
                        vh = X['v'][:, h * 64:(h + 1) * 64]
                        K.mm(ph[:, h, :], Be[:, h * 64:(h + 1) * 64], U[:, h, :], start=True, stop=False)
                        K.mm(ph[:, h, :], Ke[:, h * 64:(h + 1) * 64], vh, start=False, stop=True)
                    K.cp(Y[:], py, eng='act')
                    K.dma(yrd[d][rs, hc], Y[:].rearrange("p h e -> p (h e)"), q='sp')
                    K.tt(H[:], H[:], bc3(gcm[:, :, 0], 2, [64, HN, 64]), ALU.mult)
                    K.tt(H[:], H[:], ph, ALU.add)
        K.S.ser_engs = old_ser
        K.S.barrier()


def stage_rwkv_finish(K, RA, yrd, ln_g, ln_b, yrf):
    cfg = K.cfg
    RW, RH = cfg['RW'], cfg['RH']
    T = cfg['T']
    with contextlib.ExitStack() as es:
        lg = K.sb(es, "vlg", [128, RW])
        lb = K.sb(es, "vlb", [128, RW])
        bc_row(K, lg[:], ln_g)
        bc_row(K, lb[:], ln_b)
        y0 = [K.sb(es, f"vy0{i}", [128, RW]) for i in range(2)]
        y1 = [K.sb(es, f"vy1{i}", [128, RW]) for i in range(2)]
        vv = [K.sb(es, f"vvv{i}", [128, RW]) for i in range(2)]
        gg = [K.sb(es, f"vgg{i}", [128, RW]) for i in range(2)]
        bo = [K.sb(es, f"vbo{i}", [128, RH]) for i in range(2)]
        tmp = K.sb(es, "vtmp", [128, RW])
        st = K.sb(es, "vst", [128, 2, RH])
        for i, t0 in enumerate(range(0, T, 128)):
            a, b, v, g, bn = y0[i % 2], y1[i % 2], vv[i % 2], gg[i % 2], bo[i % 2]
            rs = slice(t0, t0 + 128)
            K.dma(a[:], yrd[0][rs, :])
            K.dma(b[:], yrd[1][rs, :])
            K.dma(v[:], RA['v'][rs, :])
            K.dma(g[:], RA['g'][rs, :])
            K.dma(bn[:], RA['bon'][rs, :])
            K.tt(a[:], a[:], b[:], ALU.add)
            a3 = a[:].rearrange("p (h e) -> p h e", e=64)
            t3 = tmp[:].rearrange("p (h e) -> p h e", e=64)
            K.red(st[:, 0, :], a3, ALU.add)
            K.ts(st[:, 0, :], st[:, 0, :], 1.0 / 64, ALU.mult)
            K.tt(a3, a3, bc3(st[:, 0, :], 2, [128, RH, 64]), ALU.subtract)
            K.tt(t3, a3, a3, ALU.mult)
            K.red(st[:, 1, :], t3, ALU.add)
            K.rsqrt(es, st[:, 1, :], st[:, 1, :], 1.0 / 64, 64e-5)
            K.tt(a3, a3, bc3(st[:, 1, :], 2, [128, RH, 64]), ALU.mult)
            K.tt(a[:], a[:], lg[:], ALU.mult, eng='pool')
            K.tt(a[:], a[:], lb[:], ALU.add)
            v3_ = v[:].rearrange("p (h e) -> p h e", e=64)
            K.tt(v3_, v3_, bc3(bn[:], 2, [128, RH, 64]), ALU.mult, eng='pool')
            K.tt(a[:], a[:], v[:], ALU.add)
            K.tt(a[:], a[:], g[:], ALU.mult)
            K.dma(yrf[rs, :], a[:], q='sp')
        K.S.barrier()
```

```python
import math
import contextlib
import numpy as np
import concourse.bass as bass
import concourse.mybir as mybir
from concourse.bass_utils import run_bass_kernel_spmd

F32 = mybir.dt.float32
BF16 = mybir.dt.bfloat16
AF = mybir.ActivationFunctionType
ALU = mybir.AluOpType
AX = mybir.AxisListType


def make_cfg(D=2048, SEQ=4096, CTX=256, DEPTH=2, BATCH=2):
    c = dict(D=D, SEQ=SEQ, CTX=CTX, DEPTH=DEPTH, BATCH=BATCH, GRID_W=64, CHUNK=64)
    c['T'] = CTX + SEQ
    c['MW'] = D // 2; c['MH'] = c['MW'] // 64; c['MG'] = 2; c['MS'] = 128
    c['RW'] = D // 2; c['RH'] = c['RW'] // 64
    c['GW'] = D // 2; c['GH'] = c['GW'] // 128
    c['NE'] = 32; c['FF'] = D // 4
    cols = (("m_z", c['MW']), ("m_x", c['MW']), ("m_B", 256), ("m_C", 256), ("m_dt", 2 * c['MH']),
            ("r_r", c['RW']), ("r_k", c['RW']), ("r_v", c['RW']), ("r_w", 128), ("r_a", 64), ("r_g", 128),
            ("g_q", c['GW']), ("g_k", c['GW']), ("g_v", c['GW']), ("g_a", 2 * c['GH']), ("g_b", 2 * c['GH']),
            ("g_g", c['GW']), ("gate", 3 * D))
    off = {}
    s = 0
    for n, w in cols:
        off[n] = (s, w)
        s += w
    c['off'] = off
    c['INW'] = s
    return c


class Sched:
    NDS = 24
    serialize = False
    ser_engs = ()

    def __init__(self, nc):
        self.nc = nc
        self.eng = {'pe': nc.tensor, 'act': nc.scalar, 'dve': nc.vector, 'pool': nc.gpsimd, 'sp': nc.sync}
        self.prog = {e: [] for e in self.eng}
        self.ccnt = {e: 0 for e in self.eng}
        self.sems = {}
        for e in self.eng:
            self.sems[('c', e)] = nc.alloc_semaphore(name=f"c_{e}")
        self.dcnt = [0] * self.NDS
        for i in range(self.NDS):
            self.sems[('d', i)] = nc.alloc_semaphore(name=f"d_{i}")
        self.drr = 0
        self.seen = {e: {} for e in self.eng}
        self.rec = {}
        self.rows = {}
        self.psum = set()
        self.nops = 0

    def reg_tensor(self, name, shape, space):
        rs = 1
        for s in shape[1:]:
            rs *= s
        self.rows[name] = rs if space != 'dram' else None
        if space == 'ps':
            self.psum.add(name)

    def region(self, ap):
        name = ap.name
        pat = ap.ap
        off = int(ap.offset)
        rs = self.rows[name]
        if name in self.psum:
            return name, (0, 128, 0, rs)
        if rs is None:
            lo = hi = off
            for st, n in pat:
                if n > 1:
                    if st >= 0:
                        hi += st * (n - 1)
                    else:
                        lo += st * (n - 1)
            return name, (0, 1, lo, hi + 1)
        p0 = off // rs
        f0 = off % rs
        npart = pat[0][1]
        lo = hi = f0
        for st, n in pat[1:]:
            if n > 1:
                if st >= 0:
                    hi += st * (n - 1)
                else:
                    lo += st * (n - 1)
        return name, (p0, p0 + npart, lo, hi + 1)

    def dense(self, ap):
        name = ap.name
        if name in self.psum:
            return True
        pat = ap.ap
        rs = self.rows[name]
        n = 1
        for st, c in (pat if rs is None else pat[1:]):
            n *= c if st != 0 else 1
        _, reg = self.region(ap)
        return n == reg[3] - reg[2]

    @staticmethod
    def _ov(a, b):
        return a[0] < b[1] and b[0] < a[1] and a[2] < b[3] and b[2] < a[3]

    @staticmethod
    def _cov(a, b):
        return a[0] <= b[0] and a[1] >= b[1] and a[2] <= b[2] and a[3] >= b[3]

    def _deps(self, reads, writes, me=None):
        deps = {}
        rr = [self.region(a) for a in reads]
        ww = [self.region(a) + (self.dense(a),) for a in writes]
        for name, reg in rr:
            ps = name in self.psum
            for r in self.rec.get(name, ()):
                if (r[1] == 'W' or (ps and r[2] != me)) and self._ov(r[0], reg):
                    deps[r[2]] = max(deps.get(r[2], 0), r[3])
        for name, reg, _dn in ww:
            for r in self.rec.get(name, ()):
                if self._ov(r[0], reg):
                    deps[r[2]] = max(deps.get(r[2], 0), r[3])
        return deps, rr, ww

    def _record(self, rr, ww, key, val):
        for name, reg, dn in ww:
            lst = self.rec.setdefault(name, [])
            if dn:
                lst[:] = [r for r in lst if not self._cov(reg, r[0])]
            lst.append([reg, 'W', key, val])
        for name, reg in rr:
            lst = self.rec.setdefault(name, [])
            for r in lst:
                if r[1] == 'R' and r[2] == key and r[0] == reg:
                    r[3] = max(r[3], val)
                    break
            else:
                lst.append([reg, 'R', key, val])

    def _emit_waits(self, e, deps, skip_self=False):
        for key, val in deps.items():
            if skip_self and key == ('c', e):
                continue
            if key == ('c', 'pe'):
                if e == 'pe':
                    continue
                if self.ccnt['pe'] <= val:
                    self.ccnt['pe'] += 1
                    self.prog['pe'].append(('op', self.dummy_fn, key, 1))
                val = val + 1
            if self.seen[e].get(key, 0) >= val:
                continue
            self.seen[e][key] = val
            self.prog[e].append(('wait', key, val))

    def op(self, e, fn, reads, writes, pe_chain=False):
        deps, rr, ww = self._deps(reads, writes, me=('c', e))
        self._emit_waits(e, deps, skip_self=pe_chain)
        self.ccnt[e] += 1
        key = ('c', e)
        self.prog[e].append(('op', fn, key, 1))
        self._record(rr, ww, key, self.ccnt[e])
        self.nops += 1
        if self.serialize or e in self.ser_engs:
            self.barrier()

    def dma(self, out, in_, q='sp', **kw):
        deps, rr, ww = self._deps([in_], [out])
        k = self.drr
        self.drr = (self.drr + 1) % self.NDS
        key = ('d', k)
        if self.dcnt[k] > 0:
            deps[key] = max(deps.get(key, 0), self.dcnt[k])
        self._emit_waits(q, deps)
        self.dcnt[k] += 16

        def fn(eng, out=out, in_=in_, kw=kw):
            return eng.dma_start(out=out, in_=in_, **kw)
        self.prog[q].append(('op', fn, key, 16))
        self._record(rr, ww, key, self.dcnt[k])
        self.nops += 1
        if self.serialize or 'dma' in self.ser_engs:
            self.barrier()

    def _all_tokens(self):
        final = {}
        for e in self.eng:
            if self.ccnt[e] > 0:
                final[('c', e)] = self.ccnt[e]
        for i in range(self.NDS):
            if self.dcnt[i] > 0:
                final[('d', i)] = self.dcnt[i]
        return final

    def barrier(self, drop=()):
        final = self._all_tokens()
        for e in self.eng:
            self._emit_waits(e, dict(final))
        for n in drop:
            self.rec.pop(n, None)

    def finalize(self, block):
        self._emit_waits('sp', self._all_tokens())
        sems = self.sems

        def mk(e):
            prog = self.prog[e]

            def body(eng):
                for it in prog:
                    if it[0] == 'wait':
                        eng.wait_ge(sems[it[1]], it[2])
                    else:
                        it[1](eng).then_inc(sems[it[2]], it[3])
            return body
        block.tensor(mk('pe'))
        block.scalar(mk('act'))
        block.vector(mk('dve'))
        block.gpsimd(mk('pool'))
        block.sync(mk('sp'))


def _is_ap(x):
    return hasattr(x, 'ap') and hasattr(x, 'offset')


class KB:
    def __init__(self, nc, cfg, es):
        self.nc = nc
        self.cfg = cfg
        self.S = Sched(nc)
        self.uid = 0
        self.rr = 0
        self.pool_eng = 'pool'
        self.dma_queues = ('sp', 'act')
        self.es = es
        dsb = self.sb(es, "dmy_sb", [128, 8], BF16)
        dps = self.ps(es, "dmy_ps", [128, 8])
        self.S.dummy_fn = lambda e: e.matmul(dps[0:8, 0:8], lhsT=dsb[0:8, 0:8], rhs=dsb[0:8, 0:8], start=True, stop=True)
        self.memset(dsb[:], 0.0)
        self.mm(dps[0:8, 0:8], dsb[0:8, 0:8], dsb[0:8, 0:8])

    def dram(self, name, shape, dt=F32, kind=None):
        if kind is None:
            t = self.nc.dram_tensor(name, list(shape), dt)
        else:
            t = self.nc.dram_tensor(name, list(shape), dt, kind=kind)
        self.S.reg_tensor(name, shape, 'dram')
        return t.ap()

    def sb(self, es, name, shape, dt=F32):
        self.uid += 1
        name = f"{name}_{self.uid}"
        t = es.enter_context(self.nc.sbuf_tensor(name, list(shape), dt))
        self.S.reg_tensor(name, shape, 'sb')
        return t

    def ps(self, es, name, shape, dt=F32):
        self.uid += 1
        name = f"{name}_{self.uid}"
        full = [128, 512] if dt == F32 else [128, 1024]
        t = es.enter_context(self.nc.psum_tensor(name, full, dt))
        self.S.reg_tensor(name, full, 'ps')
        n = 1
        for d in shape[1:]:
            n *= d
        assert n <= full[1] and shape[0] <= 128
        v = t[0:shape[0], 0:n]
        if len(shape) == 3:
            v = v.rearrange("p (a b) -> p a b", b=shape[2])
        return v

    def dma(self, out, in_, q=None, **kw):
        if q is None:
            q = self.dma_queues[self.rr % len(self.dma_queues)]
            self.rr += 1
        self.S.dma(out, in_, q=q, **kw)

    def mm(self, out, lhsT, rhs, start=True, stop=True):
        self.S.op('pe', lambda e: e.matmul(out, lhsT=lhsT, rhs=rhs, start=start, stop=stop),
                  [lhsT, rhs] + ([] if start else [out]), [out], pe_chain=not start)

    def tr(self, out, in_, ident):
        self.S.op('pe', lambda e: e.transpose(out, in_, ident), [in_, ident], [out])

    def act(self, out, in_, func, bias=None, scale=None, accum_out=None, eng='act'):
        kw = {}
        rd = [in_]
        wr = [out]
        if bias is not None:
            kw['bias'] = bias
            if _is_ap(bias):
                rd.append(bias)
        if scale is not None:
            kw['scale'] = scale
            if _is_ap(scale):
                rd.append(scale)
        if accum_out is not None:
            kw['accum_out'] = accum_out
            wr.append(accum_out)
        self.S.op('act', lambda e: e.activation(out=out, in_=in_, func=func, **kw), rd, wr)

    def tt(self, out, in0, in1, op, eng='dve'):
        self.S.op(eng, lambda e: e.tensor_tensor(out=out, in0=in0, in1=in1, op=op), [in0, in1], [out])

    def ts(self, out, in0, s1, op0, s2=None, op1=None, eng='dve', accum_out=None):
        rd = [in0] + [s for s in (s1, s2) if _is_ap(s)]
        kw = {}
        wr = [out]
        if op1 is not None:
            kw['op1'] = op1
        if accum_out is not None:
            kw['accum_out'] = accum_out
            wr.append(accum_out)
        self.S.op(eng, lambda e: e.tensor_scalar(out=out, in0=in0, scalar1=s1, scalar2=s2, op0=op0, **kw), rd, wr)

    def stt(self, out, in0, scalar, in1, op0, op1):
        rd = [in0, in1] + ([scalar] if _is_ap(scalar) else [])
        self.S.op('dve', lambda e: e.scalar_tensor_tensor(out=out, in0=in0, scalar=scalar, in1=in1, op0=op0, op1=op1),
                  rd, [out])

    def red(self, out, in_, op, axis=AX.X):
        self.S.op('dve', lambda e: e.tensor_reduce(out=out, in_=in_, axis=axis, op=op), [in_], [out])

    def cp(self, out, in_, eng='dve'):
        if eng == 'act':
            self.S.op('act', lambda e: e.activation(out=out, in_=in_, func=AF.Identity), [in_], [out])
        else:
            self.S.op(eng, lambda e: e.tensor_copy(out=out, in_=in_), [in_], [out])

    def memset(self, ap, val, eng='dve'):
        self.S.op(eng, lambda e: e.memset(ap, val), [], [ap])

    def recip(self, out, in_):
        self.S.op('dve', lambda e: e.reciprocal(out=out, in_=in_), [in_], [out])

    def rsqrt(self, es_tmp, out, in_, scale, eps):
        self.ts(out, in_, scale, ALU.mult, eps, ALU.add)
        self.act(out, out, AF.Sqrt)
        self.recip(out, out)


def stage_modvec(K, es0, c_b, c_ctx, ada_w, ada_b, n1g, n2g, modx):
    cfg = K.cfg
    D = cfg['D']
    KC = D // 128
    with contextlib.ExitStack() as es:
        cT = K.sb(es, "cT", [128, KC, 2])
        K.dma(cT[:, :, 0], c_b.rearrange("(k p) -> p k", p=128), q='sp', allow_slow_non_contiguous=True)
        K.dma(cT[:, :, 1], c_ctx.rearrange("(k p) -> p k", p=128), q='sp', allow_slow_non_contiguous=True)
        K.act(cT[:], cT[:], AF.Silu)
        mod = K.sb(es, "mod", [2, 6 * D])
        bt = [K.sb(es, f"mbt{i}", [2, 512]) for i in range(2)]
        wt = [K.sb(es, f"mw{i}", [128, KC, 512]) for i in range(2)]
        acc = [K.ps(es, f"macc{i}", [2, 512]) for i in range(2)]
        nb = 6 * D // 512
        for cb in range(nb):
            w = wt[cb % 2]
            bb = bt[cb % 2]
            dma_w(K, w[:], ada_w[:, cb * 512:(cb + 1) * 512])
            bc_row(K, bb[:], ada_b[cb * 512:(cb + 1) * 512], n=2, q='sp')
            a = acc[cb % 2]
            for k in range(KC):
                K.mm(a[:], cT[:, k, :], w[:, k, :], start=(k == 0), stop=(k == KC - 1))
            K.tt(mod[:, cb * 512:(cb + 1) * 512], a[:], bb[:], ALU.add)
        g = K.sb(es, "mg", [2, 2, D])
        bc_row(K, g[:, 0, :], n1g, n=2, q='sp')
        bc_row(K, g[:, 1, :], n2g, n=2, q='sp')
        K.stt(mod[:, 1 * D:2 * D], mod[:, 1 * D:2 * D], 1.0, g[:, 0, :], ALU.add, ALU.mult)
        K.stt(mod[:, 4 * D:5 * D], mod[:, 4 * D:5 * D], 1.0, g[:, 1, :], ALU.add, ALU.mult)
        for r_, src in enumerate((1, 0, 2, 4, 3, 5)):
            K.dma(modx[:, r_, :], mod[:, src * D:(src + 1) * D], q='sp')
        K.S.barrier()


def dma_w(K, dst, src, q=None):
    KCn = dst.shape[1]
    for k0 in range(0, KCn, 4):
        k1 = min(KCn, k0 + 4)
        K.dma(dst[:, k0:k1, :], src[k0 * 128:k1 * 128, :].rearrange("(k p) n -> p k n", p=128), q=q)


def bc_row(K, dst, row_ap, n=128, q=None):
    K.dma(dst, row_ap.rearrange("(o f) -> o f", o=1).partition_broadcast(n).rearrange("p o f -> p (o f)"), q=q)


def norm_mod_tile(K, es, xt, hb, a_bc, sh_bc, ss, D, junk):
    K.act(junk, xt, AF.Square, accum_out=ss)
    K.rsqrt(es, ss, ss, 1.0 / D, 1e-6)
    K.stt(junk, xt, ss, a_bc, ALU.mult, ALU.mult)
    K.tt(hb, junk, sh_bc, ALU.add)


def stage_inproj(K, lat, modx, w_in, P, PG, identb):
    cfg = K.cfg
    D = cfg['D']
    KC = D // 128
    INW = cfg['INW']
    nct = cfg['CTX'] // 128
    nlt = cfg['SEQ'] // 128
    g0 = cfg['off']['gate'][0]
    groups = [(0, nct, 1)]
    t = nct
    while t < nct + nlt:
        n = min(8, nct + nlt - t)
        groups.append((t, n, 0))
        t += n
    blocks = []
    c = 0
    while c < INW:
        lim = g0 if c < g0 else INW
        n = min(512, lim - c)
        blocks.append((c, n, c >= g0))
        c += n
    with contextlib.ExitStack() as es:
        xt = [K.sb(es, f"xt{i}", [128, D]) for i in range(2)]
        junk = K.sb(es, "junk", [128, D])
        hb = K.sb(es, "hb", [128, D], BF16)
        a_bc = K.sb(es, "abc", [128, D])
        sh_bc = K.sb(es, "shbc", [128, D])
        ss = K.sb(es, "ss", [128, 2])
        hT = K.sb(es, "hT", [128, KC, 8 * 128], BF16)
        wt = [K.sb(es, f"wt{i}", [128, KC, 512], BF16) for i in range(2)]
        ev = [K.sb(es, f"ev{i}", [128, 512]) for i in range(3)]
        pt = [K.ps(es, f"pt{i}", [128, 8, 128], BF16) for i in range(2)]
        acc = [K.ps(es, f"acc{i}", [128, 512]) for i in range(3)]
        it = 0
        wi = 0
        for (t0, n, mi) in groups:
            bc_row(K, a_bc[:], modx[mi, 0, :], q='sp')
            bc_row(K, sh_bc[:], modx[mi, 1, :], q='sp')
            for j in range(n):
                x = xt[j % 2]
                K.dma(x[:], lat[(t0 + j) * 128:(t0 + j + 1) * 128, :])
                norm_mod_tile(K, es, x[:], hb[:], a_bc[:], sh_bc[:], ss[:, 0:1], D, junk[:])
                for k0 in range(0, KC, 8):
                    p = pt[(k0 // 8) % 2]
                    kn = min(8, KC - k0)
                    for k in range(kn):
                        K.tr(p[:, k, :], hb[:, (k0 + k) * 128:(k0 + k + 1) * 128], identb[:])
                    K.cp(hT[:, k0:k0 + kn, j * 128:(j + 1) * 128], p[:, 0:kn, :], eng=('dve' if (k0 // 8) % 2 else 'act'))
            for (c0, ncol, isg) in blocks:
                w = wt[wi % 2]
                wi += 1
                dma_w(K, w[:, :, 0:ncol], w_in[:, c0:c0 + ncol], q='pool')
                for j in range(n):
                    a = acc[it % 3]
                    e = ev[it % 3]
                    for k in range(KC):
                        K.mm(a[:, 0:ncol], hT[:, k, j * 128:(j + 1) * 128], w[:, k, 0:ncol], start=(k == 0), stop=(k == KC - 1))
                    if isg:
                        K.act(e[:, 0:ncol], a[:, 0:ncol], AF.Sigmoid)
                    elif it % 2 == 0:
                        K.cp(e[:, 0:ncol], a[:, 0:ncol], eng='act')
                    else:
                        K.cp(e[:, 0:ncol], a[:, 0:ncol], eng='dve')
                    if isg:
                        K.dma(PG[(t0 + j) * 128:(t0 + j + 1) * 128, c0 - g0:c0 - g0 + ncol], e[:, 0:ncol], q='sp')
                    else:
                        K.dma(P[(t0 + j) * 128:(t0 + j + 1) * 128, c0:c0 + ncol], e[:, 0:ncol], q='sp')
                    it += 1
        K.S.barrier()


def load_rows(K, dst, arr, c0, ncols, base, seglen, s0, n, perm=None, q=None):
    lo = max(s0, 0)
    hi = min(s0 + n, seglen)
    if hi <= lo:
        return
    if perm is None:
        K.dma(dst[lo - s0:hi - s0, 0:ncols], arr[base + lo:base + hi, c0:c0 + ncols], q=q)
        return
    A, B = perm
    i = lo
    while i < hi:
        a, b = divmod(i, B)
        m = min(B - b, hi - i)
        r0 = base + b * A + a
        K.dma(dst[i - s0:i - s0 + m, 0:ncols], arr[r0:r0 + (m - 1) * A + 1:A, c0:c0 + ncols], q=q)
        i += m


def bc3(ap, axis, shape):
    return ap.unsqueeze(axis).to_broadcast(list(shape))


def softplus_(K, x, bias_bc):
    K.tt(x, x, bias_bc, ALU.add)
    K.act(x, x, AF.Exp)
    K.act(x, x, AF.Ln, bias=1.0)


def stage_mamba_prep(K, P, conv_w, conv_b, A_log, dt_bias, mxbc, mdt, mdA):
    cfg = K.cfg
    MW, MH = cfg['MW'], cfg['MH']
    NCH = MW + 512
    CTX, SEQ = cfg['CTX'], cfg['SEQ']
    rows = SEQ // 64
    ox = cfg['off']['m_x'][0]
    odt = cfg['off']['m_dt'][0]
    with contextlib.ExitStack() as es:
        wk = K.sb(es, "mcw", [128, 5, NCH])
        for k in range(5):
            bc_row(K, wk[:, k, :], conv_w[k, :])
        cb = K.sb(es, "mcb", [128, NCH])
        bc_row(K, cb[:], conv_b)
        aneg = K.sb(es, "aneg", [128, 2 * MH])
        bc_row(K, aneg[:], A_log.rearrange("a b -> (a b)"))
        K.act(aneg[:], aneg[:], AF.Exp)
        dtb = K.sb(es, "dtb", [128, 2 * MH])
        bc_row(K, dtb[:], dt_bias.rearrange("a b -> (a b)"))
        xs = [K.sb(es, f"mx{k}", [128, NCH]) for k in range(5)]
        acc = K.sb(es, "macc", [128, NCH])
        dt = K.sb(es, "mdt", [128, 2 * MH])
        dA = K.sb(es, "mdA", [128, 2 * MH])
        for seg, (base, seglen, perm) in enumerate([(0, CTX, None), (CTX, SEQ, (64, rows))]):
            for s0 in range(0, seglen, 128):
                for k in range(5):
                    edge = (s0 + k - 2 < 0) or (s0 + k - 2 + 128 > seglen)
                    if edge:
                        K.memset(xs[k][:], 0.0, eng='pool')
                    load_rows(K, xs[k][:], P, ox, NCH, base, seglen, s0 + k - 2, 128, perm)
                K.tt(acc[:], xs[0][:], wk[:, 0, :], ALU.mult)
                for k in range(1, 5):
                    K.tt(xs[k][:], xs[k][:], wk[:, k, :], ALU.mult, eng='pool')
                    K.tt(acc[:], acc[:], xs[k][:], ALU.add)
                K.tt(acc[:], acc[:], cb[:], ALU.add)
                K.act(acc[:], acc[:], AF.Silu)
                K.dma(mxbc[base + s0:base + s0 + 128, :], acc[:], q='sp')
                load_rows(K, dt[:], P, odt, 2 * MH, base, seglen, s0, 128, perm)
                softplus_(K, dt[:], dtb[:])
                K.stt(dA[:], dt[:], -1.0, aneg[:], ALU.mult, ALU.mult)
                K.dma(mdt[base + s0:base + s0 + 128, :], dt[:], q='sp')
                K.dma(mdA[base + s0:base + s0 + 128, :], dA[:], q='sp')
        K.S.barrier()


def stage_mamba_scan(K, cst, mxbc, mdt, mdA, ymd):
    cfg = K.cfg
    MW, MH = cfg['MW'], cfg['MH']
    HG = MH // 2
    CTX, SEQ, T = cfg['CTX'], cfg['SEQ'], cfg['T']
    nchunk = T // 64
    ident, ones = cst[:, 0, :], cst[:, 1, :]
    HB = min(8, MH)
    with contextlib.ExitStack() as es:
        H = K.sb(es, "mH", [128, MH, 64])
        X = [K.sb(es, f"mX{i}", [64, MW + 512]) for i in range(2)]
        dtt = [K.sb(es, f"mdtt{i}", [64, 2 * MH]) for i in range(2)]
        dAt = [K.sb(es, f"mdAt{i}", [64, 2 * MH]) for i in range(2)]
        BCT = K.sb(es, "mBCT", [128, 4, 64])
        Z = K.sb(es, "mZ", [64, MH, 64])
        G = K.sb(es, "mG", [64, MH])
        eG = K.sb(es, "meG", [64, MH])
        eE = K.sb(es, "meE", [64, MH])
        gC = K.sb(es, "mgC", [128, MH])
        Dm = K.sb(es, "mD", [64, MH, 64])
        sc = K.sb(es, "msc", [64, 2, 64])
        xdt = K.sb(es, "mxdt", [64, MH, 64])
        xe = K.sb(es, "mxe", [64, MH, 64])
        Y = K.sb(es, "mY", [64, MH, 64])
        p_t = K.ps(es, "mp_t", [128, 4, 64])[:]
        p_g = K.ps(es, "mp_g", [128, 4, MH])[:]
        p_sc = K.ps(es, "mp_sc", [64, 2, 64])[:]
        p_gb = K.ps(es, "mp_gb", [64, HB, 64])
        p_y = K.ps(es, "mp_y", [64, HB, 64])
        p_c = K.ps(es, "mp_c", [64, HB, 64])
        p_h = K.ps(es, "mp_h", [128, HB, 64])
        for d in range(2):
            tri = cst[0:64, 2 + d, 0:64]
            K.memset(H[:], 0.0)
            order = list(range(nchunk))
            if d == 1:
                order = list(range(CTX // 64 - 1, -1, -1)) + list(range(nchunk - 1, CTX // 64 - 1, -1))
            for ci, c in enumerate(order):
                x = X[ci % 2]
                dt_ = dtt[ci % 2]
                dA_ = dAt[ci % 2]
                K.dma(x[:], mxbc[c * 64:(c + 1) * 64, :])
                K.dma(dt_[:], mdt[c * 64:(c + 1) * 64, :])
                K.dma(dA_[:], mdA[c * 64:(c + 1) * 64, :])
                dAd = dA_[:, d * MH:(d + 1) * MH]
                dtd = dt_[:, d * MH:(d + 1) * MH]
                for j in range(4):
                    K.tr(p_t[:, j, :], x[:, MW + j * 128:MW + (j + 1) * 128], ident[0:64, 0:64])
                K.cp(BCT[:], p_t, eng='act')
                K.mm(p_g[0:64, 0, :], tri, dAd)
                K.mm(p_g[0:64, 1, :], ones[0:64, 0:64], dAd)
                K.mm(p_g[:, 2, :], ones[0:64, :], dAd)
                K.cp(G[:], p_g[0:64, 0, :])
                K.act(eG[:], p_g[0:64, 0, :], AF.Exp)
                K.tt(eE[:], p_g[0:64, 1, :], G[:], ALU.subtract)
                K.act(eE[:], eE[:], AF.Exp)
                K.act(gC[:], p_g[:, 2, :], AF.Exp)
                K.tt(Z[:], bc3(dAd, 2, [64, MH, 64]), bc3(tri, 1, [64, MH, 64]), ALU.mult, eng=K.pool_eng)
                for g in range(2):
                    K.mm(p_sc[:, g, :], BCT[:, g, :], BCT[:, 2 + g, :])
                K.tt(sc[:], p_sc, bc3(tri, 1, [64, 2, 64]), ALU.mult)
                xs3 = x[:, 0:MW].rearrange("p (h e) -> p h e", e=64)
                K.tt(xdt[:], xs3, bc3(dtd, 2, [64, MH, 64]), ALU.mult, eng=K.pool_eng)
                K.tt(xe[:], xdt[:], bc3(eE[:], 2, [64, MH, 64]), ALU.mult, eng=K.pool_eng)
                for h0 in range(0, MH, HB):
                    hs = slice(h0, h0 + HB)
                    K.mm(p_gb[:], ones[0:64, 0:64], Z[:, hs, :])
                    K.tt(Dm[:, hs, :], p_gb[:], bc3(G[:, hs], 2, [64, HB, 64]), ALU.subtract)
                    K.ts(Dm[:, hs, :], Dm[:, hs, :], 0.0, ALU.min)
                    K.act(Dm[:, hs, :], Dm[:, hs, :], AF.Exp)
                    for g in range(2):
                        ga, gb_ = max(h0, g * HG), min(h0 + HB, (g + 1) * HG)
                        if gb_ > ga:
                            K.tt(Dm[:, ga:gb_, :], Dm[:, ga:gb_, :], bc3(sc[:, g, :], 1, [64, gb_ - ga, 64]), ALU.mult)
                    for h in range(h0, h0 + HB):
                        g = h // HG
                        K.mm(p_y[:, h - h0, :], Dm[:, h, :], xdt[:, h, :])
                        K.mm(p_c[:, h - h0, :], BCT[:, 2 + g, :], H[:, h, :])
                    for h in range(h0, h0 + HB):
                        g = h // HG
                        K.mm(p_h[:, h - h0, :], x[:, MW + g * 128:MW + (g + 1) * 128], xe[:, h, :])
                    K.cp(Y[:, hs, :], p_y[:], eng='act')
                    K.tt(Z[:, hs, :], p_c[:], bc3(eG[:, hs], 2, [64, HB, 64]), ALU.mult)
                    K.tt(Y[:, hs, :], Y[:, hs, :], Z[:, hs, :], ALU.add)
                    K.tt(H[:, hs, :], H[:, hs, :], bc3(gC[:, hs], 2, [128, HB, 64]), ALU.mult)
                    K.tt(H[:, hs, :], H[:, hs, :], p_h[:], ALU.add)
                K.dma(ymd[d][c * 64:(c + 1) * 64, :], Y[:].rearrange("p h e -> p (h e)"), q='sp')
        K.S.barrier()


def stage_mamba_finish(K, P, mxbc, ymd, D_skip, norm_g, ymf):
    cfg = K.cfg
    MW, MH = cfg['MW'], cfg['MH']
    CTX, SEQ = cfg['CTX'], cfg['SEQ']
    rows = SEQ // 64
    oz = cfg['off']['m_z'][0]
    GWD = MW // 2
    with contextlib.ExitStack() as es:
        dsk = K.sb(es, "dsk", [128, MH])
        bc_row(K, dsk[:], D_skip)
        ng = K.sb(es, "mng", [128, MW])
        bc_row(K, ng[:], norm_g)
        y0 = [K.sb(es, f"fy0{i}", [128, MW]) for i in range(2)]
        y1 = [K.sb(es, f"fy1{i}", [128, MW]) for i in range(2)]
        xs = [K.sb(es, f"fxs{i}", [128, MW]) for i in range(2)]
        z = [K.sb(es, f"fz{i}", [128, MW]) for i in range(2)]
        junk = K.sb(es, "fjunk", [128, GWD])
        ss = K.sb(es, "fss", [128, 2])
        i = 0
        for (base, seglen, perm) in [(0, CTX, None), (CTX, SEQ, (rows, 64))]:
            for s0 in range(0, seglen, 128):
                a, b, c, zz = y0[i % 2], y1[i % 2], xs[i % 2], z[i % 2]
                i += 1
                load_rows(K, a[:], ymd[0], 0, MW, base, seglen, s0, 128, perm)
                load_rows(K, b[:], ymd[1], 0, MW, base, seglen, s0, 128, perm)
                load_rows(K, c[:], mxbc, 0, MW, base, seglen, s0, 128, perm)
                K.dma(zz[:], P[base + s0:base + s0 + 128, oz:oz + MW])
                K.tt(a[:], a[:], b[:], ALU.add)
                c3 = c[:].rearrange("p (h e) -> p h e", e=64)
                K.tt(c3, c3, bc3(dsk[:], 2, [128, MH, 64]), ALU.mult, eng='pool')
                K.tt(a[:], a[:], c[:], ALU.add)
                K.act(zz[:], zz[:], AF.Silu)
                K.tt(a[:], a[:], zz[:], ALU.mult)
                for g in range(2):
                    K.act(junk[:], a[:, g * GWD:(g + 1) * GWD], AF.Square, accum_out=ss[:, g:g + 1])
                K.rsqrt(es, ss[:], ss[:], 1.0 / GWD, 1e-6)
                for g in range(2):
                    K.stt(a[:, g * GWD:(g + 1) * GWD], a[:, g * GWD:(g + 1) * GWD], ss[:, g:g + 1],
                          ng[:, g * GWD:(g + 1) * GWD], ALU.mult, ALU.mult)
                K.dma(ymf[base + s0:base + s0 + 128, :], a[:], q='sp')
        K.S.barrier()


def to_featmajor(K, src_bf, dstT, j, KCn, pt, identb):
    for k0 in range(0, KCn, 8):
        p = pt[(k0 // 8) % 2]
        kn = min(8, KCn - k0)
        for k in range(kn):
            K.tr(p[:, k, :], src_bf[:, (k0 + k) * 128:(k0 + k + 1) * 128], identb[:])
        K.cp(dstT[:, k0:k0 + kn, j * 128:(j + 1) * 128], p[:, 0:kn, :], eng=('dve' if (k0 // 8) % 2 else 'act'))


def token_groups(cfg, gs):
    nct = cfg['CTX'] // 128
    nlt = cfg['SEQ'] // 128
    groups = []
    t = 0
    while t < nct:
        n = min(gs, nct - t)
        groups.append((t, n, 1))
        t += n
    while t < nct + nlt:
        n = min(gs, nct + nlt - t)
        groups.append((t, n, 0))
        t += n
    return groups


def stage_merge(K, PG, ybr, w_br, w_out, modx, lat, identb, t_lo=0):
    cfg = K.cfg
    D = cfg['D']
    KC = D // 128
    BW = cfg['MW']
    KB_ = BW // 128
    g0 = cfg['off']['gate'][0]
    GS = 4
    NCB = D // 512
    with contextlib.ExitStack() as es:
        yt = [K.sb(es, f"gy{i}", [128, BW]) for i in range(2)]
        yb = K.sb(es, "gyb", [128, BW], BF16)
        yT = K.sb(es, "gyT", [128, KB_, GS * 128], BF16)
        wb = [K.sb(es, f"gwb{i}", [128, KB_, 512], BF16) for i in range(2)]
        mg = K.sb(es, "gmg", [128, GS, D])
        gt = [K.sb(es, f"ggt{i}", [128, 512]) for i in range(2)]
        tmp = K.sb(es, "gtmp", [128, 512])
        mb = K.sb(es, "gmb", [128, D], BF16)
        mT = K.sb(es, "gmT", [128, KC, GS * 128], BF16)
        wo = [K.sb(es, f"gwo{i}", [128, KC, 512], BF16) for i in range(2)]
        g1 = K.sb(es, "gg1", [128, D])
        lt = [K.sb(es, f"glt{i}", [128, 512]) for i in range(2)]
        pt = [K.ps(es, f"gpt{i}", [128, 8, 128], BF16) for i in range(2)]
        acc = [K.ps(es, f"gacc{i}", [128, 512]) for i in range(3)]
        it = 0
        wi = 0
        for (t0, n, mi) in token_groups(cfg, GS):
            if t0 < t_lo:
                continue
            bc_row(K, g1[:], modx[mi, 2, :], q='sp')
            for br in range(3):
                for j in range(n):
                    y = yt[j % 2]
                    K.dma(y[:], ybr[br][(t0 + j) * 128:(t0 + j + 1) * 128, :])
                    K.cp(yb[:], y[:], eng='pool')
                    to_featmajor(K, yb, yT, j, KB_, pt, identb)
                for cb in range(NCB):
                    w = wb[wi % 2]
                    wi += 1
                    dma_w(K, w[:], w_br[br][:, cb * 512:(cb + 1) * 512], q='pool')
                    for j in range(n):
                        a = acc[it % 3]
                        g = gt[it % 2]
                        it += 1
                        K.dma(g[:], PG[(t0 + j) * 128:(t0 + j + 1) * 128, br * D + cb * 512:br * D + (cb + 1) * 512])
                        for k in range(KB_):
                            K.mm(a[:], yT[:, k, j * 128:(j + 1) * 128], w[:, k, :], start=(k == 0), stop=(k == KB_ - 1))
                        if br == 0:
                            K.tt(mg[:, j, cb * 512:(cb + 1) * 512], a[:], g[:], ALU.mult)
                        else:
                            K.tt(tmp[:], a[:], g[:], ALU.mult)
                            K.tt(mg[:, j, cb * 512:(cb + 1) * 512], mg[:, j, cb * 512:(cb + 1) * 512], tmp[:], ALU.add, eng='pool')
            for j in range(n):
                K.cp(mb[:], mg[:, j, :], eng='act')
                to_featmajor(K, mb, mT, j, KC, pt, identb)
            for cb in range(NCB):
                w = wo[wi % 2]
                wi += 1
                dma_w(K, w[:], w_out[:, cb * 512:(cb + 1) * 512], q='pool')
                for j in range(n):
                    a = acc[it % 3]
                    l_ = lt[it % 2]
                    it += 1
                    rs = slice((t0 + j) * 128, (t0 + j + 1) * 128)
                    K.dma(l_[:], lat[rs, cb * 512:(cb + 1) * 512])
                    for k in range(KC):
                        K.mm(a[:], mT[:, k, j * 128:(j + 1) * 128], w[:, k, :], start=(k == 0), stop=(k == KC - 1))
                    K.tt(tmp[:], a[:], g1[:, cb * 512:(cb + 1) * 512], ALU.mult)
                    K.tt(l_[:], l_[:], tmp[:], ALU.add)
                    K.dma(lat[rs, cb * 512:(cb + 1) * 512], l_[:], q='sp')
        K.S.barrier()


def stage_moe(K, lat, modx, grp_w, grp_b, exp_w, exp_b, w1, w3, w2, ident, identb, t_lo=0):
    cfg = K.cfg
    D = cfg['D']
    KC = D // 128
    FF = cfg['FF']
    FC = FF // 128
    NE = cfg['NE']
    GS = 4
    NCB = D // 512
    NR = 4 + NE
    with contextlib.ExitStack() as es:
        xt0 = K.sb(es, "ex0", [128, D])
        xt = [xt0, xt0]
        h = K.sb(es, "eh", [128, D])
        hb = K.sb(es, "ehb", [128, D], BF16)
        a_bc = K.sb(es, "eabc", [128, D])
        sh_bc = K.sb(es, "eshbc", [128, D])
        g2 = K.sb(es, "eg2", [128, D])
        ss = K.sb(es, "ess", [128, 2])
        hT = K.sb(es, "ehT", [128, KC, GS * 128], BF16)
        hTf = K.sb(es, "ehTf", [128, KC, 128])
        rw = K.sb(es, "erw", [128, KC, NR])
        rb = K.sb(es, "erb", [128, NR])
        K.dma(rw[:, :, 0:4], grp_w.rearrange("(k p) n -> p k n", p=128), q='sp', allow_slow_non_contiguous=True)
        K.dma(rw[:, :, 4:NR], exp_w.rearrange("(k p) n -> p k n", p=128), q='sp', allow_slow_non_contiguous=True)
        bc_row(K, rb[:, 0:4], grp_b, q='sp')
        bc_row(K, rb[:, 4:NR], exp_b, q='sp')
        lg = K.sb(es, "elg", [128, NR])
        sm = K.sb(es, "esm", [128, 16])
        oh = K.sb(es, "eoh", [128, 4])
        ig = K.sb(es, "eig", [128, 8])
        i2 = K.sb(es, "ei2", [128, 8])
        m1 = K.sb(es, "em1", [128, 8])
        m2 = K.sb(es, "em2", [128, 8])
        comb = K.sb(es, "ecomb", [128, GS, NE])
        wa = [K.sb(es, "ewa0", [128, KC, FF], BF16)] * 2
        wc = [K.sb(es, "ewc0", [128, KC, FF], BF16)] * 2
        wd = [K.sb(es, "ewd0", [128, FC, D], BF16)] * 2
        sl = K.sb(es, "esl", [128, 512])
        hidT = K.sb(es, "ehidT", [128, FC, GS * 128], BF16)
        yacc = K.sb(es, "eyacc", [128, GS, D])
        junk = yacc[:, 0, :]
        pt0 = K.ps(es, "ept0", [128, 8, 128], BF16)
        pt = [pt0, pt0]
        ptf = K.ps(es, "eptf", [128, 4, 128])
        pr = K.ps(es, "epr", [128, NR])
        p1 = K.ps(es, "ep1", [128, 512])
        p3 = K.ps(es, "ep3", [128, 512])
        py0 = K.ps(es, "epy0", [128, 512])
        py = [py0, py0]
        wi = 0
        it = 0
        for (t0, n, mi) in token_groups(cfg, GS):
            if t0 < t_lo:
                continue
            bc_row(K, a_bc[:], modx[mi, 3, :], q='sp')
            bc_row(K, sh_bc[:], modx[mi, 4, :], q='sp')
            bc_row(K, g2[:], modx[mi, 5, :], q='sp')
            for j in range(n):
                x = xt[j % 2]
                K.dma(x[:], lat[(t0 + j) * 128:(t0 + j + 1) * 128, :])
                norm_mod_tile(K, es, x[:], h[:], a_bc[:], sh_bc[:], ss[:, 0:1], D, junk)
                K.cp(hb[:], h[:], eng='pool')
                to_featmajor(K, hb, hT, j, KC, pt, identb)
                for k0 in range(0, KC, 4):
                    for k in range(4):
                        K.tr(ptf[:, k, :], h[:, (k0 + k) * 128:(k0 + k + 1) * 128], ident[:])
                    K.cp(hTf[:, k0:k0 + 4, :], ptf[:], eng='act')
                for k in range(KC):
                    K.mm(pr[:], hTf[:, k, :], rw[:, k, :], start=(k == 0), stop=(k == KC - 1))
                K.tt(lg[:], pr[:], rb[:], ALU.add)
                K.red(sm[:, 0:1], lg[:, 0:4], ALU.max)
                K.ts(oh[:], lg[:, 0:4], sm[:, 0:1], ALU.is_equal)
                K.ts(sm[:, 4:8], lg[:, 0:4], sm[:, 0:1], ALU.subtract)
                K.act(sm[:, 4:8], sm[:, 4:8], AF.Exp)
                K.red(sm[:, 1:2], sm[:, 4:8], ALU.add)
                K.recip(sm[:, 1:2], sm[:, 1:2])
                K.ts(ig[:], lg[:, 4:12], oh[:, 0:1], ALU.mult)
                for g in range(1, 4):
                    K.stt(ig[:], lg[:, 4 + 8 * g:12 + 8 * g], oh[:, g:g + 1], ig[:], ALU.mult, ALU.add)
                K.red(sm[:, 2:3], ig[:], ALU.max)
                K.ts(m1[:], ig[:], sm[:, 2:3], ALU.is_equal)
                K.stt(i2[:], m1[:], -1e30, ig[:], ALU.mult, ALU.add)
                K.red(sm[:, 3:4], i2[:], ALU.max)
                K.ts(m2[:], i2[:], sm[:, 3:4], ALU.is_equal)
                K.tt(sm[:, 8:9], sm[:, 3:4], sm[:, 2:3], ALU.subtract)
                K.act(sm[:, 8:9], sm[:, 8:9], AF.Exp)
                K.ts(sm[:, 9:10], sm[:, 8:9], 1.0, ALU.add)
                K.recip(sm[:, 9:10], sm[:, 9:10])
                K.tt(sm[:, 9:10], sm[:, 9:10], sm[:, 1:2], ALU.mult)
                K.tt(sm[:, 10:11], sm[:, 9:10], sm[:, 8:9], ALU.mult)
                K.ts(m1[:], m1[:], sm[:, 9:10], ALU.mult)
                K.stt(m1[:], m2[:], sm[:, 10:11], m1[:], ALU.mult, ALU.add)
                for g in range(4):
                    K.ts(comb[:, j, g * 8:(g + 1) * 8], m1[:], oh[:, g:g + 1], ALU.mult)
            K.memset(yacc[:], 0.0, eng='pool')
            ntok = n * 128
            for e in range(NE):
                a_, c_, d_ = wa[wi % 2], wc[wi % 2], wd[wi % 2]
                wi += 1
                dma_w(K, a_[:], w1[e], q='pool')
                dma_w(K, c_[:], w3[e], q='pool')
                for cb in range(NCB):
                    K.dma(d_[:, :, cb * 512:(cb + 1) * 512], w2[e][:, cb * 512:(cb + 1) * 512].rearrange("(k p) n -> p k n", p=128), q='pool')
                for fc in range(FC):
                    for tb in range(0, ntok, 512):
                        tn = min(512, ntok - tb)
                        for k in range(KC):
                            K.mm(p1[:, 0:tn], a_[:, k, fc * 128:(fc + 1) * 128], hT[:, k, tb:tb + tn], start=(k == 0), stop=(k == KC - 1))
                        for k in range(KC):
                            K.mm(p3[:, 0:tn], c_[:, k, fc * 128:(fc + 1) * 128], hT[:, k, tb:tb + tn], start=(k == 0), stop=(k == KC - 1))
                        K.act(sl[:, 0:tn], p1[:, 0:tn], AF.Silu)
                        K.tt(hidT[:, fc, tb:tb + tn], sl[:, 0:tn], p3[:, 0:tn], ALU.mult)
                for j in range(n):
                    for cb in range(NCB):
                        p = py[it % 2]
                        it += 1
                        for fc in range(FC):
                            K.mm(p[:], hidT[:, fc, j * 128:(j + 1) * 128], d_[:, fc, cb * 512:(cb + 1) * 512], start=(fc == 0), stop=(fc == FC - 1))
                        ysl = yacc[:, j, cb * 512:(cb + 1) * 512]
                        K.stt(ysl, p[:], comb[:, j, e:e + 1], ysl, ALU.mult, ALU.add)
            for j in range(n):
                x = xt[j % 2]
                rs = slice((t0 + j) * 128, (t0 + j + 1) * 128)
                K.dma(x[:], lat[rs, :])
                K.tt(yacc[:, j, :], yacc[:, j, :], g2[:], ALU.mult)
                K.tt(x[:], x[:], yacc[:, j, :], ALU.add)
                K.dma(lat[rs, :], x[:], q='sp')
        K.S.barrier()


def stage_final(K, lat, fg, out):
    cfg = K.cfg
    D = cfg['D']
    nct = cfg['CTX'] // 128
    nlt = cfg['SEQ'] // 128
    with contextlib.ExitStack() as es:
        g = K.sb(es, "fg", [128, D])
        bc_row(K, g[:], fg, q='sp')
        xt = [K.sb(es, f"fx{i}", [128, D]) for i in range(2)]
        junk = K.sb(es, "fjk", [128, D])
        ss = K.sb(es, "fss2", [128, 1])
        for j in range(nlt):
            x = xt[j % 2]
            K.dma(x[:], lat[(nct + j) * 128:(nct + j + 1) * 128, :])
            K.act(junk[:], x[:], AF.Square, accum_out=ss[:])
            K.rsqrt(es, ss[:], ss[:], 1.0 / D, 1e-6)
            K.stt(x[:], x[:], ss[:], g[:], ALU.mult, ALU.mult)
            K.dma(out[j * 128:(j + 1) * 128, :], x[:], q='sp')
        K.S.barrier()


PARAMS = [("ada_w", lambda c: [c['DEPTH'], c['D'], 6 * c['D']]), ("ada_b", lambda c: [c['DEPTH'], 6 * c['D']]),
          ("norm1_g", lambda c: [c['DEPTH'], c['D']]), ("norm2_g", lambda c: [c['DEPTH'], c['D']]),
          ("w_in", lambda c: [c['DEPTH'], c['D'], c['INW']]),
          ("m_conv_w", lambda c: [c['DEPTH'], 5, c['MW'] + 512]), ("m_conv_b", lambda c: [c['DEPTH'], c['MW'] + 512]),
          ("m_A_log", lambda c: [c['DEPTH'], 2, c['MH']]), ("m_dt_bias", lambda c: [c['DEPTH'], 2, c['MH']]),
          ("m_D", lambda c: [c['DEPTH'], c['MH']]), ("m_norm_g", lambda c: [c['DEPTH'], c['MW']]),
          ("r_shift_mu", lambda c: [c['DEPTH'], 2, 3 * c['RW']]), ("r_w0", lambda c: [c['DEPTH'], 2, c['RW']]),
          ("r_w_up", lambda c: [c['DEPTH'], 2, 64, c['RW']]), ("r_a0", lambda c: [c['DEPTH'], c['RW']]),
          ("r_a_up", lambda c: [c['DEPTH'], 64, c['RW']]), ("r_g_up", lambda c: [c['DEPTH'], 128, c['RW']]),
          ("r_k_k", lambda c: [c['DEPTH'], c['RW']]), ("r_k_a", lambda c: [c['DEPTH'], c['RW']]),
          ("r_r_k", lambda c: [c['DEPTH'], c['RH'], 64]), ("r_ln_g", lambda c: [c['DEPTH'], c['RW']]),
          ("r_ln_b", lambda c: [c['DEPTH'], c['RW']]),
          ("g_conv_w", lambda c: [c['DEPTH'], 5, 3 * c['GW']]), ("g_A_log", lambda c: [c['DEPTH'], 2, c['GH']]),
          ("g_dt_bias", lambda c: [c['DEPTH'], 2, c['GH']]), ("g_norm_g", lambda c: [c['DEPTH'], 128]),
          ("w_br_m", lambda c: [c['DEPTH'], c['MW'], c['D']]), ("w_br_r", lambda c: [c['DEPTH'], c['RW'], c['D']]),
          ("w_br_g", lambda c: [c['DEPTH'], c['GW'], c['D']]), ("w_out", lambda c: [c['DEPTH'], c['D'], c['D']]),
          ("moe_grp_w", lambda c: [c['DEPTH'], c['D'], 4]), ("moe_grp_b", lambda c: [c['DEPTH'], 4]),
          ("moe_exp_w", lambda c: [c['DEPTH'], c['D'], c['NE']]), ("moe_exp_b", lambda c: [c['DEPTH'], c['NE']]),
          ("moe_w1", lambda c: [c['DEPTH'], c['NE'], c['D'], c['FF']]), ("moe_w3", lambda c: [c['DEPTH'], c['NE'], c['D'], c['FF']]),
          ("moe_w2", lambda c: [c['DEPTH'], c['NE'], c['FF'], c['D']]), ("final_norm_g", lambda c: [c['D']])]


def make_cst():
    c = np.zeros((128, 8, 128), np.float32)
    i = np.arange(128)
    c[:, 0, :] = np.eye(128)
    c[:, 1, :] = 1.0
    s = (i % 64)[:, None]
    t = (i % 64)[None, :]
    c[:, 2, :] = (s <= t)
    c[:, 3, :] = (s >= t)
    c[:, 4, :] = (s < t)
    c[:, 5, :] = (s > t)
    return c


def build_program(cfg, stages=None):
    nc = bass.Bass("TRN2", target_bir_lowering=False)
    D, T, SEQ, CTX, INW, MW, MH = (cfg[k] for k in ("D", "T", "SEQ", "CTX", "INW", "MW", "MH"))
    es = contextlib.ExitStack()
    with es:
        K = KB(nc, cfg, es)
        import os
        if os.environ.get('KM_SER'):
            K.S.ser_engs = tuple(os.environ['KM_SER'].split(','))
        x = K.dram("x", [SEQ, D], kind="ExternalInput")
        ctx = K.dram("ctx", [CTX, D], kind="ExternalInput")
        c_b = K.dram("c", [D], kind="ExternalInput")
        c_ctx = K.dram("c_ctx", [D], kind="ExternalInput")
        cstd = K.dram("cst", [128, 8, 128], kind="ExternalInput")
        W = {n: K.dram(n, f(cfg), kind="ExternalInput") for n, f in PARAMS}
        out = K.dram("out", [SEQ, D], kind="ExternalOutput")
        lat = K.dram("lat", [T, D])
        P = K.dram("P", [T, cfg['off']['gate'][0]])
        PG = K.dram("PG", [T, 3 * D])
        modx = K.dram("modx", [2, 6, D])
        mxbc = K.dram("mxbc", [T, MW + 512])
        mdt = K.dram("mdt", [T, 2 * MH])
        mdA = K.dram("mdA", [T, 2 * MH])
        ymd = [K.dram(f"ymd{d}", [T, MW]) for d in range(2)]
        ybr = [K.dram(f"ybr{i}", [T, MW]) for i in range(3)]
        cst = K.sb(es, "cst", [128, 8, 128])
        identb = K.sb(es, "identb", [128, 128], BF16)
        K.dma(cst[:], cstd, q='sp')
        K.cp(identb[:], cst[:, 0, :])
        ident = cst[:, 0, :]
        for r0 in range(0, CTX, 128):
            K.dma(lat[r0:r0 + 128, :], ctx[r0:r0 + 128, :])
        for r0 in range(0, SEQ, 128):
            K.dma(lat[CTX + r0:CTX + r0 + 128, :], x[r0:r0 + 128, :])
        GW, GH, RW, RH = cfg['GW'], cfg['GH'], cfg['RW'], cfg['RH']
        gqkv = K.dram("gqkv", [T, 3 * GW])
        glog = K.dram("glog", [T, 2 * GH])
        gbeta = K.dram("gbeta", [T, 2 * GH])
        ygd = [K.dram(f"ygd{d}", [T, GW]) for d in range(2)]
        RA = {n: K.dram("ra_" + n, [T, RW]) for n in ('r', 'k', 'v', 'kk', 'a', 'g')}
        RA['lw'] = K.dram("ra_lw", [T, 2 * RW])
        RA['bon'] = K.dram("ra_bon", [T, RH])
        yrd = [K.dram(f"yrd{d}", [T, RW]) for d in range(2)]
        nct = CTX // 128
        on = (lambda n: True) if stages is None else (lambda n: n in stages)
        for l in range(cfg['DEPTH']):
            last = (l == cfg['DEPTH'] - 1)
            if on('modvec'):
                stage_modvec(K, es, c_b, c_ctx, W['ada_w'][l], W['ada_b'][l], W['norm1_g'][l], W['norm2_g'][l], modx)
            if on('inproj'):
                stage_inproj(K, lat, modx, W['w_in'][l], P, PG, identb)
            if on('mprep'):
                stage_mamba_prep(K, P, W['m_conv_w'][l], W['m_conv_b'][l], W['m_A_log'][l], W['m_dt_bias'][l], mxbc, mdt, mdA)
            if on('mscan'):
                stage_mamba_scan(K, cst, mxbc, mdt, mdA, ymd)
            if on('mfin'):
                stage_mamba_finish(K, P, mxbc, ymd, W['m_D'][l], W['m_norm_g'][l], ybr[0])
            if on('rwkv'):
                stage_rwkv_prep(K, cst, P, W['r_shift_mu'][l], W['r_w0'][l], W['r_w_up'][l], W['r_a0'][l], W['r_a_up'][l],
                                W['r_g_up'][l], W['r_k_k'][l], W['r_k_a'][l], W['r_r_k'][l], RA)
                stage_rwkv_scan(K, cst, RA, yrd)
                stage_rwkv_finish(K, RA, yrd, W['r_ln_g'][l], W['r_ln_b'][l], ybr[1])
            if on('gdn'):
                stage_gdn_prep(K, P, W['g_conv_w'][l], W['g_A_log'][l], W['g_dt_bias'][l], gqkv, glog, gbeta)
                stage_gdn_scan(K, cst, gqkv, glog, gbeta, ygd)
                stage_gdn_finish(K, P, ygd, W['g_norm_g'][l], ybr[2])
            t_lo = nct if last else 0
            if on('merge'):
                stage_merge(K, PG, ybr, [W['w_br_m'][l], W['w_br_r'][l], W['w_br_g'][l]], W['w_out'][l], modx, lat, identb, t_lo=t_lo)
            if on('moe'):
                stage_moe(K, lat, modx, W['moe_grp_w'][l], W['moe_grp_b'][l], W['moe_exp_w'][l], W['moe_exp_b'][l],
                          W['moe_w1'][l], W['moe_w3'][l], W['moe_w2'][l], ident, identb, t_lo=t_lo)
        if on('final'):
            stage_final(K, lat, W['final_norm_g'], out)
        with nc.Block() as block:
            K.S.finalize(block)
    return nc, K


def kernel(**inputs):
    cfg = make_cfg()
    nc, _ = build_program(cfg)
    cst = make_cst()
    f32 = lambda a: np.ascontiguousarray(np.asarray(a, dtype=np.float32))
    shared = {n: f32(inputs[n]) for n, _ in PARAMS}
    shared["c_ctx"] = f32(inputs["c_ctx"])
    shared["cst"] = cst
    in_maps = []
    for b in range(cfg['BATCH']):
        m = dict(shared)
        m["x"] = f32(inputs["x"][b])
        m["ctx"] = f32(inputs["ctx"][b])
        m["c"] = f32(inputs["c"][b])
        in_maps.append(m)
    res = run_bass_kernel_spmd(nc, in_maps, core_ids=list(range(cfg['BATCH'])))
    return np.stack([np.asarray(r["out"], dtype=np.float32) for r in res.results], axis=0)


def conv_tile(K, xs, wk, acc, P, col0, NCH, base, seglen, s0, perm, bias=None):
    for k in range(5):
        edge = (s0 + k - 2 < 0) or (s0 + k - 2 + 128 > seglen)
        if edge:
            K.memset(xs[k][:], 0.0, eng='pool')
        load_rows(K, xs[k][:], P, col0, NCH, base, seglen, s0 + k - 2, 128, perm)
    K.tt(acc[:], xs[0][:], wk[:, 0, :], ALU.mult)
    for k in range(1, 5):
        K.tt(xs[k][:], xs[k][:], wk[:, k, :], ALU.mult, eng='pool')
        K.tt(acc[:], acc[:], xs[k][:], ALU.add)
    if bias is not None:
        K.tt(acc[:], acc[:], bias, ALU.add)
    K.act(acc[:], acc[:], AF.Silu)


def l2norm_heads(K, x3, tmp3, ssq, nh, hd, scale=1.0):
    K.tt(tmp3, x3, x3, ALU.mult)
    K.red(ssq, tmp3, ALU.add)
    K.ts(ssq, ssq, 1e-6, ALU.add)
    K.act(ssq, ssq, AF.Sqrt)
    K.recip(ssq, ssq)
    if scale != 1.0:
        K.ts(ssq, ssq, scale, ALU.mult)
    K.tt(x3, x3, bc3(ssq, 2, [x3.shape[0], nh, hd]), ALU.mult)


def solve_nilpotent(K, Mb, Nb, Pm, pM, pN, pP, ident64, nh):
    K.tt(Pm, Mb[0], bc3(ident64, 1, [64, nh, 64]), ALU.add)
    cur = 0
    for lvl in range(5):
        nxt = 1 - cur
        for h in range(nh):
            K.mm(pN[:, h, :], Mb[cur][:, h, :], Nb[cur][:, h, :])
            if lvl < 4:
                K.mm(pM[:, h, :], Nb[cur][:, h, :], Mb[cur][:, h, :])
        K.cp(Nb[nxt], pN, eng='act')
        if lvl < 4:
            K.cp(Mb[nxt], pM, eng='dve')
        for h in range(nh):
            K.mm(pP[:, h, :], Nb[nxt][:, h, :], Pm[:, h, :])
        K.tt(Pm, Pm, pP, ALU.add)
        cur = nxt


def stage_gdn_prep(K, P, conv_w, A_log, dt_bias, gqkv, glog, gbeta):
    cfg = K.cfg
    GW, GH = cfg['GW'], cfg['GH']
    NCH = 3 * GW
    CTX, SEQ = cfg['CTX'], cfg['SEQ']
    oq = cfg['off']['g_q'][0]
    oa = cfg['off']['g_a'][0]
    ob = cfg['off']['g_b'][0]
    with contextlib.ExitStack() as es:
        wk = K.sb(es, "gcw", [128, 5, NCH])
        for k in range(5):
            bc_row(K, wk[:, k, :], conv_w[k, :])
        aneg = K.sb(es, "ganeg", [128, 2 * GH])
        bc_row(K, aneg[:], A_log.rearrange("a b -> (a b)"))
        K.act(aneg[:], aneg[:], AF.Exp)
        dtb = K.sb(es, "gdtb", [128, 2 * GH])
        bc_row(K, dtb[:], dt_bias.rearrange("a b -> (a b)"))
        xs = [K.sb(es, f"gx{k}", [128, NCH]) for k in range(5)]
        acc = K.sb(es, "gacc", [128, NCH])
        ssq = K.sb(es, "gssq", [128, 2 * GH])
        la = K.sb(es, "gla", [128, 2 * GH])
        bt = K.sb(es, "gbt", [128, 2 * GH])
        for (base, seglen) in [(0, CTX), (CTX, SEQ)]:
            for s0 in range(0, seglen, 128):
                conv_tile(K, xs, wk, acc, P, oq, NCH, base, seglen, s0, None)
                qk3 = acc[:, 0:2 * GW].rearrange("p (h e) -> p h e", e=128)
                tmp3 = xs[0][:, 0:2 * GW].rearrange("p (h e) -> p h e", e=128)
                l2norm_heads(K, qk3, tmp3, ssq[:], 2 * GH, 128)
                K.ts(acc[:, 0:GW], acc[:, 0:GW], 128.0 ** -0.5, ALU.mult)
                rs = slice(base + s0, base + s0 + 128)
                K.dma(gqkv[rs, :], acc[:], q='sp')
                K.dma(la[:], P[rs, oa:oa + 2 * GH])
                softplus_(K, la[:], dtb[:])
                K.stt(la[:], la[:], -1.0, aneg[:], ALU.mult, ALU.mult)
                K.dma(glog[rs, :], la[:], q='sp')
                K.dma(bt[:], P[rs, ob:ob + 2 * GH])
                K.act(bt[:], bt[:], AF.Sigmoid)
                K.dma(gbeta[rs, :], bt[:], q='sp')
        K.S.barrier()


def stage_gdn_scan(K, cst, gqkv, glog, gbeta, ygd):
    cfg = K.cfg
    GW, GH = cfg['GW'], cfg['GH']
    CTX, T = cfg['CTX'], cfg['T']
    nchunk = T // 64
    HN = min(4, GH)
    ident, ones = cst[:, 0, :], cst[:, 1, :]
    i64 = cst[0:64, 0, 0:64]
    with contextlib.ExitStack() as es:
        pb = [K.ps(es, f"dpb{i}", [128, 512]) for i in range(7)]

        def chain(dsel):
            H = K.sb(es, "dH", [128, HN, 128])
            qkv = [K.sb(es, f"dqkv{i}", [64, 3, HN * 128]) for i in range(2)]
            lgt = [K.sb(es, f"dlg{i}", [64, 2 * GH]) for i in range(2)]
            btt = [K.sb(es, f"dbt{i}", [64, 2 * GH]) for i in range(2)]
            kqT = K.sb(es, "dkqT", [128, 2 * HN, 64])
            Z = K.sb(es, "dZ", [64, HN, 64])
            G = K.sb(es, "dG", [64, HN])
            eG = K.sb(es, "deG", [64, HN])
            eE = K.sb(es, "deE", [64, HN])
            gC = K.sb(es, "dgC", [128, HN])
            beG = K.sb(es, "dbeG", [64, HN])
            E = K.sb(es, "dE", [64, HN, 64])
            Dt = K.sb(es, "dDt", [64, HN, 64])
            Dn = K.sb(es, "dDn", [64, HN, 64])
            Mb = [K.sb(es, f"dM{i}", [64, HN, 64]) for i in range(2)]
            Nb = [K.sb(es, f"dN{i}", [64, HN, 64]) for i in range(2)]
            Pm = K.sb(es, "dPm", [64, HN, 64])
            A1 = K.sb(es, "dA1", [64, HN, 64])
            bv = K.sb(es, "dbv", [64, HN, 128])
            bke = K.sb(es, "dbke", [64, HN, 128])
            u = K.sb(es, "du", [64, HN, 128])
            w = K.sb(es, "dw", [64, HN, 128])
            wT = K.sb(es, "dwT", [128, HN, 64])
            vn = K.sb(es, "dvn", [64, HN, 128])
            vE = K.sb(es, "dvE", [64, HN, 128])
            o = K.sb(es, "do", [64, HN, 128])

            def v64(t, n, e):
                return t[0:64, 0:n * e].rearrange("p (a b) -> p a b", b=e)

            def v128(t, n, e):
                return t[:, 0:n * e].rearrange("p (a b) -> p a b", b=e)
            p_g = v128(pb[0], 4, HN)
            p_gb = v64(pb[1], HN, 64)
            p_tr = v128(pb[2], 2 * HN, 64)
            p_gr = v64(pb[3], 2 * HN, 64)
            pM, pN, pP = v64(pb[4], HN, 64), v64(pb[5], HN, 64), v64(pb[6], HN, 64)
            p_u, p_w = v64(pb[1], HN, 128), v64(pb[3], HN, 128)
            p_mt = v64(pb[2], HN, 64)
            p_wt = v128(pb[2], HN, 64)
            p_v, p_a, p_b = v64(pb[4], HN, 128), v64(pb[5], HN, 128), v64(pb[6], HN, 128)
            p_h = v128(pb[1], HN, 128)
            for hg in range(GH // HN):
                hc = slice(hg * HN * 128, (hg + 1) * HN * 128)
                for d in (dsel,):
                    tri = cst[0:64, 2 + d, 0:64]
                    strict_n = cst[0:64, 4 + (1 - d), 0:64]
                    K.memset(H[:], 0.0)
                    order = list(range(nchunk))
                    if d == 1:
                        order = list(range(CTX // 64 - 1, -1, -1)) + list(range(nchunk - 1, CTX // 64 - 1, -1))
                    for ci, c in enumerate(order):
                        x = qkv[ci % 2]
                        lg_, bt_ = lgt[ci % 2], btt[ci % 2]
                        rs = slice(c * 64, (c + 1) * 64)
                        for j in range(3):
                            K.dma(x[:, j, :], gqkv[rs, j * GW + hg * HN * 128:j * GW + (hg + 1) * HN * 128])
                        K.dma(lg_[:], glog[rs, :])
                        K.dma(bt_[:], gbeta[rs, :])
                        q3 = x[:, 0, :].rearrange("p (h e) -> p h e", e=128)
                        k3 = x[:, 1, :].rearrange("p (h e) -> p h e", e=128)
                        v3 = x[:, 2, :].rearrange("p (h e) -> p h e", e=128)
                        lgd = lg_[:, d * GH + hg * HN:d * GH + (hg + 1) * HN]
                        btd = bt_[:, d * GH + hg * HN:d * GH + (hg + 1) * HN]
                        K.mm(p_g[0:64, 0, :], tri, lgd)
                        K.mm(p_g[0:64, 1, :], ones[0:64, 0:64], lgd)
                        K.mm(p_g[:, 2, :], ones[0:64, :], lgd)
                        K.cp(G[:], p_g[0:64, 0, :])
                        K.act(eG[:], p_g[0:64, 0, :], AF.Exp)
                        K.tt(eE[:], p_g[0:64, 1, :], G[:], ALU.subtract)
                        K.act(eE[:], eE[:], AF.Exp)
                        K.act(gC[:], p_g[:, 2, :], AF.Exp)
                        K.tt(Z[:], bc3(lgd, 2, [64, HN, 64]), bc3(tri, 1, [64, HN, 64]), ALU.mult, eng='pool')
                        K.mm(p_gb, ones[0:64, 0:64], Z[:])
                        K.tt(E[:], p_gb, bc3(G[:], 2, [64, HN, 64]), ALU.subtract)
                        K.ts(Dt[:], E[:], 0.0, ALU.min)
                        K.act(Dt[:], Dt[:], AF.Exp)
                        K.ts(Dn[:], E[:], -1.0, ALU.mult, 0.0, ALU.min)
                        K.act(Dn[:], Dn[:], AF.Exp)
                        for h in range(HN):
                            K.tr(p_tr[:, h, :], k3[:, h, :], i64)
                            K.tr(p_tr[:, HN + h, :], q3[:, h, :], i64)
                        K.cp(kqT[:], p_tr, eng='act')
                        for h in range(HN):
                            K.mm(p_gr[:, h, :], kqT[:, h, :], kqT[:, h, :])
                            K.mm(p_gr[:, HN + h, :], kqT[:, h, :], kqT[:, HN + h, :])
                        K.tt(Nb[0][:], p_gr[:, 0:HN, :], Dn[:], ALU.mult)
                        K.tt(Nb[0][:], Nb[0][:], bc3(btd, 2, [64, HN, 64]), ALU.mult)
                        K.stt(Nb[0][:], Nb[0][:], -1.0, bc3(strict_n, 1, [64, HN, 64]), ALU.mult, ALU.mult)
                        K.tt(A1[:], p_gr[:, HN:2 * HN, :], Dt[:], ALU.mult)
                        K.tt(A1[:], A1[:], bc3(tri, 1, [64, HN, 64]), ALU.mult)
                        for h in range(HN):
                            K.tr(p_mt[:, h, :], Nb[0][:, h, :], i64)
                        K.cp(Mb[0][:], p_mt, eng='act')
                        solve_nilpotent(K, [Mb[0][:], Mb[1][:]], [Nb[0][:], Nb[1][:]], Pm[:], pM, pN, pP, i64, HN)
                        K.tt(bv[:], v3, bc3(btd, 2, [64, HN, 128]), ALU.mult, eng='pool')
                        K.tt(beG[:], btd, eG[:], ALU.mult)
                        K.tt(bke[:], k3, bc3(beG[:], 2, [64, HN, 128]), ALU.mult, eng='pool')
                        for h in range(HN):
                            K.mm(p_u[:, h, :], Pm[:, h, :], bv[:, h, :])
                            K.mm(p_w[:, h, :], Pm[:, h, :], bke[:, h, :])
                        K.cp(u[:], p_u, eng='act')
                        K.cp(w[:], p_w, eng='dve')
                        for h in range(HN):
                            K.tr(p_wt[:, h, :], w[:, h, :], i64)
                        K.cp(wT[:], p_wt, eng='act')
                        for h in range(HN):
                            K.mm(p_v[:, h, :], wT[:, h, :], H[:, h, :])
                            K.mm(p_a[:, h, :], kqT[:, HN + h, :], H[:, h, :])
                        K.tt(vn[:], u[:], p_v, ALU.subtract)
                        for h in range(HN):
                            K.mm(p_b[:, h, :], A1[:, h, :], vn[:, h, :])
                        K.tt(vE[:], vn[:], bc3(eE[:], 2, [64, HN, 128]), ALU.mult, eng='pool')
                        for h in range(HN):
                            K.mm(p_h[:, h, :], k3[:, h, :], vE[:, h, :])
                        K.tt(o[:], p_a, bc3(eG[:], 2, [64, HN, 128]), ALU.mult)
                        K.tt(o[:], o[:], p_b, ALU.add)
                        K.dma(ygd[d][rs, hc], o[:].rearrange("p h e -> p (h e)"), q='sp')
                        K.tt(H[:], H[:], bc3(gC[:], 2, [128, HN, 128]), ALU.mult)
                        K.tt(H[:], H[:], p_h, ALU.add)
                        yield
        alive = [chain(0), chain(1)]
        while alive:
            for g in list(alive):
                try:
                    next(g)
                except StopIteration:
                    alive.remove(g)
        K.S.barrier()


def stage_gdn_finish(K, P, ygd, norm_g, ygf):
    cfg = K.cfg
    GW, GH = cfg['GW'], cfg['GH']
    T = cfg['T']
    og = cfg['off']['g_g'][0]
    with contextlib.ExitStack() as es:
        ng = K.sb(es, "gng", [128, 128])
        bc_row(K, ng[:], norm_g)
        y0 = [K.sb(es, f"hy0{i}", [128, GW]) for i in range(2)]
        y1 = [K.sb(es, f"hy1{i}", [128, GW]) for i in range(2)]
        gg = [K.sb(es, f"hgg{i}", [128, GW]) for i in range(2)]
        tmp = K.sb(es, "htmp", [128, GW])
        ssq = K.sb(es, "hssq", [128, GH])
        for i, t0 in enumerate(range(0, T, 128)):
            a, b, g = y0[i % 2], y1[i % 2], gg[i % 2]
            rs = slice(t0, t0 + 128)
            K.dma(a[:], ygd[0][rs, :])
            K.dma(b[:], ygd[1][rs, :])
            K.dma(g[:], P[rs, og:og + GW])
            K.tt(a[:], a[:], b[:], ALU.add)
            a3 = a[:].rearrange("p (h e) -> p h e", e=128)
            t3 = tmp[:].rearrange("p (h e) -> p h e", e=128)
            K.tt(t3, a3, a3, ALU.mult)
            K.red(ssq[:], t3, ALU.add)
            K.rsqrt(es, ssq[:], ssq[:], 1.0 / 128, 1e-6)
            K.tt(a3, a3, bc3(ssq[:], 2, [128, GH, 128]), ALU.mult)
            K.tt(a3, a3, bc3(ng[:], 1, [128, GH, 128]), ALU.mult, eng='pool')
            K.act(g[:], g[:], AF.Silu)
            K.tt(a[:], a[:], g[:], ALU.mult)
            K.dma(ygf[rs, :], a[:], q='sp')
        K.S.barrier()


def stage_rwkv_prep(K, cst, P, shift_mu, w0, w_up, a0, a_up, g_up, k_k, k_a, r_k, RA):
    cfg = K.cfg
    RW, RH = cfg['RW'], cfg['RH']
    CTX, SEQ = cfg['CTX'], cfg['SEQ']
    orr = cfg['off']['r_r'][0]
    ow = cfg['off']['r_w'][0]
    ident = cst[:, 0, :]
    NB = (RW + 511) // 512
    with contextlib.ExitStack() as es:
        mu = K.sb(es, "rmu", [128, 3, 3 * RW])
        bc_row(K, mu[:, 0, :], shift_mu[0, :])
        bc_row(K, mu[:, 1, :], shift_mu[1, :])
        K.tt(mu[:, 2, :], mu[:, 0, :], mu[:, 1, :], ALU.add)
        K.ts(mu[:, 2, :], mu[:, 2, :], -1.0, ALU.mult, 1.0, ALU.add)
        pv = K.sb(es, "rpv", [128, 5, RW])
        bc_row(K, pv[:, 0, :], w0[0, :])
        bc_row(K, pv[:, 1, :], w0[1, :])
        bc_row(K, pv[:, 2, :], a0)
        bc_row(K, pv[:, 3, :], k_k)
        bc_row(K, pv[:, 4, :], k_a)
        rkb = K.sb(es, "rrk", [128, RW])
        bc_row(K, rkb[:], r_k.rearrange("a b -> (a b)"))
        wu = K.sb(es, "rwu", [64, 3, RW])
        K.dma(wu[:, 0, :], w_up[0])
        K.dma(wu[:, 1, :], w_up[1])
        K.dma(wu[:, 2, :], a_up)
        gu = K.sb(es, "rgu", [128, RW])
        K.dma(gu[:], g_up)
        x = K.sb(es, "rx", [128, 3 * RW])
        xp = K.sb(es, "rxp", [128, 3 * RW])
        xn = K.sb(es, "rxn", [128, 3 * RW])
        lo = K.sb(es, "rlo", [128, 320])
        loT = K.sb(es, "rloT", [128, 4, 128])
        t1 = K.sb(es, "rt1", [128, RW])
        t2 = K.sb(es, "rt2", [128, RW])
        av = K.sb(es, "rav", [128, RW])
        ssq = K.sb(es, "rssq", [128, RH])
        p_tr = K.ps(es, "rp_tr", [128, 4, 128])
        p_l = [K.ps(es, f"rp_l{i}", [128, 512]) for i in range(2)]
        li = 0

        def lora(dst, lhsT, rhs):
            nonlocal li
            for nb in range(NB):
                n0, n1 = nb * 512, min(RW, (nb + 1) * 512)
                p = p_l[li % 2]
                li += 1
                K.mm(p[:, 0:n1 - n0], lhsT, rhs[:, n0:n1])
                K.cp(dst[:, n0:n1], p[:, 0:n1 - n0], eng='act')
        for (base, seglen) in [(0, CTX), (CTX, SEQ)]:
            for s0 in range(0, seglen, 128):
                rs = slice(base + s0, base + s0 + 128)
                K.dma(x[:], P[rs, orr:orr + 3 * RW])
                if s0 == 0:
                    K.memset(xp[:], 0.0, eng='pool')
                if s0 + 128 >= seglen:
                    K.memset(xn[:], 0.0, eng='pool')
                load_rows(K, xp[:], P, orr, 3 * RW, base, seglen, s0 - 1, 128)
                load_rows(K, xn[:], P, orr, 3 * RW, base, seglen, s0 + 1, 128)
                K.tt(x[:], x[:], mu[:, 2, :], ALU.mult)
                K.tt(xp[:], xp[:], mu[:, 0, :], ALU.mult, eng='pool')
                K.tt(xn[:], xn[:], mu[:, 1, :], ALU.mult, eng='pool')
                K.tt(x[:], x[:], xp[:], ALU.add)
                K.tt(x[:], x[:], xn[:], ALU.add)
                r_, k_, v_ = x[:, 0:RW], x[:, RW:2 * RW], x[:, 2 * RW:3 * RW]
                K.dma(RA['r'][rs, :], r_, q='sp')
                K.dma(RA['v'][rs, :], v_, q='sp')
                K.dma(lo[:], P[rs, ow:ow + 320])
                K.act(lo[:, 0:128], lo[:, 0:128], AF.Tanh)
                K.act(lo[:, 192:320], lo[:, 192:320], AF.Sigmoid)
                K.tr(p_tr[0:64, 0, :], lo[:, 0:64], ident)
                K.tr(p_tr[0:64, 1, :], lo[:, 64:128], ident)
                K.tr(p_tr[0:64, 2, :], lo[:, 128:192], ident)
                K.tr(p_tr[:, 3, :], lo[:, 192:320], ident)
                K.cp(loT[0:64, 0:3, :], p_tr[0:64, 0:3, :], eng='act')
                K.cp(loT[:, 3, :], p_tr[:, 3, :], eng='dve')
                for d in range(2):
                    lora(t1, loT[0:64, d, :], wu[:, d, :])
                    K.tt(t1[:], t1[:], pv[:, d, :], ALU.add)
                    K.act(t1[:], t1[:], AF.Sigmoid)
                    K.ts(t1[:], t1[:], -math.exp(-0.5), ALU.mult)
                    K.dma(RA['lw'][rs, d * RW:(d + 1) * RW], t1[:], q='sp')
                lora(av, loT[0:64, 2, :], wu[:, 2, :])
                K.tt(av[:], av[:], pv[:, 2, :], ALU.add)
                K.act(av[:], av[:], AF.Sigmoid)
                K.dma(RA['a'][rs, :], av[:], q='sp')
                lora(t2, loT[:, 3, :], gu[:])
                K.dma(RA['g'][rs, :], t2[:], q='sp')
                K.tt(t1[:], k_, pv[:, 3, :], ALU.mult)
                l2norm_heads(K, t1[:].rearrange("p (h e) -> p h e", e=64), t2[:].rearrange("p (h e) -> p h e", e=64),
                             ssq[:], RH, 64)
                K.dma(RA['kk'][rs, :], t1[:], q='sp')
                K.stt(t2[:], av[:], -1.0, pv[:, 4, :], ALU.add, ALU.mult)
                K.stt(t2[:], t2[:], 1.0, k_, ALU.add, ALU.mult)
                K.dma(RA['k'][rs, :], t2[:], q='sp')
                K.tt(t2[:], t2[:], r_, ALU.mult)
                K.tt(t2[:], t2[:], rkb[:], ALU.mult)
                K.red(ssq[:], t2[:].rearrange("p (h e) -> p h e", e=64), ALU.add)
                K.dma(RA['bon'][rs, :], ssq[:], q='sp')
        K.S.barrier()


def stage_rwkv_scan(K, cst, RA, yrd):
    cfg = K.cfg
    RW, RH = cfg['RW'], cfg['RH']
    CTX, T = cfg['CTX'], cfg['T']
    nchunk = T // 64
    HN = min(4, RH)
    W_ = HN * 64
    ones = cst[:, 1, :]
    i64 = cst[0:64, 0, 0:64]
    MID = 32
    with contextlib.ExitStack() as es:
        pb = [K.ps(es, f"wpb{i}", [64, 512]) for i in range(7)]

        def chain(dsel):
            H = K.sb(es, "wH", [64, HN, 64])
            Hs = K.sb(es, "wHs", [64, HN, 64])
            names = ('r', 'k', 'v', 'kk', 'a')
            IN = [{n: K.sb(es, f"w{n}{i}", [64, W_]) for n in names + ('lw',)} for i in range(2)]
            trimid = K.sb(es, "wtrimid", [64, 64])
            col2 = K.sb(es, "wcol2", [64, 2])
            Gm = K.sb(es, "wGm", [64, W_])
            eP = K.sb(es, "weP", [64, W_])
            eN = K.sb(es, "weN", [64, W_])
            eX = K.sb(es, "weX", [64, W_])
            eE = K.sb(es, "weE", [64, W_])
            gcm = K.sb(es, "wgcm", [64, HN, 2])
            bt = K.sb(es, "wbt", [64, W_])
            SC = K.sb(es, "wSC", [64, 4, W_])
            Ke = K.sb(es, "wKe", [64, W_])
            Be = K.sb(es, "wBe", [64, W_])
            FT = K.sb(es, "wFT", [64, 4, HN, 64])
            Mb = [K.sb(es, f"wM{i}", [64, HN, 64]) for i in range(2)]
            Nb = [K.sb(es, f"wN{i}", [64, HN, 64]) for i in range(2)]
            Pm = K.sb(es, "wPm", [64, HN, 64])
            AK = K.sb(es, "wAK", [64, HN, 64])
            QK = K.sb(es, "wQK", [64, HN, 64])
            QB = K.sb(es, "wQB", [64, HN, 64])
            Z0 = K.sb(es, "wZ0", [64, HN, 64])
            Ut = K.sb(es, "wUt", [64, HN, 64])
            WT = K.sb(es, "wWT", [64, HN, 64])
            U = K.sb(es, "wU", [64, HN, 64])
            Y = K.sb(es, "wY", [64, HN, 64])

            def v3(t, n):
                return t[:, 0:n * 64].rearrange("p (a b) -> p a b", b=64)
            old_ser = K.S.ser_engs
            import os
            sf, st_ = int(os.environ.get('RW_SER_FROM', '0')), int(os.environ.get('RW_SER_TO', '0'))

            def sec(i):
                K.S.ser_engs = tuple(set(old_ser) | {'act', 'dve'}) if sf <= i < st_ else old_ser
            for hg in range(RH // HN):
                hc = slice(hg * W_, (hg + 1) * W_)
                for d in (dsel,):
                    tri = cst[0:64, 2 + d, 0:64]
                    stri = cst[0:64, 4 + d, 0:64]
                    strict_n = cst[0:64, 4 + (1 - d), 0:64]
                    K.ts(trimid[:], tri, tri[:, MID:MID + 1], ALU.subtract)
                    K.cp(col2[:, 0:1], ones[0:64, 0:1])
                    K.cp(col2[:, 1:2], tri[:, MID:MID + 1])
                    K.memset(H[:], 0.0)
                    order = list(range(nchunk))
                    if d == 1:
                        order = list(range(CTX // 64 - 1, -1, -1)) + list(range(nchunk - 1, CTX // 64 - 1, -1))
                    for ci, c in enumerate(order):
                        X = IN[ci % 2]
                        rs = slice(c * 64, (c + 1) * 64)
                        for n in names:
                            K.dma(X[n][:], RA[n][rs, hc])
                        K.dma(X['lw'][:], RA['lw'][rs, d * RW + hg * W_:d * RW + (hg + 1) * W_])
                        sec(0)
                        lw = X['lw'][:]
                        K.mm(pb[0][:, 0:W_], trimid[:], lw)
                        K.mm(pb[0][:, W_:2 * W_], strict_n, lw)
                        pgc = pb[1][:, 0:2 * HN].rearrange("p (a b) -> p a b", b=2)
                        for h in range(HN):
                            K.mm(pgc[:, h, :], X['lw'][:, h * 64:(h + 1) * 64], col2[:])
                        K.cp(Gm[:], pb[0][:, 0:W_])
                        K.act(eP[:], pb[0][:, 0:W_], AF.Exp)
                        K.ts(eN[:], Gm[:], -1.0, ALU.mult)
                        K.act(eN[:], eN[:], AF.Exp)
                        K.act(eE[:], pb[0][:, W_:2 * W_], AF.Exp)
                        K.tt(eX[:], Gm[:], lw, ALU.subtract)
                        K.act(eX[:], eX[:], AF.Exp)
                        K.act(gcm[:], pgc, AF.Exp)
                        sec(1)
                        K.tt(bt[:], X['kk'][:], X['a'][:], ALU.mult, eng='pool')
                        K.tt(SC[:, 0, :], X['k'][:], eN[:], ALU.mult)
                        K.tt(SC[:, 1, :], bt[:], eN[:], ALU.mult)
                        K.stt(SC[:, 2, :], X['kk'][:], -1.0, eX[:], ALU.mult, ALU.mult)
                        K.tt(SC[:, 3, :], X['r'][:], eP[:], ALU.mult)
                        K.tt(Ke[:], X['k'][:], eE[:], ALU.mult, eng='pool')
                        K.tt(Be[:], bt[:], eE[:], ALU.mult, eng='pool')
                        K.tt(Hs[:], H[:], bc3(gcm[:, :, 1], 2, [64, HN, 64]), ALU.mult)
                        sec(2)
                        for j in range(4):
                            pt = pb[2 + (j % 2)]
                            for h in range(HN):
                                K.tr(v3(pt, HN)[:, h, :], SC[:, j, h * 64:(h + 1) * 64], i64)
                            K.cp(FT[:, j, :, :], v3(pt, HN), eng=('act' if j % 2 else 'dve'))
                        kT, bT, aT, qT = (FT[:, j, :, :] for j in range(4))
                        sec(3)
                        pMN = v3(pb[4], 2 * HN)
                        pG2 = v3(pb[5], 2 * HN)
                        pG3 = v3(pb[6], HN)
                        for h in range(HN):
                            K.mm(pMN[:, h, :], bT[:, h, :], aT[:, h, :])
                            K.mm(pMN[:, HN + h, :], aT[:, h, :], bT[:, h, :])
                            K.mm(pG2[:, h, :], kT[:, h, :], aT[:, h, :])
                            K.mm(pG2[:, HN + h, :], kT[:, h, :], qT[:, h, :])
                            K.mm(pG3[:, h, :], bT[:, h, :], qT[:, h, :])
                        K.tt(Mb[0][:], pMN[:, 0:HN, :], bc3(stri, 1, [64, HN, 64]), ALU.mult)
                        K.tt(Nb[0][:], pMN[:, HN:2 * HN, :], bc3(strict_n, 1, [64, HN, 64]), ALU.mult)
                        K.tt(AK[:], pG2[:, 0:HN, :], bc3(stri, 1, [64, HN, 64]), ALU.mult)
                        K.tt(QK[:], pG2[:, HN:2 * HN, :], bc3(tri, 1, [64, HN, 64]), ALU.mult)
                        K.tt(QB[:], pG3, bc3(tri, 1, [64, HN, 64]), ALU.mult)
                        sec(4)
                        solve_nilpotent(K, [Mb[0][:], Mb[1][:]], [Nb[0][:], Nb[1][:]], Pm[:],
                                        v3(pb[2], HN), v3(pb[3], HN), v3(pb[4], HN), i64, HN)
                        sec(5)
                        pz = v3(pb[5], HN)
                        for h in range(HN):
                            K.mm(pz[:, h, :], AK[:, h, :], X['v'][:, h * 64:(h + 1) * 64])
                        K.cp(Z0[:], pz, eng='act')
                        pu = v3(pb[6], HN)
                        pw = v3(pb[5], HN)
                        for h in range(HN):
                            K.mm(pu[:, h, :], Pm[:, h, :], Z0[:, h, :])
                            K.mm(pw[:, h, :], SC[:, 2, h * 64:(h + 1) * 64], Pm[:, h, :])
                        K.cp(Ut[:], pu, eng='act')
                        K.cp(WT[:], pw, eng='dve')
                        sec(6)
                        p1 = v3(pb[2], HN)
                        for h in range(HN):
                            K.mm(p1[:, h, :], WT[:, h, :], Hs[:, h, :])
                        K.tt(U[:], Ut[:], p1, ALU.add)
                        py = v3(pb[3], HN)
                        ph = v3(pb[4], HN)
                        for h in range(HN):
                            vh = X['v'][:, h * 64:(h + 1) * 64]
                            K.mm(py[:, h, :], qT[:, h, :], Hs[:, h, :], start=True, stop=False)
                            K.mm(py[:, h, :], QB[:, h, :], U[:, h, :], start=False, stop=False)
                            K.mm(py[:, h, :], QK[:, h, :], vh, start=False, stop=True)
                        for h in range(HN):
                            vh = X['v'][:, h * 64:(h + 1) * 64]
                            K.mm(ph[:, h, :], Be[:, h * 64:(h + 1) * 64], U[:, h, :], start=True, stop=False)
                            K.mm(ph[:, h, :], Ke[:, h * 64:(h + 1) * 64], vh, start=False, stop=True)
                        K.cp(Y[:], py, eng='act')
                        K.dma(yrd[d][rs, hc], Y[:].rearrange("p h e -> p (h e)"), q='sp')
                        K.tt(H[:], H[:], bc3(gcm[:, :, 0], 2, [64, HN, 64]), ALU.mult)
                        K.tt(H[:], H[:], ph, ALU.add)
                        yield
        alive = [chain(0), chain(1)]
        while alive:
            for g in list(alive):
                try:
                    next(g)
                except StopIteration:
                    alive.remove(g)
        K.S.ser_engs = ()
        K.S.barrier()


def stage_rwkv_finish(K, RA, yrd, ln_g, ln_b, yrf):
    cfg = K.cfg
    RW, RH = cfg['RW'], cfg['RH']
    T = cfg['T']
    with contextlib.ExitStack() as es:
        lg = K.sb(es, "vlg", [128, RW])
        lb = K.sb(es, "vlb", [128, RW])
        bc_row(K, lg[:], ln_g)
        bc_row(K, lb[:], ln_b)
        y0 = [K.sb(es, f"vy0{i}", [128, RW]) for i in range(2)]
        y1 = [K.sb(es, f"vy1{i}", [128, RW]) for i in range(2)]
        vv = [K.sb(es, f"vvv{i}", [128, RW]) for i in range(2)]
        gg = [K.sb(es, f"vgg{i}", [128, RW]) for i in range(2)]
        bo = [K.sb(es, f"vbo{i}", [128, RH]) for i in range(2)]
        tmp = K.sb(es, "vtmp", [128, RW])
        st = K.sb(es, "vst", [128, 2, RH])
        for i, t0 in enumerate(range(0, T, 128)):
            a, b, v, g, bn = y0[i % 2], y1[i % 2], vv[i % 2], gg[i % 2], bo[i % 2]
            rs = slice(t0, t0 + 128)
            K.dma(a[:], yrd[0][rs, :])
            K.dma(b[:], yrd[1][rs, :])
            K.dma(v[:], RA['v'][rs, :])
            K.dma(g[:], RA['g'][rs, :])
            K.dma(bn[:], RA['bon'][rs, :])
            K.tt(a[:], a[:], b[:], ALU.add)
            a3 = a[:].rearrange("p (h e) -> p h e", e=64)
            t3 = tmp[:].rearrange("p (h e) -> p h e", e=64)
            K.red(st[:, 0, :], a3, ALU.add)
            K.ts(st[:, 0, :], st[:, 0, :], 1.0 / 64, ALU.mult)
            K.tt(a3, a3, bc3(st[:, 0, :], 2, [128, RH, 64]), ALU.subtract)
            K.tt(t3, a3, a3, ALU.mult)
            K.red(st[:, 1, :], t3, ALU.add)
            K.rsqrt(es, st[:, 1, :], st[:, 1, :], 1.0 / 64, 64e-5)
            K.tt(a3, a3, bc3(st[:, 1, :], 2, [128, RH, 64]), ALU.mult)
            K.tt(a[:], a[:], lg[:], ALU.mult, eng='pool')
            K.tt(a[:], a[:], lb[:], ALU.add)
            v3_ = v[:].rearrange("p (h e) -> p h e", e=64)
            K.tt(v3_, v3_, bc3(bn[:], 2, [128, RH, 64]), ALU.mult, eng='pool')
            K.tt(a[:], a[:], v[:], ALU.add)
            K.tt(a[:], a[:], g[:], ALU.mult)
            K.dma(yrf[rs, :], a[:], q='sp')
        K.S.barrier()
```

```python
import math
import contextlib
import numpy as np
import concourse.bass as bass
import concourse.mybir as mybir
from concourse.bass_utils import run_bass_kernel_spmd

F32 = mybir.dt.float32
BF16 = mybir.dt.bfloat16
AF = mybir.ActivationFunctionType
ALU = mybir.AluOpType
AX = mybir.AxisListType


def make_cfg(D=2048, SEQ=4096, CTX=256, DEPTH=2, BATCH=2):
    c = dict(D=D, SEQ=SEQ, CTX=CTX, DEPTH=DEPTH, BATCH=BATCH, GRID_W=64, CHUNK=64)
    c['T'] = CTX + SEQ
    c['MW'] = D // 2; c['MH'] = c['MW'] // 64; c['MG'] = 2; c['MS'] = 128
    c['RW'] = D // 2; c['RH'] = c['RW'] // 64
    c['GW'] = D // 2; c['GH'] = c['GW'] // 128
    c['NE'] = 32; c['FF'] = D // 4
    cols = (("m_z", c['MW']), ("m_x", c['MW']), ("m_B", 256), ("m_C", 256), ("m_dt", 2 * c['MH']),
            ("r_r", c['RW']), ("r_k", c['RW']), ("r_v", c['RW']), ("r_w", 128), ("r_a", 64), ("r_g", 128),
            ("g_q", c['GW']), ("g_k", c['GW']), ("g_v", c['GW']), ("g_a", 2 * c['GH']), ("g_b", 2 * c['GH']),
            ("g_g", c['GW']), ("gate", 3 * D))
    off = {}
    s = 0
    for n, w in cols:
        off[n] = (s, w)
        s += w
    c['off'] = off
    c['INW'] = s
    return c


class Sched:
    NDS = 24
    serialize = False
    ser_engs = ()

    def __init__(self, nc):
        self.nc = nc
        self.eng = {'pe': nc.tensor, 'act': nc.scalar, 'dve': nc.vector, 'pool': nc.gpsimd, 'sp': nc.sync}
        self.prog = {e: [] for e in self.eng}
        self.ccnt = {e: 0 for e in self.eng}
        self.sems = {}
        for e in self.eng:
            self.sems[('c', e)] = nc.alloc_semaphore(name=f"c_{e}")
        self.dcnt = [0] * self.NDS
        for i in range(self.NDS):
            self.sems[('d', i)] = nc.alloc_semaphore(name=f"d_{i}")
        self.drr = 0
        self.seen = {e: {} for e in self.eng}
        self.rec = {}
        self.rows = {}
        self.psum = set()
        self.nops = 0

    def reg_tensor(self, name, shape, space):
        rs = 1
        for s in shape[1:]:
            rs *= s
        self.rows[name] = rs if space != 'dram' else None
        if space == 'ps':
            self.psum.add(name)

    def region(self, ap):
        name = ap.name
        pat = ap.ap
        off = int(ap.offset)
        rs = self.rows[name]
        if name in self.psum:
            return name, (0, 128, 0, rs)
        if rs is None:
            lo = hi = off
            for st, n in pat:
                if n > 1:
                    if st >= 0:
                        hi += st * (n - 1)
                    else:
                        lo += st * (n - 1)
            return name, (0, 1, lo, hi + 1)
        p0 = off // rs
        f0 = off % rs
        npart = pat[0][1]
        lo = hi = f0
        for st, n in pat[1:]:
            if n > 1:
                if st >= 0:
                    hi += st * (n - 1)
                else:
                    lo += st * (n - 1)
        return name, (p0, p0 + npart, lo, hi + 1)

    def dense(self, ap):
        name = ap.name
        if name in self.psum:
            return True
        pat = ap.ap
        rs = self.rows[name]
        n = 1
        for st, c in (pat if rs is None else pat[1:]):
            n *= c if st != 0 else 1
        _, reg = self.region(ap)
        return n == reg[3] - reg[2]

    @staticmethod
    def _ov(a, b):
        return a[0] < b[1] and b[0] < a[1] and a[2] < b[3] and b[2] < a[3]

    @staticmethod
    def _cov(a, b):
        return a[0] <= b[0] and a[1] >= b[1] and a[2] <= b[2] and a[3] >= b[3]

    def _deps(self, reads, writes, me=None):
        deps = {}
        rr = [self.region(a) for a in reads]
        ww = [self.region(a) + (self.dense(a),) for a in writes]
        for name, reg in rr:
            ps = name in self.psum
            for r in self.rec.get(name, ()):
                if (r[1] == 'W' or (ps and r[2] != me)) and self._ov(r[0], reg):
                    deps[r[2]] = max(deps.get(r[2], 0), r[3])
        for name, reg, _dn in ww:
            for r in self.rec.get(name, ()):
                if self._ov(r[0], reg):
                    deps[r[2]] = max(deps.get(r[2], 0), r[3])
        return deps, rr, ww

    def _record(self, rr, ww, key, val):
        for name, reg, dn in ww:
            lst = self.rec.setdefault(name, [])
            if dn:
                lst[:] = [r for r in lst if not self._cov(reg, r[0])]
            lst.append([reg, 'W', key, val])
        for name, reg in rr:
            lst = self.rec.setdefault(name, [])
            for r in lst:
                if r[1] == 'R' and r[2] == key and r[0] == reg:
                    r[3] = max(r[3], val)
                    break
            else:
                lst.append([reg, 'R', key, val])

    def _emit_waits(self, e, deps, skip_self=False):
        for key, val in deps.items():
            if skip_self and key == ('c', e):
                continue
            if key == ('c', 'pe'):
                if e == 'pe':
                    continue
                if self.ccnt['pe'] <= val:
                    self.ccnt['pe'] += 1
                    self.prog['pe'].append(('op', self.dummy_fn, key, 1))
                val = val + 1
            if self.seen[e].get(key, 0) >= val:
                continue
            self.seen[e][key] = val
            self.prog[e].append(('wait', key, val))

    def op(self, e, fn, reads, writes, pe_chain=False):
        deps, rr, ww = self._deps(reads, writes, me=('c', e))
        self._emit_waits(e, deps, skip_self=pe_chain)
        self.ccnt[e] += 1
        key = ('c', e)
        self.prog[e].append(('op', fn, key, 1))
        self._record(rr, ww, key, self.ccnt[e])
        self.nops += 1
        if self.serialize or e in self.ser_engs:
            self.barrier()

    def dma(self, out, in_, q='sp', **kw):
        deps, rr, ww = self._deps([in_], [out])
        k = self.drr
        self.drr = (self.drr + 1) % self.NDS
        key = ('d', k)
        if self.dcnt[k] > 0:
            deps[key] = max(deps.get(key, 0), self.dcnt[k])
        self._emit_waits(q, deps)
        self.dcnt[k] += 16

        def fn(eng, out=out, in_=in_, kw=kw):
            return eng.dma_start(out=out, in_=in_, **kw)
        self.prog[q].append(('op', fn, key, 16))
        self._record(rr, ww, key, self.dcnt[k])
        self.nops += 1
        if self.serialize or 'dma' in self.ser_engs:
            self.barrier()

    def _all_tokens(self):
        final = {}
        for e in self.eng:
            if self.ccnt[e] > 0:
                final[('c', e)] = self.ccnt[e]
        for i in range(self.NDS):
            if self.dcnt[i] > 0:
                final[('d', i)] = self.dcnt[i]
        return final

    def barrier(self, drop=()):
        final = self._all_tokens()
        for e in self.eng:
            self._emit_waits(e, dict(final))
        for n in drop:
            self.rec.pop(n, None)

    def finalize(self, block):
        self._emit_waits('sp', self._all_tokens())
        sems = self.sems

        def mk(e):
            prog = self.prog[e]

            def body(eng):
                for it in prog:
                    if it[0] == 'wait':
                        eng.wait_ge(sems[it[1]], it[2])
                    else:
                        it[1](eng).then_inc(sems[it[2]], it[3])
            return body
        block.tensor(mk('pe'))
        block.scalar(mk('act'))
        block.vector(mk('dve'))
        block.gpsimd(mk('pool'))
        block.sync(mk('sp'))


def _is_ap(x):
    return hasattr(x, 'ap') and hasattr(x, 'offset')


class KB:
    def __init__(self, nc, cfg, es):
        self.nc = nc
        self.cfg = cfg
        self.S = Sched(nc)
        self.uid = 0
        self.rr = 0
        self.pool_eng = 'pool'
        self.dma_queues = ('sp', 'act')
        import os
        self.f32r = bool(os.environ.get('KM_F32R'))
        self.es = es
        dsb = self.sb(es, "dmy_sb", [128, 8], BF16)
        dps = self.ps(es, "dmy_ps", [128, 8])
        self.S.dummy_fn = lambda e: e.matmul(dps[0:8, 0:8], lhsT=dsb[0:8, 0:8], rhs=dsb[0:8, 0:8], start=True, stop=True)
        self.memset(dsb[:], 0.0)
        self.mm(dps[0:8, 0:8], dsb[0:8, 0:8], dsb[0:8, 0:8])

    def dram(self, name, shape, dt=F32, kind=None):
        if kind is None:
            t = self.nc.dram_tensor(name, list(shape), dt)
        else:
            t = self.nc.dram_tensor(name, list(shape), dt, kind=kind)
        self.S.reg_tensor(name, shape, 'dram')
        return t.ap()

    def sb(self, es, name, shape, dt=F32):
        self.uid += 1
        name = f"{name}_{self.uid}"
        t = es.enter_context(self.nc.sbuf_tensor(name, list(shape), dt))
        self.S.reg_tensor(name, shape, 'sb')
        return t

    def ps(self, es, name, shape, dt=F32):
        self.uid += 1
        name = f"{name}_{self.uid}"
        full = [128, 512] if dt == F32 else [128, 1024]
        t = es.enter_context(self.nc.psum_tensor(name, full, dt))
        self.S.reg_tensor(name, full, 'ps')
        n = 1
        for d in shape[1:]:
            n *= d
        assert n <= full[1] and shape[0] <= 128
        v = t[0:shape[0], 0:n]
        if len(shape) == 3:
            v = v.rearrange("p (a b) -> p a b", b=shape[2])
        return v

    def dma(self, out, in_, q=None, **kw):
        if q is None:
            q = self.dma_queues[self.rr % len(self.dma_queues)]
            self.rr += 1
        self.S.dma(out, in_, q=q, **kw)

    def mm(self, out, lhsT, rhs, start=True, stop=True):
        if self.f32r and lhsT.dtype == F32 and rhs.dtype == F32:
            lhsT = lhsT.bitcast(mybir.dt.float32r)
            rhs = rhs.bitcast(mybir.dt.float32r)
        self.S.op('pe', lambda e: e.matmul(out, lhsT=lhsT, rhs=rhs, start=start, stop=stop),
                  [lhsT, rhs] + ([] if start else [out]), [out], pe_chain=not start)

    def tr(self, out, in_, ident):
        self.S.op('pe', lambda e: e.transpose(out, in_, ident), [in_, ident], [out])

    def act(self, out, in_, func, bias=None, scale=None, accum_out=None, eng='act'):
        kw = {}
        rd = [in_]
        wr = [out]
        if bias is not None:
            kw['bias'] = bias
            if _is_ap(bias):
                rd.append(bias)
        if scale is not None:
            kw['scale'] = scale
            if _is_ap(scale):
                rd.append(scale)
        if accum_out is not None:
            kw['accum_out'] = accum_out
            wr.append(accum_out)
        self.S.op('act', lambda e: e.activation(out=out, in_=in_, func=func, **kw), rd, wr)

    def tt(self, out, in0, in1, op, eng='dve'):
        self.S.op(eng, lambda e: e.tensor_tensor(out=out, in0=in0, in1=in1, op=op), [in0, in1], [out])

    def ts(self, out, in0, s1, op0, s2=None, op1=None, eng='dve', accum_out=None):
        rd = [in0] + [s for s in (s1, s2) if _is_ap(s)]
        kw = {}
        wr = [out]
        if op1 is not None:
            kw['op1'] = op1
        if accum_out is not None:
            kw['accum_out'] = accum_out
            wr.append(accum_out)
        self.S.op(eng, lambda e: e.tensor_scalar(out=out, in0=in0, scalar1=s1, scalar2=s2, op0=op0, **kw), rd, wr)

    def stt(self, out, in0, scalar, in1, op0, op1):
        rd = [in0, in1] + ([scalar] if _is_ap(scalar) else [])
        self.S.op('dve', lambda e: e.scalar_tensor_tensor(out=out, in0=in0, scalar=scalar, in1=in1, op0=op0, op1=op1),
                  rd, [out])

    def red(self, out, in_, op, axis=AX.X):
        self.S.op('dve', lambda e: e.tensor_reduce(out=out, in_=in_, axis=axis, op=op), [in_], [out])

    def cp(self, out, in_, eng='dve'):
        if eng == 'act':
            self.S.op('act', lambda e: e.activation(out=out, in_=in_, func=AF.Identity), [in_], [out])
        else:
            self.S.op(eng, lambda e: e.tensor_copy(out=out, in_=in_), [in_], [out])

    def memset(self, ap, val, eng='dve'):
        self.S.op(eng, lambda e: e.memset(ap, val), [], [ap])

    def recip(self, out, in_):
        self.S.op('dve', lambda e: e.reciprocal(out=out, in_=in_), [in_], [out])

    def rsqrt(self, es_tmp, out, in_, scale, eps):
        self.ts(out, in_, scale, ALU.mult, eps, ALU.add)
        self.act(out, out, AF.Sqrt)
        self.recip(out, out)


def stage_modvec(K, es0, c_b, c_ctx, ada_w, ada_b, n1g, n2g, modx):
    cfg = K.cfg
    D = cfg['D']
    KC = D // 128
    with contextlib.ExitStack() as es:
        cT = K.sb(es, "cT", [128, KC, 2])
        K.dma(cT[:, :, 0], c_b.rearrange("(k p) -> p k", p=128), q='sp', allow_slow_non_contiguous=True)
        K.dma(cT[:, :, 1], c_ctx.rearrange("(k p) -> p k", p=128), q='sp', allow_slow_non_contiguous=True)
        K.act(cT[:], cT[:], AF.Silu)
        mod = K.sb(es, "mod", [2, 6 * D])
        bt = [K.sb(es, f"mbt{i}", [2, 512]) for i in range(2)]
        wt = [K.sb(es, f"mw{i}", [128, KC, 512]) for i in range(2)]
        acc = [K.ps(es, f"macc{i}", [2, 512]) for i in range(2)]
        nb = 6 * D // 512
        for cb in range(nb):
            w = wt[cb % 2]
            bb = bt[cb % 2]
            dma_w(K, w[:], ada_w[:, cb * 512:(cb + 1) * 512])
            bc_row(K, bb[:], ada_b[cb * 512:(cb + 1) * 512], n=2, q='sp')
            a = acc[cb % 2]
            for k in range(KC):
                K.mm(a[:], cT[:, k, :], w[:, k, :], start=(k == 0), stop=(k == KC - 1))
            K.tt(mod[:, cb * 512:(cb + 1) * 512], a[:], bb[:], ALU.add)
        g = K.sb(es, "mg", [2, 2, D])
        bc_row(K, g[:, 0, :], n1g, n=2, q='sp')
        bc_row(K, g[:, 1, :], n2g, n=2, q='sp')
        K.stt(mod[:, 1 * D:2 * D], mod[:, 1 * D:2 * D], 1.0, g[:, 0, :], ALU.add, ALU.mult)
        K.stt(mod[:, 4 * D:5 * D], mod[:, 4 * D:5 * D], 1.0, g[:, 1, :], ALU.add, ALU.mult)
        for r_, src in enumerate((1, 0, 2, 4, 3, 5)):
            K.dma(modx[:, r_, :], mod[:, src * D:(src + 1) * D], q='sp')
        K.S.barrier()


def dma_w(K, dst, src, q=None):
    KCn = dst.shape[1]
    for k0 in range(0, KCn, 4):
        k1 = min(KCn, k0 + 4)
        K.dma(dst[:, k0:k1, :], src[k0 * 128:k1 * 128, :].rearrange("(k p) n -> p k n", p=128), q=q)


def bc_row(K, dst, row_ap, n=128, q=None):
    K.dma(dst, row_ap.rearrange("(o f) -> o f", o=1).partition_broadcast(n).rearrange("p o f -> p (o f)"), q=q)


def norm_mod_tile(K, es, xt, hb, a_bc, sh_bc, ss, D, junk):
    K.act(junk, xt, AF.Square, accum_out=ss)
    K.rsqrt(es, ss, ss, 1.0 / D, 1e-6)
    K.stt(junk, xt, ss, a_bc, ALU.mult, ALU.mult)
    K.tt(hb, junk, sh_bc, ALU.add)


def stage_inproj(K, lat, modx, w_in, P, PG, identb):
    cfg = K.cfg
    D = cfg['D']
    KC = D // 128
    INW = cfg['INW']
    nct = cfg['CTX'] // 128
    nlt = cfg['SEQ'] // 128
    g0 = cfg['off']['gate'][0]
    groups = [(0, nct, 1)]
    t = nct
    while t < nct + nlt:
        n = min(8, nct + nlt - t)
        groups.append((t, n, 0))
        t += n
    blocks = []
    c = 0
    while c < INW:
        lim = g0 if c < g0 else INW
        n = min(512, lim - c)
        blocks.append((c, n, c >= g0))
        c += n
    with contextlib.ExitStack() as es:
        xt = [K.sb(es, f"xt{i}", [128, D]) for i in range(2)]
        junk = K.sb(es, "junk", [128, D])
        hb = K.sb(es, "hb", [128, D], BF16)
        a_bc = K.sb(es, "abc", [128, D])
        sh_bc = K.sb(es, "shbc", [128, D])
        ss = K.sb(es, "ss", [128, 2])
        hT = K.sb(es, "hT", [128, KC, 8 * 128], BF16)
        wt = [K.sb(es, f"wt{i}", [128, KC, 512], BF16) for i in range(2)]
        ev = [K.sb(es, f"ev{i}", [128, 512]) for i in range(3)]
        pt = [K.ps(es, f"pt{i}", [128, 8, 128], BF16) for i in range(2)]
        acc = [K.ps(es, f"acc{i}", [128, 512]) for i in range(3)]
        it = 0
        wi = 0
        for (t0, n, mi) in groups:
            bc_row(K, a_bc[:], modx[mi, 0, :], q='sp')
            bc_row(K, sh_bc[:], modx[mi, 1, :], q='sp')
            for j in range(n):
                x = xt[j % 2]
                K.dma(x[:], lat[(t0 + j) * 128:(t0 + j + 1) * 128, :])
                norm_mod_tile(K, es, x[:], hb[:], a_bc[:], sh_bc[:], ss[:, 0:1], D, junk[:])
                for k0 in range(0, KC, 8):
                    p = pt[(k0 // 8) % 2]
                    kn = min(8, KC - k0)
                    for k in range(kn):
                        K.tr(p[:, k, :], hb[:, (k0 + k) * 128:(k0 + k + 1) * 128], identb[:])
                    K.cp(hT[:, k0:k0 + kn, j * 128:(j + 1) * 128], p[:, 0:kn, :], eng=('dve' if (k0 // 8) % 2 else 'act'))
            for (c0, ncol, isg) in blocks:
                w = wt[wi % 2]
                wi += 1
                dma_w(K, w[:, :, 0:ncol], w_in[:, c0:c0 + ncol], q='pool')
                for j in range(n):
                    a = acc[it % 3]
                    e = ev[it % 3]
                    for k in range(KC):
                        K.mm(a[:, 0:ncol], hT[:, k, j * 128:(j + 1) * 128], w[:, k, 0:ncol], start=(k == 0), stop=(k == KC - 1))
                    if isg:
                        K.act(e[:, 0:ncol], a[:, 0:ncol], AF.Sigmoid)
                    elif it % 2 == 0:
                        K.cp(e[:, 0:ncol], a[:, 0:ncol], eng='act')
                    else:
                        K.cp(e[:, 0:ncol], a[:, 0:ncol], eng='dve')
                    if isg:
                        K.dma(PG[(t0 + j) * 128:(t0 + j + 1) * 128, c0 - g0:c0 - g0 + ncol], e[:, 0:ncol], q='sp')
                    else:
                        K.dma(P[(t0 + j) * 128:(t0 + j + 1) * 128, c0:c0 + ncol], e[:, 0:ncol], q='sp')
                    it += 1
        K.S.barrier()


def load_rows(K, dst, arr, c0, ncols, base, seglen, s0, n, perm=None, q=None):
    lo = max(s0, 0)
    hi = min(s0 + n, seglen)
    if hi <= lo:
        return
    if perm is None:
        K.dma(dst[lo - s0:hi - s0, 0:ncols], arr[base + lo:base + hi, c0:c0 + ncols], q=q)
        return
    A, B = perm
    i = lo
    while i < hi:
        a, b = divmod(i, B)
        m = min(B - b, hi - i)
        r0 = base + b * A + a
        K.dma(dst[i - s0:i - s0 + m, 0:ncols], arr[r0:r0 + (m - 1) * A + 1:A, c0:c0 + ncols], q=q)
        i += m


def bc3(ap, axis, shape):
    return ap.unsqueeze(axis).to_broadcast(list(shape))


def softplus_(K, x, bias_bc):
    K.tt(x, x, bias_bc, ALU.add)
    K.act(x, x, AF.Exp)
    K.act(x, x, AF.Ln, bias=1.0)


def stage_mamba_prep(K, P, conv_w, conv_b, A_log, dt_bias, mxbc, mdt, mdA):
    cfg = K.cfg
    MW, MH = cfg['MW'], cfg['MH']
    NCH = MW + 512
    CTX, SEQ = cfg['CTX'], cfg['SEQ']
    rows = SEQ // 64
    ox = cfg['off']['m_x'][0]
    odt = cfg['off']['m_dt'][0]
    with contextlib.ExitStack() as es:
        wk = K.sb(es, "mcw", [128, 5, NCH])
        for k in range(5):
            bc_row(K, wk[:, k, :], conv_w[k, :])
        cb = K.sb(es, "mcb", [128, NCH])
        bc_row(K, cb[:], conv_b)
        aneg = K.sb(es, "aneg", [128, 2 * MH])
        bc_row(K, aneg[:], A_log.rearrange("a b -> (a b)"))
        K.act(aneg[:], aneg[:], AF.Exp)
        dtb = K.sb(es, "dtb", [128, 2 * MH])
        bc_row(K, dtb[:], dt_bias.rearrange("a b -> (a b)"))
        xs = [K.sb(es, f"mx{k}", [128, NCH]) for k in range(5)]
        acc = K.sb(es, "macc", [128, NCH])
        dt = K.sb(es, "mdt", [128, 2 * MH])
        dA = K.sb(es, "mdA", [128, 2 * MH])
        for seg, (base, seglen, perm) in enumerate([(0, CTX, None), (CTX, SEQ, (64, rows))]):
            for s0 in range(0, seglen, 128):
                for k in range(5):
                    edge = (s0 + k - 2 < 0) or (s0 + k - 2 + 128 > seglen)
                    if edge:
                        K.memset(xs[k][:], 0.0, eng='pool')
                    load_rows(K, xs[k][:], P, ox, NCH, base, seglen, s0 + k - 2, 128, perm)
                K.tt(acc[:], xs[0][:], wk[:, 0, :], ALU.mult)
                for k in range(1, 5):
                    K.tt(xs[k][:], xs[k][:], wk[:, k, :], ALU.mult, eng='pool')
                    K.tt(acc[:], acc[:], xs[k][:], ALU.add)
                K.tt(acc[:], acc[:], cb[:], ALU.add)
                K.act(acc[:], acc[:], AF.Silu)
                K.dma(mxbc[base + s0:base + s0 + 128, :], acc[:], q='sp')
                load_rows(K, dt[:], P, odt, 2 * MH, base, seglen, s0, 128, perm)
                softplus_(K, dt[:], dtb[:])
                K.stt(dA[:], dt[:], -1.0, aneg[:], ALU.mult, ALU.mult)
                K.dma(mdt[base + s0:base + s0 + 128, :], dt[:], q='sp')
                K.dma(mdA[base + s0:base + s0 + 128, :], dA[:], q='sp')
        K.S.barrier()


def stage_mamba_scan(K, cst, mxbc, mdt, mdA, ymd):
    cfg = K.cfg
    MW, MH = cfg['MW'], cfg['MH']
    HG = MH // 2
    CTX, SEQ, T = cfg['CTX'], cfg['SEQ'], cfg['T']
    nchunk = T // 64
    ident, ones = cst[:, 0, :], cst[:, 1, :]
    HB = min(8, MH)
    with contextlib.ExitStack() as es:
        H = K.sb(es, "mH", [128, MH, 64])
        X = [K.sb(es, f"mX{i}", [64, MW + 512]) for i in range(2)]
        dtt = [K.sb(es, f"mdtt{i}", [64, 2 * MH]) for i in range(2)]
        dAt = [K.sb(es, f"mdAt{i}", [64, 2 * MH]) for i in range(2)]
        BCT = K.sb(es, "mBCT", [128, 4, 64])
        Z = K.sb(es, "mZ", [64, MH, 64])
        G = K.sb(es, "mG", [64, MH])
        eG = K.sb(es, "meG", [64, MH])
        eE = K.sb(es, "meE", [64, MH])
        gC = K.sb(es, "mgC", [128, MH])
        Dm = K.sb(es, "mD", [64, MH, 64])
        sc = K.sb(es, "msc", [64, 2, 64])
        xdt = K.sb(es, "mxdt", [64, MH, 64])
        xe = K.sb(es, "mxe", [64, MH, 64])
        Y = K.sb(es, "mY", [64, MH, 64])
        p_t = K.ps(es, "mp_t", [128, 4, 64])[:]
        p_g = K.ps(es, "mp_g", [128, 4, MH])[:]
        p_sc = K.ps(es, "mp_sc", [64, 2, 64])[:]
        p_gb = K.ps(es, "mp_gb", [64, HB, 64])
        p_y = K.ps(es, "mp_y", [64, HB, 64])
        p_c = K.ps(es, "mp_c", [64, HB, 64])
        p_h = K.ps(es, "mp_h", [128, HB, 64])
        for d in range(2):
            tri = cst[0:64, 2 + d, 0:64]
            K.memset(H[:], 0.0)
            order = list(range(nchunk))
            if d == 1:
                order = list(range(CTX // 64 - 1, -1, -1)) + list(range(nchunk - 1, CTX // 64 - 1, -1))
            for ci, c in enumerate(order):
                x = X[ci % 2]
                dt_ = dtt[ci % 2]
                dA_ = dAt[ci % 2]
                K.dma(x[:], mxbc[c * 64:(c + 1) * 64, :])
                K.dma(dt_[:], mdt[c * 64:(c + 1) * 64, :])
                K.dma(dA_[:], mdA[c * 64:(c + 1) * 64, :])
                dAd = dA_[:, d * MH:(d + 1) * MH]
                dtd = dt_[:, d * MH:(d + 1) * MH]
                for j in range(4):
                    K.tr(p_t[:, j, :], x[:, MW + j * 128:MW + (j + 1) * 128], ident[0:64, 0:64])
                K.cp(BCT[:], p_t, eng='act')
                K.mm(p_g[0:64, 0, :], tri, dAd)
                K.mm(p_g[0:64, 1, :], ones[0:64, 0:64], dAd)
                K.mm(p_g[:, 2, :], ones[0:64, :], dAd)
                K.cp(G[:], p_g[0:64, 0, :])
                K.act(eG[:], p_g[0:64, 0, :], AF.Exp)
                K.tt(eE[:], p_g[0:64, 1, :], G[:], ALU.subtract)
                K.act(eE[:], eE[:], AF.Exp)
                K.act(gC[:], p_g[:, 2, :], AF.Exp)
                K.tt(Z[:], bc3(dAd, 2, [64, MH, 64]), bc3(tri, 1, [64, MH, 64]), ALU.mult, eng=K.pool_eng)
                for g in range(2):
                    K.mm(p_sc[:, g, :], BCT[:, g, :], BCT[:, 2 + g, :])
                K.tt(sc[:], p_sc, bc3(tri, 1, [64, 2, 64]), ALU.mult)
                xs3 = x[:, 0:MW].rearrange("p (h e) -> p h e", e=64)
                K.tt(xdt[:], xs3, bc3(dtd, 2, [64, MH, 64]), ALU.mult, eng=K.pool_eng)
                K.tt(xe[:], xdt[:], bc3(eE[:], 2, [64, MH, 64]), ALU.mult, eng=K.pool_eng)
                for h0 in range(0, MH, HB):
                    hs = slice(h0, h0 + HB)
                    K.mm(p_gb[:], ones[0:64, 0:64], Z[:, hs, :])
                    K.tt(Dm[:, hs, :], p_gb[:], bc3(G[:, hs], 2, [64, HB, 64]), ALU.subtract)
                    K.ts(Dm[:, hs, :], Dm[:, hs, :], 0.0, ALU.min)
                    K.act(Dm[:, hs, :], Dm[:, hs, :], AF.Exp)
                    for g in range(2):
                        ga, gb_ = max(h0, g * HG), min(h0 + HB, (g + 1) * HG)
                        if gb_ > ga:
                            K.tt(Dm[:, ga:gb_, :], Dm[:, ga:gb_, :], bc3(sc[:, g, :], 1, [64, gb_ - ga, 64]), ALU.mult)
                    for h in range(h0, h0 + HB):
                        g = h // HG
                        K.mm(p_y[:, h - h0, :], Dm[:, h, :], xdt[:, h, :])
                        K.mm(p_c[:, h - h0, :], BCT[:, 2 + g, :], H[:, h, :])
                    for h in range(h0, h0 + HB):
                        g = h // HG
                        K.mm(p_h[:, h - h0, :], x[:, MW + g * 128:MW + (g + 1) * 128], xe[:, h, :])
                    K.cp(Y[:, hs, :], p_y[:], eng='act')
                    K.tt(Z[:, hs, :], p_c[:], bc3(eG[:, hs], 2, [64, HB, 64]), ALU.mult)
                    K.tt(Y[:, hs, :], Y[:, hs, :], Z[:, hs, :], ALU.add)
                    K.tt(H[:, hs, :], H[:, hs, :], bc3(gC[:, hs], 2, [128, HB, 64]), ALU.mult)
                    K.tt(H[:, hs, :], H[:, hs, :], p_h[:], ALU.add)
                K.dma(ymd[d][c * 64:(c + 1) * 64, :], Y[:].rearrange("p h e -> p (h e)"), q='sp')
        K.S.barrier()


def stage_mamba_finish(K, P, mxbc, ymd, D_skip, norm_g, ymf):
    cfg = K.cfg
    MW, MH = cfg['MW'], cfg['MH']
    CTX, SEQ = cfg['CTX'], cfg['SEQ']
    rows = SEQ // 64
    oz = cfg['off']['m_z'][0]
    GWD = MW // 2
    with contextlib.ExitStack() as es:
        dsk = K.sb(es, "dsk", [128, MH])
        bc_row(K, dsk[:], D_skip)
        ng = K.sb(es, "mng", [128, MW])
        bc_row(K, ng[:], norm_g)
        y0 = [K.sb(es, f"fy0{i}", [128, MW]) for i in range(2)]
        y1 = [K.sb(es, f"fy1{i}", [128, MW]) for i in range(2)]
        xs = [K.sb(es, f"fxs{i}", [128, MW]) for i in range(2)]
        z = [K.sb(es, f"fz{i}", [128, MW]) for i in range(2)]
        junk = K.sb(es, "fjunk", [128, GWD])
        ss = K.sb(es, "fss", [128, 2])
        i = 0
        for (base, seglen, perm) in [(0, CTX, None), (CTX, SEQ, (rows, 64))]:
            for s0 in range(0, seglen, 128):
                a, b, c, zz = y0[i % 2], y1[i % 2], xs[i % 2], z[i % 2]
                i += 1
                load_rows(K, a[:], ymd[0], 0, MW, base, seglen, s0, 128, perm)
                load_rows(K, b[:], ymd[1], 0, MW, base, seglen, s0, 128, perm)
                load_rows(K, c[:], mxbc, 0, MW, base, seglen, s0, 128, perm)
                K.dma(zz[:], P[base + s0:base + s0 + 128, oz:oz + MW])
                K.tt(a[:], a[:], b[:], ALU.add)
                c3 = c[:].rearrange("p (h e) -> p h e", e=64)
                K.tt(c3, c3, bc3(dsk[:], 2, [128, MH, 64]), ALU.mult, eng='pool')
                K.tt(a[:], a[:], c[:], ALU.add)
                K.act(zz[:], zz[:], AF.Silu)
                K.tt(a[:], a[:], zz[:], ALU.mult)
                for g in range(2):
                    K.act(junk[:], a[:, g * GWD:(g + 1) * GWD], AF.Square, accum_out=ss[:, g:g + 1])
                K.rsqrt(es, ss[:], ss[:], 1.0 / GWD, 1e-6)
                for g in range(2):
                    K.stt(a[:, g * GWD:(g + 1) * GWD], a[:, g * GWD:(g + 1) * GWD], ss[:, g:g + 1],
                          ng[:, g * GWD:(g + 1) * GWD], ALU.mult, ALU.mult)
                K.dma(ymf[base + s0:base + s0 + 128, :], a[:], q='sp')
        K.S.barrier()


def to_featmajor(K, src_bf, dstT, j, KCn, pt, identb):
    for k0 in range(0, KCn, 8):
        p = pt[(k0 // 8) % 2]
        kn = min(8, KCn - k0)
        for k in range(kn):
            K.tr(p[:, k, :], src_bf[:, (k0 + k) * 128:(k0 + k + 1) * 128], identb[:])
        K.cp(dstT[:, k0:k0 + kn, j * 128:(j + 1) * 128], p[:, 0:kn, :], eng=('dve' if (k0 // 8) % 2 else 'act'))


def token_groups(cfg, gs):
    nct = cfg['CTX'] // 128
    nlt = cfg['SEQ'] // 128
    groups = []
    t = 0
    while t < nct:
        n = min(gs, nct - t)
        groups.append((t, n, 1))
        t += n
    while t < nct + nlt:
        n = min(gs, nct + nlt - t)
        groups.append((t, n, 0))
        t += n
    return groups


def stage_merge(K, PG, ybr, w_br, w_out, modx, lat, identb, t_lo=0):
    cfg = K.cfg
    D = cfg['D']
    KC = D // 128
    BW = cfg['MW']
    KB_ = BW // 128
    g0 = cfg['off']['gate'][0]
    GS = 4
    NCB = D // 512
    with contextlib.ExitStack() as es:
        yt = [K.sb(es, f"gy{i}", [128, BW]) for i in range(2)]
        yb = K.sb(es, "gyb", [128, BW], BF16)
        yT = K.sb(es, "gyT", [128, KB_, GS * 128], BF16)
        wb = [K.sb(es, f"gwb{i}", [128, KB_, 512], BF16) for i in range(2)]
        mg = K.sb(es, "gmg", [128, GS, D])
        gt = [K.sb(es, f"ggt{i}", [128, 512]) for i in range(2)]
        tmp = K.sb(es, "gtmp", [128, 512])
        mb = K.sb(es, "gmb", [128, D], BF16)
        mT = K.sb(es, "gmT", [128, KC, GS * 128], BF16)
        wo = [K.sb(es, f"gwo{i}", [128, KC, 512], BF16) for i in range(2)]
        g1 = K.sb(es, "gg1", [128, D])
        lt = [K.sb(es, f"glt{i}", [128, 512]) for i in range(2)]
        pt = [K.ps(es, f"gpt{i}", [128, 8, 128], BF16) for i in range(2)]
        acc = [K.ps(es, f"gacc{i}", [128, 512]) for i in range(3)]
        it = 0
        wi = 0
        for (t0, n, mi) in token_groups(cfg, GS):
            if t0 < t_lo:
                continue
            bc_row(K, g1[:], modx[mi, 2, :], q='sp')
            for br in range(3):
                for j in range(n):
                    y = yt[j % 2]
                    K.dma(y[:], ybr[br][(t0 + j) * 128:(t0 + j + 1) * 128, :])
                    K.cp(yb[:], y[:], eng='pool')
                    to_featmajor(K, yb, yT, j, KB_, pt, identb)
                for cb in range(NCB):
                    w = wb[wi % 2]
                    wi += 1
                    dma_w(K, w[:], w_br[br][:, cb * 512:(cb + 1) * 512], q='pool')
                    for j in range(n):
                        a = acc[it % 3]
                        g = gt[it % 2]
                        it += 1
                        K.dma(g[:], PG[(t0 + j) * 128:(t0 + j + 1) * 128, br * D + cb * 512:br * D + (cb + 1) * 512])
                        for k in range(KB_):
                            K.mm(a[:], yT[:, k, j * 128:(j + 1) * 128], w[:, k, :], start=(k == 0), stop=(k == KB_ - 1))
                        if br == 0:
                            K.tt(mg[:, j, cb * 512:(cb + 1) * 512], a[:], g[:], ALU.mult)
                        else:
                            K.tt(tmp[:], a[:], g[:], ALU.mult)
                            K.tt(mg[:, j, cb * 512:(cb + 1) * 512], mg[:, j, cb * 512:(cb + 1) * 512], tmp[:], ALU.add, eng='pool')
            for j in range(n):
                K.cp(mb[:], mg[:, j, :], eng='act')
                to_featmajor(K, mb, mT, j, KC, pt, identb)
            for cb in range(NCB):
                w = wo[wi % 2]
                wi += 1
                dma_w(K, w[:], w_out[:, cb * 512:(cb + 1) * 512], q='pool')
                for j in range(n):
                    a = acc[it % 3]
                    l_ = lt[it % 2]
                    it += 1
                    rs = slice((t0 + j) * 128, (t0 + j + 1) * 128)
                    K.dma(l_[:], lat[rs, cb * 512:(cb + 1) * 512])
                    for k in range(KC):
                        K.mm(a[:], mT[:, k, j * 128:(j + 1) * 128], w[:, k, :], start=(k == 0), stop=(k == KC - 1))
                    K.tt(tmp[:], a[:], g1[:, cb * 512:(cb + 1) * 512], ALU.mult)
                    K.tt(l_[:], l_[:], tmp[:], ALU.add)
                    K.dma(lat[rs, cb * 512:(cb + 1) * 512], l_[:], q='sp')
        K.S.barrier()


def stage_moe(K, lat, modx, grp_w, grp_b, exp_w, exp_b, w1, w3, w2, ident, identb, t_lo=0):
    cfg = K.cfg
    D = cfg['D']
    KC = D // 128
    FF = cfg['FF']
    FC = FF // 128
    NE = cfg['NE']
    GS = 4
    NCB = D // 512
    NR = 4 + NE
    with contextlib.ExitStack() as es:
        xt0 = K.sb(es, "ex0", [128, D])
        xt = [xt0, xt0]
        h = K.sb(es, "eh", [128, D])
        hb = K.sb(es, "ehb", [128, D], BF16)
        a_bc = K.sb(es, "eabc", [128, D])
        sh_bc = K.sb(es, "eshbc", [128, D])
        g2 = K.sb(es, "eg2", [128, D])
        ss = K.sb(es, "ess", [128, 2])
        hT = K.sb(es, "ehT", [128, KC, GS * 128], BF16)
        hTf = K.sb(es, "ehTf", [128, KC, 128])
        rw = K.sb(es, "erw", [128, KC, NR])
        rb = K.sb(es, "erb", [128, NR])
        K.dma(rw[:, :, 0:4], grp_w.rearrange("(k p) n -> p k n", p=128), q='sp', allow_slow_non_contiguous=True)
        K.dma(rw[:, :, 4:NR], exp_w.rearrange("(k p) n -> p k n", p=128), q='sp', allow_slow_non_contiguous=True)
        bc_row(K, rb[:, 0:4], grp_b, q='sp')
        bc_row(K, rb[:, 4:NR], exp_b, q='sp')
        lg = K.sb(es, "elg", [128, NR])
        sm = K.sb(es, "esm", [128, 16])
        oh = K.sb(es, "eoh", [128, 4])
        ig = K.sb(es, "eig", [128, 8])
        i2 = K.sb(es, "ei2", [128, 8])
        m1 = K.sb(es, "em1", [128, 8])
        m2 = K.sb(es, "em2", [128, 8])
        comb = K.sb(es, "ecomb", [128, GS, NE])
        wa = [K.sb(es, f"ewa{i}", [128, KC, FF], BF16) for i in range(2)]
        wc = [K.sb(es, f"ewc{i}", [128, KC, FF], BF16) for i in range(2)]
        wd = [K.sb(es, "ewd0", [128, FC, D], BF16)] * 2
        sl = K.sb(es, "esl", [128, 512])
        hidT = K.sb(es, "ehidT", [128, FC, GS * 128], BF16)
        yacc = K.sb(es, "eyacc", [128, GS, D])
        junk = yacc[:, 0, :]
        pt0 = K.ps(es, "ept0", [128, 8, 128], BF16)
        pt = [pt0, pt0]
        ptf = K.ps(es, "eptf", [128, 4, 128])
        pr = K.ps(es, "epr", [128, NR])
        p1 = K.ps(es, "ep1", [128, 512])
        p3 = K.ps(es, "ep3", [128, 512])
        py0 = K.ps(es, "epy0", [128, 512])
        py = [py0, py0]
        wi = 0
        it = 0
        for (t0, n, mi) in token_groups(cfg, GS):
            if t0 < t_lo:
                continue
            bc_row(K, a_bc[:], modx[mi, 3, :], q='sp')
            bc_row(K, sh_bc[:], modx[mi, 4, :], q='sp')
            bc_row(K, g2[:], modx[mi, 5, :], q='sp')
            for j in range(n):
                x = xt[j % 2]
                K.dma(x[:], lat[(t0 + j) * 128:(t0 + j + 1) * 128, :])
                norm_mod_tile(K, es, x[:], h[:], a_bc[:], sh_bc[:], ss[:, 0:1], D, junk)
                K.cp(hb[:], h[:], eng='pool')
                to_featmajor(K, hb, hT, j, KC, pt, identb)
                for k0 in range(0, KC, 4):
                    for k in range(4):
                        K.tr(ptf[:, k, :], h[:, (k0 + k) * 128:(k0 + k + 1) * 128], ident[:])
                    K.cp(hTf[:, k0:k0 + 4, :], ptf[:], eng='act')
                for k in range(KC):
                    K.mm(pr[:], hTf[:, k, :], rw[:, k, :], start=(k == 0), stop=(k == KC - 1))
                K.tt(lg[:], pr[:], rb[:], ALU.add)
                K.red(sm[:, 0:1], lg[:, 0:4], ALU.max)
                K.ts(oh[:], lg[:, 0:4], sm[:, 0:1], ALU.is_equal)
                K.ts(sm[:, 4:8], lg[:, 0:4], sm[:, 0:1], ALU.subtract)
                K.act(sm[:, 4:8], sm[:, 4:8], AF.Exp)
                K.red(sm[:, 1:2], sm[:, 4:8], ALU.add)
                K.recip(sm[:, 1:2], sm[:, 1:2])
                K.ts(ig[:], lg[:, 4:12], oh[:, 0:1], ALU.mult)
                for g in range(1, 4):
                    K.stt(ig[:], lg[:, 4 + 8 * g:12 + 8 * g], oh[:, g:g + 1], ig[:], ALU.mult, ALU.add)
                K.red(sm[:, 2:3], ig[:], ALU.max)
                K.ts(m1[:], ig[:], sm[:, 2:3], ALU.is_equal)
                K.stt(i2[:], m1[:], -1e30, ig[:], ALU.mult, ALU.add)
                K.red(sm[:, 3:4], i2[:], ALU.max)
                K.ts(m2[:], i2[:], sm[:, 3:4], ALU.is_equal)
                K.tt(sm[:, 8:9], sm[:, 3:4], sm[:, 2:3], ALU.subtract)
                K.act(sm[:, 8:9], sm[:, 8:9], AF.Exp)
                K.ts(sm[:, 9:10], sm[:, 8:9], 1.0, ALU.add)
                K.recip(sm[:, 9:10], sm[:, 9:10])
                K.tt(sm[:, 9:10], sm[:, 9:10], sm[:, 1:2], ALU.mult)
                K.tt(sm[:, 10:11], sm[:, 9:10], sm[:, 8:9], ALU.mult)
                K.ts(m1[:], m1[:], sm[:, 9:10], ALU.mult)
                K.stt(m1[:], m2[:], sm[:, 10:11], m1[:], ALU.mult, ALU.add)
                for g in range(4):
                    K.ts(comb[:, j, g * 8:(g + 1) * 8], m1[:], oh[:, g:g + 1], ALU.mult)
            K.memset(yacc[:], 0.0, eng='pool')
            ntok = n * 128
            for e in range(NE):
                a_, c_, d_ = wa[wi % 2], wc[wi % 2], wd[wi % 2]
                wi += 1
                dma_w(K, a_[:], w1[e], q='pool')
                dma_w(K, c_[:], w3[e], q='pool')
                for cb in range(NCB):
                    K.dma(d_[:, :, cb * 512:(cb + 1) * 512], w2[e][:, cb * 512:(cb + 1) * 512].rearrange("(k p) n -> p k n", p=128), q='pool')
                for fc in range(FC):
                    for tb in range(0, ntok, 512):
                        tn = min(512, ntok - tb)
                        for k in range(KC):
                            K.mm(p1[:, 0:tn], a_[:, k, fc * 128:(fc + 1) * 128], hT[:, k, tb:tb + tn], start=(k == 0), stop=(k == KC - 1))
                        for k in range(KC):
                            K.mm(p3[:, 0:tn], c_[:, k, fc * 128:(fc + 1) * 128], hT[:, k, tb:tb + tn], start=(k == 0), stop=(k == KC - 1))
                        K.act(sl[:, 0:tn], p1[:, 0:tn], AF.Silu)
                        K.tt(hidT[:, fc, tb:tb + tn], sl[:, 0:tn], p3[:, 0:tn], ALU.mult)
                for j in range(n):
                    for cb in range(NCB):
                        p = py[it % 2]
                        it += 1
                        for fc in range(FC):
                            K.mm(p[:], hidT[:, fc, j * 128:(j + 1) * 128], d_[:, fc, cb * 512:(cb + 1) * 512], start=(fc == 0), stop=(fc == FC - 1))
                        ysl = yacc[:, j, cb * 512:(cb + 1) * 512]
                        K.stt(ysl, p[:], comb[:, j, e:e + 1], ysl, ALU.mult, ALU.add)
            for j in range(n):
                x = xt[j % 2]
                rs = slice((t0 + j) * 128, (t0 + j + 1) * 128)
                K.dma(x[:], lat[rs, :])
                K.tt(yacc[:, j, :], yacc[:, j, :], g2[:], ALU.mult)
                K.tt(x[:], x[:], yacc[:, j, :], ALU.add)
                K.dma(lat[rs, :], x[:], q='sp')
        K.S.barrier()


def stage_final(K, lat, fg, out):
    cfg = K.cfg
    D = cfg['D']
    nct = cfg['CTX'] // 128
    nlt = cfg['SEQ'] // 128
    with contextlib.ExitStack() as es:
        g = K.sb(es, "fg", [128, D])
        bc_row(K, g[:], fg, q='sp')
        xt = [K.sb(es, f"fx{i}", [128, D]) for i in range(2)]
        junk = K.sb(es, "fjk", [128, D])
        ss = K.sb(es, "fss2", [128, 1])
        for j in range(nlt):
            x = xt[j % 2]
            K.dma(x[:], lat[(nct + j) * 128:(nct + j + 1) * 128, :])
            K.act(junk[:], x[:], AF.Square, accum_out=ss[:])
            K.rsqrt(es, ss[:], ss[:], 1.0 / D, 1e-6)
            K.stt(x[:], x[:], ss[:], g[:], ALU.mult, ALU.mult)
            K.dma(out[j * 128:(j + 1) * 128, :], x[:], q='sp')
        K.S.barrier()


PARAMS = [("ada_w", lambda c: [c['DEPTH'], c['D'], 6 * c['D']]), ("ada_b", lambda c: [c['DEPTH'], 6 * c['D']]),
          ("norm1_g", lambda c: [c['DEPTH'], c['D']]), ("norm2_g", lambda c: [c['DEPTH'], c['D']]),
          ("w_in", lambda c: [c['DEPTH'], c['D'], c['INW']]),
          ("m_conv_w", lambda c: [c['DEPTH'], 5, c['MW'] + 512]), ("m_conv_b", lambda c: [c['DEPTH'], c['MW'] + 512]),
          ("m_A_log", lambda c: [c['DEPTH'], 2, c['MH']]), ("m_dt_bias", lambda c: [c['DEPTH'], 2, c['MH']]),
          ("m_D", lambda c: [c['DEPTH'], c['MH']]), ("m_norm_g", lambda c: [c['DEPTH'], c['MW']]),
          ("r_shift_mu", lambda c: [c['DEPTH'], 2, 3 * c['RW']]), ("r_w0", lambda c: [c['DEPTH'], 2, c['RW']]),
          ("r_w_up", lambda c: [c['DEPTH'], 2, 64, c['RW']]), ("r_a0", lambda c: [c['DEPTH'], c['RW']]),
          ("r_a_up", lambda c: [c['DEPTH'], 64, c['RW']]), ("r_g_up", lambda c: [c['DEPTH'], 128, c['RW']]),
          ("r_k_k", lambda c: [c['DEPTH'], c['RW']]), ("r_k_a", lambda c: [c['DEPTH'], c['RW']]),
          ("r_r_k", lambda c: [c['DEPTH'], c['RH'], 64]), ("r_ln_g", lambda c: [c['DEPTH'], c['RW']]),
          ("r_ln_b", lambda c: [c['DEPTH'], c['RW']]),
          ("g_conv_w", lambda c: [c['DEPTH'], 5, 3 * c['GW']]), ("g_A_log", lambda c: [c['DEPTH'], 2, c['GH']]),
          ("g_dt_bias", lambda c: [c['DEPTH'], 2, c['GH']]), ("g_norm_g", lambda c: [c['DEPTH'], 128]),
          ("w_br_m", lambda c: [c['DEPTH'], c['MW'], c['D']]), ("w_br_r", lambda c: [c['DEPTH'], c['RW'], c['D']]),
          ("w_br_g", lambda c: [c['DEPTH'], c['GW'], c['D']]), ("w_out", lambda c: [c['DEPTH'], c['D'], c['D']]),
          ("moe_grp_w", lambda c: [c['DEPTH'], c['D'], 4]), ("moe_grp_b", lambda c: [c['DEPTH'], 4]),
          ("moe_exp_w", lambda c: [c['DEPTH'], c['D'], c['NE']]), ("moe_exp_b", lambda c: [c['DEPTH'], c['NE']]),
          ("moe_w1", lambda c: [c['DEPTH'], c['NE'], c['D'], c['FF']]), ("moe_w3", lambda c: [c['DEPTH'], c['NE'], c['D'], c['FF']]),
          ("moe_w2", lambda c: [c['DEPTH'], c['NE'], c['FF'], c['D']]), ("final_norm_g", lambda c: [c['D']])]


def make_cst():
    c = np.zeros((128, 8, 128), np.float32)
    i = np.arange(128)
    c[:, 0, :] = np.eye(128)
    c[:, 1, :] = 1.0
    s = (i % 64)[:, None]
    t = (i % 64)[None, :]
    c[:, 2, :] = (s <= t)
    c[:, 3, :] = (s >= t)
    c[:, 4, :] = (s < t)
    c[:, 5, :] = (s > t)
    return c


def build_program(cfg, stages=None):
    nc = bass.Bass("TRN2", target_bir_lowering=False)
    D, T, SEQ, CTX, INW, MW, MH = (cfg[k] for k in ("D", "T", "SEQ", "CTX", "INW", "MW", "MH"))
    es = contextlib.ExitStack()
    with es:
        K = KB(nc, cfg, es)
        import os
        if os.environ.get('KM_SER'):
            K.S.ser_engs = tuple(os.environ['KM_SER'].split(','))
        x = K.dram("x", [SEQ, D], kind="ExternalInput")
        ctx = K.dram("ctx", [CTX, D], kind="ExternalInput")
        c_b = K.dram("c", [D], kind="ExternalInput")
        c_ctx = K.dram("c_ctx", [D], kind="ExternalInput")
        cstd = K.dram("cst", [128, 8, 128], kind="ExternalInput")
        W = {n: K.dram(n, f(cfg), kind="ExternalInput") for n, f in PARAMS}
        out = K.dram("out", [SEQ, D], kind="ExternalOutput")
        lat = K.dram("lat", [T, D])
        P = K.dram("P", [T, cfg['off']['gate'][0]])
        PG = K.dram("PG", [T, 3 * D])
        modx = K.dram("modx", [2, 6, D])
        mxbc = K.dram("mxbc", [T, MW + 512])
        mdt = K.dram("mdt", [T, 2 * MH])
        mdA = K.dram("mdA", [T, 2 * MH])
        ymd = [K.dram(f"ymd{d}", [T, MW]) for d in range(2)]
        ybr = [K.dram(f"ybr{i}", [T, MW]) for i in range(3)]
        cst = K.sb(es, "cst", [128, 8, 128])
        identb = K.sb(es, "identb", [128, 128], BF16)
        K.dma(cst[:], cstd, q='sp')
        K.cp(identb[:], cst[:, 0, :])
        ident = cst[:, 0, :]
        for r0 in range(0, CTX, 128):
            K.dma(lat[r0:r0 + 128, :], ctx[r0:r0 + 128, :])
        for r0 in range(0, SEQ, 128):
            K.dma(lat[CTX + r0:CTX + r0 + 128, :], x[r0:r0 + 128, :])
        GW, GH, RW, RH = cfg['GW'], cfg['GH'], cfg['RW'], cfg['RH']
        gqkv = K.dram("gqkv", [T, 3 * GW])
        glog = K.dram("glog", [T, 2 * GH])
        gbeta = K.dram("gbeta", [T, 2 * GH])
        ygd = [K.dram(f"ygd{d}", [T, GW]) for d in range(2)]
        RA = {n: K.dram("ra_" + n, [T, RW]) for n in ('r', 'k', 'v', 'kk', 'a', 'g')}
        RA['lw'] = K.dram("ra_lw", [T, 2 * RW])
        RA['bon'] = K.dram("ra_bon", [T, RH])
        yrd = [K.dram(f"yrd{d}", [T, RW]) for d in range(2)]
        nct = CTX // 128
        on = (lambda n: True) if stages is None else (lambda n: n in stages)
        for l in range(cfg['DEPTH']):
            last = (l == cfg['DEPTH'] - 1)
            if on('modvec'):
                stage_modvec(K, es, c_b, c_ctx, W['ada_w'][l], W['ada_b'][l], W['norm1_g'][l], W['norm2_g'][l], modx)
            if on('inproj'):
                stage_inproj(K, lat, modx, W['w_in'][l], P, PG, identb)
            if on('mprep'):
                stage_mamba_prep(K, P, W['m_conv_w'][l], W['m_conv_b'][l], W['m_A_log'][l], W['m_dt_bias'][l], mxbc, mdt, mdA)
            if on('mscan'):
                stage_mamba_scan(K, cst, mxbc, mdt, mdA, ymd)
            if on('mfin'):
                stage_mamba_finish(K, P, mxbc, ymd, W['m_D'][l], W['m_norm_g'][l], ybr[0])
            if on('rwkv'):
                stage_rwkv_prep(K, cst, P, W['r_shift_mu'][l], W['r_w0'][l], W['r_w_up'][l], W['r_a0'][l], W['r_a_up'][l],
                                W['r_g_up'][l], W['r_k_k'][l], W['r_k_a'][l], W['r_r_k'][l], RA)
                stage_rwkv_scan(K, cst, RA, yrd)
                stage_rwkv_finish(K, RA, yrd, W['r_ln_g'][l], W['r_ln_b'][l], ybr[1])
            if on('gdn'):
                stage_gdn_prep(K, P, W['g_conv_w'][l], W['g_A_log'][l], W['g_dt_bias'][l], gqkv, glog, gbeta)
                stage_gdn_scan(K, cst, gqkv, glog, gbeta, ygd)
                stage_gdn_finish(K, P, ygd, W['g_norm_g'][l], ybr[2])
            t_lo = nct if last else 0
            if on('merge'):
                stage_merge(K, PG, ybr, [W['w_br_m'][l], W['w_br_r'][l], W['w_br_g'][l]], W['w_out'][l], modx, lat, identb, t_lo=t_lo)
            if on('moe'):
                stage_moe(K, lat, modx, W['moe_grp_w'][l], W['moe_grp_b'][l], W['moe_exp_w'][l], W['moe_exp_b'][l],
                          W['moe_w1'][l], W['moe_w3'][l], W['moe_w2'][l], ident, identb, t_lo=t_lo)
        if on('final'):
            stage_final(K, lat, W['final_norm_g'], out)
        with nc.Block() as block:
            K.S.finalize(block)
    return nc, K


def kernel(**inputs):
    cfg = make_cfg()
    nc, _ = build_program(cfg)
    cst = make_cst()
    f32 = lambda a: np.ascontiguousarray(np.asarray(a, dtype=np.float32))
    shared = {n: f32(inputs[n]) for n, _ in PARAMS}
    shared["c_ctx"] = f32(inputs["c_ctx"])
    shared["cst"] = cst
    in_maps = []
    for b in range(cfg['BATCH']):
        m = dict(shared)
        m["x"] = f32(inputs["x"][b])
        m["ctx"] = f32(inputs["ctx"][b])
        m["c"] = f32(inputs["c"][b])
        in_maps.append(m)
    res = run_bass_kernel_spmd(nc, in_maps, core_ids=list(range(cfg['BATCH'])))
    return np.stack([np.asarray(r["out"], dtype=np.float32) for r in res.results], axis=0)


def conv_tile(K, xs, wk, acc, P, col0, NCH, base, seglen, s0, perm, bias=None):
    for k in range(5):
        edge = (s0 + k - 2 < 0) or (s0 + k - 2 + 128 > seglen)
        if edge:
            K.memset(xs[k][:], 0.0, eng='pool')
        load_rows(K, xs[k][:], P, col0, NCH, base, seglen, s0 + k - 2, 128, perm)
    K.tt(acc[:], xs[0][:], wk[:, 0, :], ALU.mult)
    for k in range(1, 5):
        K.tt(xs[k][:], xs[k][:], wk[:, k, :], ALU.mult, eng='pool')
        K.tt(acc[:], acc[:], xs[k][:], ALU.add)
    if bias is not None:
        K.tt(acc[:], acc[:], bias, ALU.add)
    K.act(acc[:], acc[:], AF.Silu)


def l2norm_heads(K, x3, tmp3, ssq, nh, hd, scale=1.0):
    K.tt(tmp3, x3, x3, ALU.mult)
    K.red(ssq, tmp3, ALU.add)
    K.ts(ssq, ssq, 1e-6, ALU.add)
    K.act(ssq, ssq, AF.Sqrt)
    K.recip(ssq, ssq)
    if scale != 1.0:
        K.ts(ssq, ssq, scale, ALU.mult)
    K.tt(x3, x3, bc3(ssq, 2, [x3.shape[0], nh, hd]), ALU.mult)


def solve_nilpotent(K, Mb, Nb, Pw, Pm, pM, pN, pP, ident64, nh):
    K.tt(Pm, Mb[0], bc3(ident64, 1, [64, nh, 64]), ALU.add)
    K.cp(Pw, Pm, eng='act')
    cur = 0
    for lvl in range(5):
        nxt = 1 - cur
        for h in range(nh):
            K.mm(pN[:, h, :], Mb[cur][:, h, :], Nb[cur][:, h, :])
            if lvl < 4:
                K.mm(pM[:, h, :], Nb[cur][:, h, :], Mb[cur][:, h, :])
        K.cp(Nb[nxt], pN, eng='act')
        if lvl < 4:
            K.cp(Mb[nxt], pM, eng='dve')
        for h in range(nh):
            K.mm(pP[:, h, :], Nb[nxt][:, h, :], Pw[:, h, :])
        K.tt(Pm, Pm, pP, ALU.add)
        if lvl < 4:
            K.cp(Pw, Pm, eng='act')
        cur = nxt


def stage_gdn_prep(K, P, conv_w, A_log, dt_bias, gqkv, glog, gbeta):
    cfg = K.cfg
    GW, GH = cfg['GW'], cfg['GH']
    NCH = 3 * GW
    CTX, SEQ = cfg['CTX'], cfg['SEQ']
    oq = cfg['off']['g_q'][0]
    oa = cfg['off']['g_a'][0]
    ob = cfg['off']['g_b'][0]
    with contextlib.ExitStack() as es:
        wk = K.sb(es, "gcw", [128, 5, NCH])
        for k in range(5):
            bc_row(K, wk[:, k, :], conv_w[k, :])
        aneg = K.sb(es, "ganeg", [128, 2 * GH])
        bc_row(K, aneg[:], A_log.rearrange("a b -> (a b)"))
        K.act(aneg[:], aneg[:], AF.Exp)
        dtb = K.sb(es, "gdtb", [128, 2 * GH])
        bc_row(K, dtb[:], dt_bias.rearrange("a b -> (a b)"))
        xs = [K.sb(es, f"gx{k}", [128, NCH]) for k in range(5)]
        acc = K.sb(es, "gacc", [128, NCH])
        ssq = K.sb(es, "gssq", [128, 2 * GH])
        la = K.sb(es, "gla", [128, 2 * GH])
        bt = K.sb(es, "gbt", [128, 2 * GH])
        for (base, seglen) in [(0, CTX), (CTX, SEQ)]:
            for s0 in range(0, seglen, 128):
                conv_tile(K, xs, wk, acc, P, oq, NCH, base, seglen, s0, None)
                qk3 = acc[:, 0:2 * GW].rearrange("p (h e) -> p h e", e=128)
                tmp3 = xs[0][:, 0:2 * GW].rearrange("p (h e) -> p h e", e=128)
                l2norm_heads(K, qk3, tmp3, ssq[:], 2 * GH, 128)
                K.ts(acc[:, 0:GW], acc[:, 0:GW], 128.0 ** -0.5, ALU.mult)
                rs = slice(base + s0, base + s0 + 128)
                K.dma(gqkv[rs, :], acc[:], q='sp')
                K.dma(la[:], P[rs, oa:oa + 2 * GH])
                softplus_(K, la[:], dtb[:])
                K.stt(la[:], la[:], -1.0, aneg[:], ALU.mult, ALU.mult)
                K.dma(glog[rs, :], la[:], q='sp')
                K.dma(bt[:], P[rs, ob:ob + 2 * GH])
                K.act(bt[:], bt[:], AF.Sigmoid)
                K.dma(gbeta[rs, :], bt[:], q='sp')
        K.S.barrier()


def stage_gdn_scan(K, cst, gqkv, glog, gbeta, ygd):
    cfg = K.cfg
    GW, GH = cfg['GW'], cfg['GH']
    CTX, T = cfg['CTX'], cfg['T']
    nchunk = T // 64
    HN = min(4, GH)
    ident, ones = cst[:, 0, :], cst[:, 1, :]
    i64 = cst[0:64, 0, 0:64]
    with contextlib.ExitStack() as es:
        pb = [K.ps(es, f"dpb{i}", [128, 512]) for i in range(7)]

        def chain(dsel):
            H = K.sb(es, "dH", [128, HN, 128])
            qkv = [K.sb(es, f"dqkv{i}", [64, 3, HN * 128]) for i in range(2)]
            lgt = [K.sb(es, f"dlg{i}", [64, 2 * GH]) for i in range(2)]
            btt = [K.sb(es, f"dbt{i}", [64, 2 * GH]) for i in range(2)]
            kqT = K.sb(es, "dkqT", [128, 2 * HN, 64])
            Z = K.sb(es, "dZ", [64, HN, 64])
            G = K.sb(es, "dG", [64, HN])
            eG = K.sb(es, "deG", [64, HN])
            eE = K.sb(es, "deE", [64, HN])
            gC = K.sb(es, "dgC", [128, HN])
            beG = K.sb(es, "dbeG", [64, HN])
            E = K.sb(es, "dE", [64, HN, 64])
            Dt = K.sb(es, "dDt", [64, HN, 64])
            Dn = K.sb(es, "dDn", [64, HN, 64])
            Mb = [K.sb(es, f"dM{i}", [64, HN, 64], BF16) for i in range(2)]
            Nb = [K.sb(es, f"dN{i}", [64, HN, 64], BF16) for i in range(2)]
            N0 = K.sb(es, "dN0", [64, HN, 64])
            Pw = K.sb(es, "dPw", [64, HN, 64], BF16)
            Pm = K.sb(es, "dPm", [64, HN, 64])
            A1 = K.sb(es, "dA1", [64, HN, 64])
            bv = K.sb(es, "dbv", [64, HN, 128])
            bke = K.sb(es, "dbke", [64, HN, 128])
            u = K.sb(es, "du", [64, HN, 128])
            w = K.sb(es, "dw", [64, HN, 128])
            wT = K.sb(es, "dwT", [128, HN, 64])
            vn = K.sb(es, "dvn", [64, HN, 128])
            vE = K.sb(es, "dvE", [64, HN, 128])
            o = K.sb(es, "do", [64, HN, 128])

            def v64(t, n, e):
                return t[0:64, 0:n * e].rearrange("p (a b) -> p a b", b=e)

            def v128(t, n, e):
                return t[:, 0:n * e].rearrange("p (a b) -> p a b", b=e)
            p_g = v128(pb[0], 4, HN)
            p_gb = v64(pb[1], HN, 64)
            p_tr = v128(pb[2], 2 * HN, 64)
            p_gr = v64(pb[3], 2 * HN, 64)
            pM, pN, pP = v64(pb[4], HN, 64), v64(pb[5], HN, 64), v64(pb[6], HN, 64)
            p_u, p_w = v64(pb[1], HN, 128), v64(pb[3], HN, 128)
            p_mt = v64(pb[2], HN, 64)
            p_wt = v128(pb[2], HN, 64)
            p_v, p_a, p_b = v64(pb[4], HN, 128), v64(pb[5], HN, 128), v64(pb[6], HN, 128)
            p_h = v128(pb[1], HN, 128)
            for hg in range(GH // HN):
                hc = slice(hg * HN * 128, (hg + 1) * HN * 128)
                for d in (dsel,):
                    tri = cst[0:64, 2 + d, 0:64]
                    strict_n = cst[0:64, 4 + (1 - d), 0:64]
                    K.memset(H[:], 0.0)
                    order = list(range(nchunk))
                    if d == 1:
                        order = list(range(CTX // 64 - 1, -1, -1)) + list(range(nchunk - 1, CTX // 64 - 1, -1))
                    for ci, c in enumerate(order):
                        x = qkv[ci % 2]
                        lg_, bt_ = lgt[ci % 2], btt[ci % 2]
                        rs = slice(c * 64, (c + 1) * 64)
                        for j in range(3):
                            K.dma(x[:, j, :], gqkv[rs, j * GW + hg * HN * 128:j * GW + (hg + 1) * HN * 128])
                        K.dma(lg_[:], glog[rs, :])
                        K.dma(bt_[:], gbeta[rs, :])
                        q3 = x[:, 0, :].rearrange("p (h e) -> p h e", e=128)
                        k3 = x[:, 1, :].rearrange("p (h e) -> p h e", e=128)
                        v3 = x[:, 2, :].rearrange("p (h e) -> p h e", e=128)
                        lgd = lg_[:, d * GH + hg * HN:d * GH + (hg + 1) * HN]
                        btd = bt_[:, d * GH + hg * HN:d * GH + (hg + 1) * HN]
                        K.mm(p_g[0:64, 0, :], tri, lgd)
                        K.mm(p_g[0:64, 1, :], ones[0:64, 0:64], lgd)
                        K.mm(p_g[:, 2, :], ones[0:64, :], lgd)
                        K.cp(G[:], p_g[0:64, 0, :])
                        K.act(eG[:], p_g[0:64, 0, :], AF.Exp)
                        K.tt(eE[:], p_g[0:64, 1, :], G[:], ALU.subtract)
                        K.act(eE[:], eE[:], AF.Exp)
                        K.act(gC[:], p_g[:, 2, :], AF.Exp)
                        K.tt(Z[:], bc3(lgd, 2, [64, HN, 64]), bc3(tri, 1, [64, HN, 64]), ALU.mult, eng='pool')
                        K.mm(p_gb, ones[0:64, 0:64], Z[:])
                        K.tt(E[:], p_gb, bc3(G[:], 2, [64, HN, 64]), ALU.subtract)
                        K.ts(Dt[:], E[:], 0.0, ALU.min)
                        K.act(Dt[:], Dt[:], AF.Exp)
                        K.ts(Dn[:], E[:], -1.0, ALU.mult, 0.0, ALU.min)
                        K.act(Dn[:], Dn[:], AF.Exp)
                        for h in range(HN):
                            K.tr(p_tr[:, h, :], k3[:, h, :], i64)
                            K.tr(p_tr[:, HN + h, :], q3[:, h, :], i64)
                        K.cp(kqT[:], p_tr, eng='act')
                        for h in range(HN):
                            K.mm(p_gr[:, h, :], kqT[:, h, :], kqT[:, h, :])
                            K.mm(p_gr[:, HN + h, :], kqT[:, h, :], kqT[:, HN + h, :])
                        K.tt(N0[:], p_gr[:, 0:HN, :], Dn[:], ALU.mult)
                        K.tt(N0[:], N0[:], bc3(btd, 2, [64, HN, 64]), ALU.mult)
                        K.stt(N0[:], N0[:], -1.0, bc3(strict_n, 1, [64, HN, 64]), ALU.mult, ALU.mult)
                        K.cp(Nb[0][:], N0[:], eng='pool')
                        K.tt(A1[:], p_gr[:, HN:2 * HN, :], Dt[:], ALU.mult)
                        K.tt(A1[:], A1[:], bc3(tri, 1, [64, HN, 64]), ALU.mult)
                        for h in range(HN):
                            K.tr(p_mt[:, h, :], N0[:, h, :], i64)
                        K.cp(Mb[0][:], p_mt, eng='act')
                        solve_nilpotent(K, [Mb[0][:], Mb[1][:]], [Nb[0][:], Nb[1][:]], Pw[:], Pm[:], pM, pN, pP, i64, HN)
                        K.tt(bv[:], v3, bc3(btd, 2, [64, HN, 128]), ALU.mult, eng='pool')
                        K.tt(beG[:], btd, eG[:], ALU.mult)
                        K.tt(bke[:], k3, bc3(beG[:], 2, [64, HN, 128]), ALU.mult, eng='pool')
                        for h in range(HN):
                            K.mm(p_u[:, h, :], Pm[:, h, :], bv[:, h, :])
                            K.mm(p_w[:, h, :], Pm[:, h, :], bke[:, h, :])
                        K.cp(u[:], p_u, eng='act')
                        K.cp(w[:], p_w, eng='dve')
                        for h in range(HN):
                            K.tr(p_wt[:, h, :], w[:, h, :], i64)
                        K.cp(wT[:], p_wt, eng='act')
                        for h in range(HN):
                            K.mm(p_v[:, h, :], wT[:, h, :], H[:, h, :])
                            K.mm(p_a[:, h, :], kqT[:, HN + h, :], H[:, h, :])
                        K.tt(vn[:], u[:], p_v, ALU.subtract)
                        for h in range(HN):
                            K.mm(p_b[:, h, :], A1[:, h, :], vn[:, h, :])
                        K.tt(vE[:], vn[:], bc3(eE[:], 2, [64, HN, 128]), ALU.mult, eng='pool')
                        for h in range(HN):
                            K.mm(p_h[:, h, :], k3[:, h, :], vE[:, h, :])
                        K.tt(o[:], p_a, bc3(eG[:], 2, [64, HN, 128]), ALU.mult)
                        K.tt(o[:], o[:], p_b, ALU.add)
                        K.dma(ygd[d][rs, hc], o[:].rearrange("p h e -> p (h e)"), q='sp')
                        K.tt(H[:], H[:], bc3(gC[:], 2, [128, HN, 128]), ALU.mult)
                        K.tt(H[:], H[:], p_h, ALU.add)
                        yield
        alive = [chain(0), chain(1)]
        while alive:
            for g in list(alive):
                try:
                    next(g)
                except StopIteration:
                    alive.remove(g)
        K.S.barrier()


def stage_gdn_finish(K, P, ygd, norm_g, ygf):
    cfg = K.cfg
    GW, GH = cfg['GW'], cfg['GH']
    T = cfg['T']
    og = cfg['off']['g_g'][0]
    with contextlib.ExitStack() as es:
        ng = K.sb(es, "gng", [128, 128])
        bc_row(K, ng[:], norm_g)
        y0 = [K.sb(es, f"hy0{i}", [128, GW]) for i in range(2)]
        y1 = [K.sb(es, f"hy1{i}", [128, GW]) for i in range(2)]
        gg = [K.sb(es, f"hgg{i}", [128, GW]) for i in range(2)]
        tmp = K.sb(es, "htmp", [128, GW])
        ssq = K.sb(es, "hssq", [128, GH])
        for i, t0 in enumerate(range(0, T, 128)):
            a, b, g = y0[i % 2], y1[i % 2], gg[i % 2]
            rs = slice(t0, t0 + 128)
            K.dma(a[:], ygd[0][rs, :])
            K.dma(b[:], ygd[1][rs, :])
            K.dma(g[:], P[rs, og:og + GW])
            K.tt(a[:], a[:], b[:], ALU.add)
            a3 = a[:].rearrange("p (h e) -> p h e", e=128)
            t3 = tmp[:].rearrange("p (h e) -> p h e", e=128)
            K.tt(t3, a3, a3, ALU.mult)
            K.red(ssq[:], t3, ALU.add)
            K.rsqrt(es, ssq[:], ssq[:], 1.0 / 128, 1e-6)
            K.tt(a3, a3, bc3(ssq[:], 2, [128, GH, 128]), ALU.mult)
            K.tt(a3, a3, bc3(ng[:], 1, [128, GH, 128]), ALU.mult, eng='pool')
            K.act(g[:], g[:], AF.Silu)
            K.tt(a[:], a[:], g[:], ALU.mult)
            K.dma(ygf[rs, :], a[:], q='sp')
        K.S.barrier()


def stage_rwkv_prep(K, cst, P, shift_mu, w0, w_up, a0, a_up, g_up, k_k, k_a, r_k, RA):
    cfg = K.cfg
    RW, RH = cfg['RW'], cfg['RH']
    CTX, SEQ = cfg['CTX'], cfg['SEQ']
    orr = cfg['off']['r_r'][0]
    ow = cfg['off']['r_w'][0]
    ident = cst[:, 0, :]
    NB = (RW + 511) // 512
    with contextlib.ExitStack() as es:
        mu = K.sb(es, "rmu", [128, 3, 3 * RW])
        bc_row(K, mu[:, 0, :], shift_mu[0, :])
        bc_row(K, mu[:, 1, :], shift_mu[1, :])
        K.tt(mu[:, 2, :], mu[:, 0, :], mu[:, 1, :], ALU.add)
        K.ts(mu[:, 2, :], mu[:, 2, :], -1.0, ALU.mult, 1.0, ALU.add)
        pv = K.sb(es, "rpv", [128, 5, RW])
        bc_row(K, pv[:, 0, :], w0[0, :])
        bc_row(K, pv[:, 1, :], w0[1, :])
        bc_row(K, pv[:, 2, :], a0)
        bc_row(K, pv[:, 3, :], k_k)
        bc_row(K, pv[:, 4, :], k_a)
        rkb = K.sb(es, "rrk", [128, RW])
        bc_row(K, rkb[:], r_k.rearrange("a b -> (a b)"))
        wu = K.sb(es, "rwu", [64, 3, RW])
        K.dma(wu[:, 0, :], w_up[0])
        K.dma(wu[:, 1, :], w_up[1])
        K.dma(wu[:, 2, :], a_up)
        gu = K.sb(es, "rgu", [128, RW])
        K.dma(gu[:], g_up)
        x = K.sb(es, "rx", [128, 3 * RW])
        xp = K.sb(es, "rxp", [128, 3 * RW])
        xn = K.sb(es, "rxn", [128, 3 * RW])
        lo = K.sb(es, "rlo", [128, 320])
        loT = K.sb(es, "rloT", [128, 4, 128])
        t1 = K.sb(es, "rt1", [128, RW])
        t2 = K.sb(es, "rt2", [128, RW])
        av = K.sb(es, "rav", [128, RW])
        ssq = K.sb(es, "rssq", [128, RH])
        p_tr = K.ps(es, "rp_tr", [128, 4, 128])
        p_l = [K.ps(es, f"rp_l{i}", [128, 512]) for i in range(2)]
        li = 0

        def lora(dst, lhsT, rhs):
            nonlocal li
            for nb in range(NB):
                n0, n1 = nb * 512, min(RW, (nb + 1) * 512)
                p = p_l[li % 2]
                li += 1
                K.mm(p[:, 0:n1 - n0], lhsT, rhs[:, n0:n1])
                K.cp(dst[:, n0:n1], p[:, 0:n1 - n0], eng='act')
        for (base, seglen) in [(0, CTX), (CTX, SEQ)]:
            for s0 in range(0, seglen, 128):
                rs = slice(base + s0, base + s0 + 128)
                K.dma(x[:], P[rs, orr:orr + 3 * RW])
                if s0 == 0:
                    K.memset(xp[:], 0.0, eng='pool')
                if s0 + 128 >= seglen:
                    K.memset(xn[:], 0.0, eng='pool')
                load_rows(K, xp[:], P, orr, 3 * RW, base, seglen, s0 - 1, 128)
                load_rows(K, xn[:], P, orr, 3 * RW, base, seglen, s0 + 1, 128)
                K.tt(x[:], x[:], mu[:, 2, :], ALU.mult)
                K.tt(xp[:], xp[:], mu[:, 0, :], ALU.mult, eng='pool')
                K.tt(xn[:], xn[:], mu[:, 1, :], ALU.mult, eng='pool')
                K.tt(x[:], x[:], xp[:], ALU.add)
                K.tt(x[:], x[:], xn[:], ALU.add)
                r_, k_, v_ = x[:, 0:RW], x[:, RW:2 * RW], x[:, 2 * RW:3 * RW]
                K.dma(RA['r'][rs, :], r_, q='sp')
                K.dma(RA['v'][rs, :], v_, q='sp')
                K.dma(lo[:], P[rs, ow:ow + 320])
                K.act(lo[:, 0:128], lo[:, 0:128], AF.Tanh)
                K.act(lo[:, 192:320], lo[:, 192:320], AF.Sigmoid)
                K.tr(p_tr[0:64, 0, :], lo[:, 0:64], ident)
                K.tr(p_tr[0:64, 1, :], lo[:, 64:128], ident)
                K.tr(p_tr[0:64, 2, :], lo[:, 128:192], ident)
                K.tr(p_tr[:, 3, :], lo[:, 192:320], ident)
                K.cp(loT[0:64, 0:3, :], p_tr[0:64, 0:3, :], eng='act')
                K.cp(loT[:, 3, :], p_tr[:, 3, :], eng='dve')
                for d in range(2):
                    lora(t1, loT[0:64, d, :], wu[:, d, :])
                    K.tt(t1[:], t1[:], pv[:, d, :], ALU.add)
                    K.act(t1[:], t1[:], AF.Sigmoid)
                    K.ts(t1[:], t1[:], -math.exp(-0.5), ALU.mult)
                    K.dma(RA['lw'][rs, d * RW:(d + 1) * RW], t1[:], q='sp')
                lora(av, loT[0:64, 2, :], wu[:, 2, :])
                K.tt(av[:], av[:], pv[:, 2, :], ALU.add)
                K.act(av[:], av[:], AF.Sigmoid)
                K.dma(RA['a'][rs, :], av[:], q='sp')
                lora(t2, loT[:, 3, :], gu[:])
                K.dma(RA['g'][rs, :], t2[:], q='sp')
                K.tt(t1[:], k_, pv[:, 3, :], ALU.mult)
                l2norm_heads(K, t1[:].rearrange("p (h e) -> p h e", e=64), t2[:].rearrange("p (h e) -> p h e", e=64),
                             ssq[:], RH, 64)
                K.dma(RA['kk'][rs, :], t1[:], q='sp')
                K.stt(t2[:], av[:], -1.0, pv[:, 4, :], ALU.add, ALU.mult)
                K.stt(t2[:], t2[:], 1.0, k_, ALU.add, ALU.mult)
                K.dma(RA['k'][rs, :], t2[:], q='sp')
                K.tt(t2[:], t2[:], r_, ALU.mult)
                K.tt(t2[:], t2[:], rkb[:], ALU.mult)
                K.red(ssq[:], t2[:].rearrange("p (h e) -> p h e", e=64), ALU.add)
                K.dma(RA['bon'][rs, :], ssq[:], q='sp')
        K.S.barrier()


def stage_rwkv_scan(K, cst, RA, yrd):
    cfg = K.cfg
    RW, RH = cfg['RW'], cfg['RH']
    CTX, T = cfg['CTX'], cfg['T']
    nchunk = T // 64
    HN = min(4, RH)
    W_ = HN * 64
    ones = cst[:, 1, :]
    i64 = cst[0:64, 0, 0:64]
    MID = 32
    with contextlib.ExitStack() as es:
        pb = [K.ps(es, f"wpb{i}", [64, 512]) for i in range(7)]

        def chain(dsel):
            H = K.sb(es, "wH", [64, HN, 64])
            Hs = K.sb(es, "wHs", [64, HN, 64])
            names = ('r', 'k', 'v', 'kk', 'a')
            IN = [{n: K.sb(es, f"w{n}{i}", [64, W_]) for n in names + ('lw',)} for i in range(2)]
            trimid = K.sb(es, "wtrimid", [64, 64])
            col2 = K.sb(es, "wcol2", [64, 2])
            Gm = K.sb(es, "wGm", [64, W_])
            eP = K.sb(es, "weP", [64, W_])
            eN = K.sb(es, "weN", [64, W_])
            eX = K.sb(es, "weX", [64, W_])
            eE = K.sb(es, "weE", [64, W_])
            gcm = K.sb(es, "wgcm", [64, HN, 2])
            bt = K.sb(es, "wbt", [64, W_])
            SC = K.sb(es, "wSC", [64, 4, W_])
            Ke = K.sb(es, "wKe", [64, W_])
            Be = K.sb(es, "wBe", [64, W_])
            FT = K.sb(es, "wFT", [64, 4, HN, 64])
            Mb = [K.sb(es, f"wM{i}", [64, HN, 64], BF16) for i in range(2)]
            Nb = [K.sb(es, f"wN{i}", [64, HN, 64], BF16) for i in range(2)]
            Pw = K.sb(es, "wPw", [64, HN, 64], BF16)
            Pm = K.sb(es, "wPm", [64, HN, 64])
            AK = K.sb(es, "wAK", [64, HN, 64])
            QK = K.sb(es, "wQK", [64, HN, 64])
            QB = K.sb(es, "wQB", [64, HN, 64])
            Z0 = K.sb(es, "wZ0", [64, HN, 64])
            Ut = K.sb(es, "wUt", [64, HN, 64])
            WT = K.sb(es, "wWT", [64, HN, 64])
            U = K.sb(es, "wU", [64, HN, 64])
            Y = K.sb(es, "wY", [64, HN, 64])

            def v3(t, n):
                return t[:, 0:n * 64].rearrange("p (a b) -> p a b", b=64)
            old_ser = K.S.ser_engs
            import os
            sf, st_ = int(os.environ.get('RW_SER_FROM', '0')), int(os.environ.get('RW_SER_TO', '0'))

            def sec(i):
                K.S.ser_engs = tuple(set(old_ser) | {'act', 'dve'}) if sf <= i < st_ else old_ser
            for hg in range(RH // HN):
                hc = slice(hg * W_, (hg + 1) * W_)
                for d in (dsel,):
                    tri = cst[0:64, 2 + d, 0:64]
                    stri = cst[0:64, 4 + d, 0:64]
                    strict_n = cst[0:64, 4 + (1 - d), 0:64]
                    K.ts(trimid[:], tri, tri[:, MID:MID + 1], ALU.subtract)
                    K.cp(col2[:, 0:1], ones[0:64, 0:1])
                    K.cp(col2[:, 1:2], tri[:, MID:MID + 1])
                    K.memset(H[:], 0.0)
                    order = list(range(nchunk))
                    if d == 1:
                        order = list(range(CTX // 64 - 1, -1, -1)) + list(range(nchunk - 1, CTX // 64 - 1, -1))
                    for ci, c in enumerate(order):
                        X = IN[ci % 2]
                        rs = slice(c * 64, (c + 1) * 64)
                        for n in names:
                            K.dma(X[n][:], RA[n][rs, hc])
                        K.dma(X['lw'][:], RA['lw'][rs, d * RW + hg * W_:d * RW + (hg + 1) * W_])
                        sec(0)
                        lw = X['lw'][:]
                        K.mm(pb[0][:, 0:W_], trimid[:], lw)
                        K.mm(pb[0][:, W_:2 * W_], strict_n, lw)
                        pgc = pb[1][:, 0:2 * HN].rearrange("p (a b) -> p a b", b=2)
                        for h in range(HN):
                            K.mm(pgc[:, h, :], X['lw'][:, h * 64:(h + 1) * 64], col2[:])
                        K.cp(Gm[:], pb[0][:, 0:W_])
                        K.act(eP[:], pb[0][:, 0:W_], AF.Exp)
                        K.ts(eN[:], Gm[:], -1.0, ALU.mult)
                        K.act(eN[:], eN[:], AF.Exp)
                        K.act(eE[:], pb[0][:, W_:2 * W_], AF.Exp)
                        K.tt(eX[:], Gm[:], lw, ALU.subtract)
                        K.act(eX[:], eX[:], AF.Exp)
                        K.act(gcm[:], pgc, AF.Exp)
                        sec(1)
                        K.tt(bt[:], X['kk'][:], X['a'][:], ALU.mult, eng='pool')
                        K.tt(SC[:, 0, :], X['k'][:], eN[:], ALU.mult)
                        K.tt(SC[:, 1, :], bt[:], eN[:], ALU.mult)
                        K.stt(SC[:, 2, :], X['kk'][:], -1.0, eX[:], ALU.mult, ALU.mult)
                        K.tt(SC[:, 3, :], X['r'][:], eP[:], ALU.mult)
                        K.tt(Ke[:], X['k'][:], eE[:], ALU.mult, eng='pool')
                        K.tt(Be[:], bt[:], eE[:], ALU.mult, eng='pool')
                        K.tt(Hs[:], H[:], bc3(gcm[:, :, 1], 2, [64, HN, 64]), ALU.mult)
                        sec(2)
                        for j in range(4):
                            pt = pb[2 + (j % 2)]
                            for h in range(HN):
                                K.tr(v3(pt, HN)[:, h, :], SC[:, j, h * 64:(h + 1) * 64], i64)
                            K.cp(FT[:, j, :, :], v3(pt, HN), eng=('act' if j % 2 else 'dve'))
                        kT, bT, aT, qT = (FT[:, j, :, :] for j in range(4))
                        sec(3)
                        pMN = v3(pb[4], 2 * HN)
                        pG2 = v3(pb[5], 2 * HN)
                        pG3 = v3(pb[6], HN)
                        for h in range(HN):
                            K.mm(pMN[:, h, :], bT[:, h, :], aT[:, h, :])
                            K.mm(pMN[:, HN + h, :], aT[:, h, :], bT[:, h, :])
                            K.mm(pG2[:, h, :], kT[:, h, :], aT[:, h, :])
                            K.mm(pG2[:, HN + h, :], kT[:, h, :], qT[:, h, :])
                            K.mm(pG3[:, h, :], bT[:, h, :], qT[:, h, :])
                        K.tt(Mb[0][:], pMN[:, 0:HN, :], bc3(stri, 1, [64, HN, 64]), ALU.mult)
                        K.tt(Nb[0][:], pMN[:, HN:2 * HN, :], bc3(strict_n, 1, [64, HN, 64]), ALU.mult)
                        K.tt(AK[:], pG2[:, 0:HN, :], bc3(stri, 1, [64, HN, 64]), ALU.mult)
                        K.tt(QK[:], pG2[:, HN:2 * HN, :], bc3(tri, 1, [64, HN, 64]), ALU.mult)
                        K.tt(QB[:], pG3, bc3(tri, 1, [64, HN, 64]), ALU.mult)
                        sec(4)
                        solve_nilpotent(K, [Mb[0][:], Mb[1][:]], [Nb[0][:], Nb[1][:]], Pw[:], Pm[:],
                                        v3(pb[2], HN), v3(pb[3], HN), v3(pb[4], HN), i64, HN)
                        sec(5)
                        pz = v3(pb[5], HN)
                        for h in range(HN):
                            K.mm(pz[:, h, :], AK[:, h, :], X['v'][:, h * 64:(h + 1) * 64])
                        K.cp(Z0[:], pz, eng='act')
                        pu = v3(pb[6], HN)
                        pw = v3(pb[5], HN)
                        for h in range(HN):
                            K.mm(pu[:, h, :], Pm[:, h, :], Z0[:, h, :])
                            K.mm(pw[:, h, :], SC[:, 2, h * 64:(h + 1) * 64], Pm[:, h, :])
                        K.cp(Ut[:], pu, eng='act')
                        K.cp(WT[:], pw, eng='dve')
                        sec(6)
                        p1 = v3(pb[2], HN)
                        for h in range(HN):
                            K.mm(p1[:, h, :], WT[:, h, :], Hs[:, h, :])
                        K.tt(U[:], Ut[:], p1, ALU.add)
                        py = v3(pb[3], HN)
                        ph = v3(pb[4], HN)
                        for h in range(HN):
                            vh = X['v'][:, h * 64:(h + 1) * 64]
                            K.mm(py[:, h, :], qT[:, h, :], Hs[:, h, :], start=True, stop=False)
                            K.mm(py[:, h, :], QB[:, h, :], U[:, h, :], start=False, stop=False)
                            K.mm(py[:, h, :], QK[:, h, :], vh, start=False, stop=True)
                        for h in range(HN):
                            vh = X['v'][:, h * 64:(h + 1) * 64]
                            K.mm(ph[:, h, :], Be[:, h * 64:(h + 1) * 64], U[:, h, :], start=True, stop=False)
                            K.mm(ph[:, h, :], Ke[:, h * 64:(h + 1) * 64], vh, start=False, stop=True)
                        K.cp(Y[:], py, eng='act')
                        K.dma(yrd[d][rs, hc], Y[:].rearrange("p h e -> p (h e)"), q='sp')
                        K.tt(H[:], H[:], bc3(gcm[:, :, 0], 2, [64, HN, 64]), ALU.mult)
                        K.tt(H[:], H[:], ph, ALU.add)
                        yield
        alive = [chain(0), chain(1)]
        while alive:
            for g in list(alive):
                try:
                    next(g)
                except StopIteration:
                    alive.remove(g)
        K.S.ser_engs = ()
        K.S.barrier()


def stage_rwkv_finish(K, RA, yrd, ln_g, ln_b, yrf):
    cfg = K.cfg
    RW, RH = cfg['RW'], cfg['RH']
    T = cfg['T']
    with contextlib.ExitStack() as es:
        lg = K.sb(es, "vlg", [128, RW])
        lb = K.sb(es, "vlb", [128, RW])
        bc_row(K, lg[:], ln_g)
        bc_row(K, lb[:], ln_b)
        y0 = [K.sb(es, f"vy0{i}", [128, RW]) for i in range(2)]
        y1 = [K.sb(es, f"vy1{i}", [128, RW]) for i in range(2)]
        vv = [K.sb(es, f"vvv{i}", [128, RW]) for i in range(2)]
        gg = [K.sb(es, f"vgg{i}", [128, RW]) for i in range(2)]
        bo = [K.sb(es, f"vbo{i}", [128, RH]) for i in range(2)]
        tmp = K.sb(es, "vtmp", [128, RW])
        st = K.sb(es, "vst", [128, 2, RH])
        for i, t0 in enumerate(range(0, T, 128)):
            a, b, v, g, bn = y0[i % 2], y1[i % 2], vv[i % 2], gg[i % 2], bo[i % 2]
            rs = slice(t0, t0 + 128)
            K.dma(a[:], yrd[0][rs, :])
            K.dma(b[:], yrd[1][rs, :])
            K.dma(v[:], RA['v'][rs, :])
            K.dma(g[:], RA['g'][rs, :])
            K.dma(bn[:], RA['bon'][rs, :])
            K.tt(a[:], a[:], b[:], ALU.add)
            a3 = a[:].rearrange("p (h e) -> p h e", e=64)
            t3 = tmp[:].rearrange("p (h e) -> p h e", e=64)
            K.red(st[:, 0, :], a3, ALU.add)
            K.ts(st[:, 0, :], st[:, 0, :], 1.0 / 64, ALU.mult)
            K.tt(a3, a3, bc3(st[:, 0, :], 2, [128, RH, 64]), ALU.subtract)
            K.tt(t3, a3, a3, ALU.mult)
            K.red(st[:, 1, :], t3, ALU.add)
            K.rsqrt(es, st[:, 1, :], st[:, 1, :], 1.0 / 64, 64e-5)
            K.tt(a3, a3, bc3(st[:, 1, :], 2, [128, RH, 64]), ALU.mult)
            K.tt(a[:], a[:], lg[:], ALU.mult, eng='pool')
            K.tt(a[:], a[:], lb[:], ALU.add)
            v3_ = v[:].rearrange("p (h e) -> p h e", e=64)
            K.tt(v3_, v3_, bc3(bn[:], 2, [128, RH, 64]), ALU.mult, eng='pool')
            K.tt(a[:], a[:], v[:], ALU.add)
            K.tt(a[:], a[:], g[:], ALU.mult)
            K.dma(yrf[rs, :], a[:], q='sp')
        K.S.barrier()
```
